# Optimizing a Trainium2 kernel written in Bass

```python
import math
import jax, jax.numpy as jnp
from jax import lax
import numpy as np


D_MODEL = 1024
BATCH = 8
SEQ = 8192
DEPTH = 4

MLSTM_HEADS = 4
MLSTM_QK_DIM = 64
MLSTM_V_DIM = 128
MLSTM_CONV = 4
MLSTM_CHUNK = 64
DSA_HEADS = 8
DSA_HEAD_DIM = 64
DSA_LATENT = 128
IDX_HEADS = 8
IDX_DIM = 32
IDX_TOPK_MAX = 256
QUERY_BLOCK = 128
REL_BUCKETS = 32
REL_MAX_DIST = 128
D_FF = 2816
N_EXPERTS = 8
TOP_K = 2
D_FF_EXPERT = 3584
MOE_BLOCK = 256
N_DENSE = (DEPTH + 1) // 2
N_MOE = DEPTH // 2
DN_ALPHA = (2 * DEPTH) ** 0.25
DN_BETA = (8 * DEPTH) ** -0.25
LN_EPS = 1e-5

A_QK = 2 * MLSTM_HEADS * MLSTM_QK_DIM
A_V = MLSTM_HEADS * MLSTM_V_DIM
A_GATE = 2 * MLSTM_HEADS
B_Q = DSA_HEADS * DSA_HEAD_DIM
I_Q = IDX_HEADS * IDX_DIM
PROJ_SIZES = (A_QK, A_V, A_V, A_GATE, B_Q, DSA_LATENT, I_Q, IDX_DIM, IDX_HEADS, D_MODEL, D_MODEL)
D_IN_PROJ = sum(PROJ_SIZES)

kernel_name = 'hybrid_mlstm_dsa_moe_deepnorm'


def layer_norm(x, g, b):
    xf = x.astype(jnp.float32)
    mu = jnp.mean(xf, -1, keepdims=True)
    var = jnp.mean(jnp.square(xf - mu), -1, keepdims=True)
    return ((xf - mu) * lax.rsqrt(var + LN_EPS)).astype(x.dtype) * g + b


def rms_norm(x, g):
    xf = x.astype(jnp.float32)
    return (xf * lax.rsqrt(jnp.mean(jnp.square(xf), -1, keepdims=True) + LN_EPS)).astype(x.dtype) * g


def causal_depthwise_conv(u, w):
    taps, s = w.shape[0], u.shape[1]
    up = jnp.pad(u, ((0, 0), (taps - 1, 0), (0, 0)))
    return sum(up[:, j:j + s] * w[j] for j in range(taps))


def mlstm_chunkwise(q, k, v, i_pre, log_f):
    bsz, s, h, dk = q.shape
    dv = v.shape[-1]
    ch = MLSTM_CHUNK
    nc = s // ch
    to_c = lambda a: a.reshape(bsz, nc, ch, h, -1).transpose(1, 0, 3, 2, 4)
    to_cg = lambda a: a.reshape(bsz, nc, ch, h).transpose(1, 0, 3, 2)
    causal = jnp.tril(jnp.ones((ch, ch), dtype=bool))

    def step(carry, inp):
        c_st, n_st, m_st = carry
        qc, kc, vc, ic, fc = inp
        b = jnp.cumsum(fc, axis=-1)
        d_log = jnp.where(causal, b[..., :, None] - b[..., None, :] + ic[..., None, :], -jnp.inf)
        inter = b + m_st[..., None]
        m_t = jnp.maximum(inter, jnp.max(d_log, -1))
        s_w = jnp.einsum('bhtd,bhsd->bhts', qc, kc) * jnp.exp(d_log - m_t[..., None])
        inter_w = jnp.exp(inter - m_t)
        num = jnp.einsum('bhts,bhsv->bhtv', s_w, vc) + inter_w[..., None] * jnp.einsum('bhtd,bhdv->bhtv', qc, c_st)
        den = jnp.sum(s_w, -1) + inter_w * jnp.einsum('bhtd,bhd->bht', qc, n_st)
        h_out = num / jnp.maximum(jnp.abs(den), jnp.exp(-m_t))[..., None]
        b_last = b[..., -1]
        g_log = b_last[..., None] - b + ic
        m_new = jnp.maximum(b_last + m_st, jnp.max(g_log, -1))
        decay = jnp.exp(b_last + m_st - m_new)
        w_k = kc * jnp.exp(g_log - m_new[..., None])[..., None]
        c_new = decay[..., None, None] * c_st + jnp.einsum('bhsd,bhsv->bhdv', w_k, vc)
        n_new = decay[..., None] * n_st + jnp.sum(w_k, -2)
        return (c_new, n_new, m_new), h_out

    init = (jnp.zeros((bsz, h, dk, dv), jnp.float32), jnp.zeros((bsz, h, dk), jnp.float32),
            jnp.zeros((bsz, h), jnp.float32))
    _, hs = lax.scan(step, init, (to_c(q), to_c(k), to_c(v), to_cg(i_pre), to_cg(log_f)))
    return hs.transpose(1, 0, 3, 2, 4).reshape(bsz, s, h, dv)


def t5_bucket(dist):
    dist = jnp.maximum(dist, 0)
    exact = REL_BUCKETS // 2
    log_ratio = jnp.log(jnp.maximum(dist, 1).astype(jnp.float32) / exact) / math.log(REL_MAX_DIST / exact)
    large = jnp.minimum(exact + (log_ratio * (REL_BUCKETS - exact)).astype(jnp.int32), REL_BUCKETS - 1)
    return jnp.where(dist < exact, dist, large)


def dsa_sparse_attention(q_lat, c_kv, q_idx, k_idx, w_idx, rel_bias):
    bsz, s, h, dc = q_lat.shape
    nb = s // QUERY_BLOCK
    topk = min(IDX_TOPK_MAX, s // 4)
    scale = DSA_HEAD_DIM ** -0.5
    spos = jnp.arange(s)
    blocks = lambda a: a.reshape((bsz, nb, QUERY_BLOCK) + a.shape[2:]).swapaxes(0, 1)

    def one_block(args):
        ql, qi, wi, t0 = args
        tpos = t0 + jnp.arange(QUERY_BLOCK)
        score = jnp.einsum('bqhs,bqh->bqs', jax.nn.relu(jnp.einsum('bqhd,bsd->bqhs', qi, k_idx)), wi)
        score = jnp.where(spos[None, None, :] <= tpos[None, :, None], score.astype(jnp.float32), -jnp.inf)
        _, idx = lax.top_k(score, topk)
        valid = idx <= tpos[None, :, None]
        c_sel = jax.vmap(lambda cb, ib: cb[ib])(c_kv, idx)
        logits = jnp.einsum('bqhc,bqkc->bqhk', ql, c_sel).astype(jnp.float32) * scale
        bias = rel_bias[t5_bucket(tpos[None, :, None] - idx)].transpose(0, 1, 3, 2)
        logits = jnp.where(valid[:, :, None, :], logits + bias.astype(jnp.float32), -jnp.inf)
        p = jax.nn.softmax(logits, axis=-1).astype(c_sel.dtype)
        return jnp.einsum('bqhk,bqkc->bqhc', p, c_sel)

    out = lax.map(one_block, (blocks(q_lat), blocks(q_idx), blocks(w_idx), jnp.arange(nb) * QUERY_BLOCK))
    return out.swapaxes(0, 1).reshape(bsz, s, h, dc)


def token_mixer(x, w_in, conv_w, gate_bias, norm_g, kv_norm_g, w_uk, w_uv, rel_bias,
                w_branch_a, w_branch_b, w_out):
    bsz, s, _ = x.shape
    offsets = np.cumsum(PROJ_SIZES)[:-1].tolist()
    a_qk, a_v, a_o, a_if, b_q, b_c, i_q, i_k, i_w, g_a, g_b = jnp.split(x @ w_in, offsets, axis=-1)
    qk = jax.nn.silu(causal_depthwise_conv(a_qk, conv_w)).astype(jnp.float32)
    q, k = jnp.split(qk.reshape(bsz, s, 2 * MLSTM_HEADS, MLSTM_QK_DIM), 2, axis=2)
    k = k * MLSTM_QK_DIM ** -0.5
    v = a_v.astype(jnp.float32).reshape(bsz, s, MLSTM_HEADS, MLSTM_V_DIM)
    gates = a_if.astype(jnp.float32).reshape(bsz, s, 2, MLSTM_HEADS) + gate_bias
    h = mlstm_chunkwise(q, k, v, gates[:, :, 0], jax.nn.log_sigmoid(gates[:, :, 1]))
    mu = jnp.mean(h, -1, keepdims=True)
    h = (h - mu) * lax.rsqrt(jnp.mean(jnp.square(h - mu), -1, keepdims=True) + LN_EPS)
    y_a = h.reshape(bsz, s, A_V).astype(x.dtype) * norm_g * jax.nn.sigmoid(a_o)
    q_lat = jnp.einsum('bshd,hdc->bshc', b_q.reshape(bsz, s, DSA_HEADS, DSA_HEAD_DIM), w_uk)
    c_kv = rms_norm(b_c, kv_norm_g)
    w_idx = i_w * (IDX_HEADS * IDX_DIM) ** -0.5
    o_lat = dsa_sparse_attention(q_lat, c_kv, i_q.reshape(bsz, s, IDX_HEADS, IDX_DIM), i_k, w_idx, rel_bias)
    y_b = jnp.einsum('bshc,hcd->bshd', o_lat, w_uv).reshape(bsz, s, B_Q)
    merged = jax.nn.sigmoid(g_a) * (y_a @ w_branch_a) + jax.nn.sigmoid(g_b) * (y_b @ w_branch_b)
    return merged @ w_out


def swiglu(x, wg, wu, wd):
    return (jax.nn.silu(x @ wg) * (x @ wu)) @ wd


def moe_swiglu(x2, router_w, wg, wu, wd):
    n, d = x2.shape
    logits = (x2 @ router_w).astype(jnp.float32)
    top_logit, top_e = lax.top_k(logits, TOP_K)
    gates = jax.nn.softmax(top_logit, axis=-1)
    flat_e = top_e.reshape(-1)
    flat_tok = jnp.repeat(jnp.arange(n, dtype=jnp.int32), TOP_K)
    flat_g = gates.reshape(-1)
    order = jnp.argsort(flat_e, stable=True)
    se, stok, sg = flat_e[order], flat_tok[order], flat_g[order]
    counts = jnp.bincount(flat_e, length=N_EXPERTS)
    starts = jnp.cumsum(counts) - counts
    padded = ((counts + MOE_BLOCK - 1) // MOE_BLOCK) * MOE_BLOCK
    pad_end = jnp.cumsum(padded)
    pad_start = pad_end - padded
    dest = pad_start[se] + (jnp.arange(n * TOP_K) - starts[se])
    total = n * TOP_K + N_EXPERTS * MOE_BLOCK
    nblk = total // MOE_BLOCK
    buf_tok = jnp.zeros((total,), jnp.int32).at[dest].set(stok)
    buf_g = jnp.zeros((total,), jnp.float32).at[dest].set(sg)
    blk_e = jnp.minimum(jnp.searchsorted(pad_end, jnp.arange(nblk) * MOE_BLOCK, side='right'), N_EXPERTS - 1)

    def run(args):
        tok, g, e = args
        xb = x2[tok]
        hb = jax.nn.silu(xb @ wg[e]) * (xb @ wu[e])
        return (hb @ wd[e]) * g[:, None].astype(x2.dtype)

    y = lax.map(run, (buf_tok.reshape(nblk, MOE_BLOCK), buf_g.reshape(nblk, MOE_BLOCK), blk_e))
    return jnp.zeros_like(x2).at[buf_tok].add(y.reshape(total, d))


def setup_inputs(seed: int = 0) -> dict:
    key = jax.random.key(seed)
    ks = jax.random.split(key, 24)
    nrm = lambda k, shape, sc: jax.random.normal(k, shape, jnp.float32) * sc
    D = D_MODEL
    gate_bias = jnp.stack([nrm(ks[3], (DEPTH, MLSTM_HEADS), 0.1),
                           3.0 + nrm(ks[4], (DEPTH, MLSTM_HEADS), 0.5)], axis=1)
    return {
        'x': nrm(ks[0], (BATCH, SEQ, D), 1.0),
        'w_in': nrm(ks[1], (DEPTH, D, D_IN_PROJ), D ** -0.5),
        'mlstm_conv_w': nrm(ks[2], (DEPTH, MLSTM_CONV, A_QK), MLSTM_CONV ** -0.5),
        'mlstm_gate_bias': gate_bias,
        'mlstm_norm_g': 1.0 + nrm(ks[5], (DEPTH, A_V), 0.02),
        'dsa_kv_norm_g': 1.0 + nrm(ks[6], (DEPTH, DSA_LATENT), 0.02),
        'dsa_w_uk': nrm(ks[7], (DEPTH, DSA_HEADS, DSA_HEAD_DIM, DSA_LATENT), DSA_HEAD_DIM ** -0.5),
        'dsa_w_uv': nrm(ks[8], (DEPTH, DSA_HEADS, DSA_LATENT, DSA_HEAD_DIM), DSA_LATENT ** -0.5),
        'rel_bias': nrm(ks[9], (REL_BUCKETS, DSA_HEADS), 0.5),
        'w_branch_a': nrm(ks[10], (DEPTH, A_V, D), A_V ** -0.5 * DN_BETA),
        'w_branch_b': nrm(ks[11], (DEPTH, B_Q, D), B_Q ** -0.5 * DN_BETA),
        'w_out': nrm(ks[12], (DEPTH, D, D), D ** -0.5 * DN_BETA),
        'ln_g': 1.0 + nrm(ks[13], (DEPTH, 2, D), 0.02),
        'ln_b': nrm(ks[14], (DEPTH, 2, D), 0.02),
        'dense_w_gate': nrm(ks[15], (N_DENSE, D, D_FF), D ** -0.5),
        'dense_w_up': nrm(ks[16], (N_DENSE, D, D_FF), D ** -0.5),
        'dense_w_down': nrm(ks[17], (N_DENSE, D_FF, D), D_FF ** -0.5 * DN_BETA),
        'router_w': nrm(ks[18], (N_MOE, D, N_EXPERTS), D ** -0.5),
        'expert_w_gate': nrm(ks[19], (N_MOE, N_EXPERTS, D, D_FF_EXPERT), D ** -0.5),
        'expert_w_up': nrm(ks[20], (N_MOE, N_EXPERTS, D, D_FF_EXPERT), D ** -0.5),
        'expert_w_down': nrm(ks[21], (N_MOE, N_EXPERTS, D_FF_EXPERT, D), D_FF_EXPERT ** -0.5 * DN_BETA),
    }


def reference(x, w_in, mlstm_conv_w, mlstm_gate_bias, mlstm_norm_g, dsa_kv_norm_g, dsa_w_uk, dsa_w_uv,
              rel_bias, w_branch_a, w_branch_b, w_out, ln_g, ln_b, dense_w_gate, dense_w_up, dense_w_down,
              router_w, expert_w_gate, expert_w_up, expert_w_down):
    for l in range(DEPTH):
        mix = token_mixer(x, w_in[l], mlstm_conv_w[l], mlstm_gate_bias[l], mlstm_norm_g[l], dsa_kv_norm_g[l],
                          dsa_w_uk[l], dsa_w_uv[l], rel_bias, w_branch_a[l], w_branch_b[l], w_out[l])
        x = layer_norm(DN_ALPHA * x + mix, ln_g[l, 0], ln_b[l, 0])
        if l % 2 == 0:
            f = swiglu(x, dense_w_gate[l // 2], dense_w_up[l // 2], dense_w_down[l // 2])
        else:
            j = l // 2
            f = moe_swiglu(x.reshape(-1, x.shape[-1]), router_w[j], expert_w_gate[j], expert_w_up[j],
                           expert_w_down[j]).reshape(x.shape)
        x = layer_norm(DN_ALPHA * x + f, ln_g[l, 1], ln_b[l, 1])
    return x
```

```python
import numpy as np
from contextlib import ExitStack
import concourse.bass as bass
import concourse.mybir as mybir
from concourse.bass_utils import run_bass_kernel_spmd

F32, BF16 = mybir.dt.float32, mybir.dt.bfloat16
ALU = mybir.AluOpType
AF = mybir.ActivationFunctionType
AX = mybir.AxisListType
ENGS = ("pe", "act", "dve", "pool", "sp")
SFX = [""]
SCOUNT = [0]
GLOBAL = {}


def _merge(d, s):
    for k, v in s.items():
        if d.get(k, 0) < v:
            d[k] = v


class Buf:
    def __init__(self, t, multi=False):
        self.t = t
        self.multi = multi
        self.w = {}
        self.r = {}
        self.dsem = None

    def __getitem__(self, k):
        return self.t[k]


class Stage:
    def __init__(self, nc, name):
        self.nc = nc
        name = name + SFX[0]
        self.name = name
        self.es = ExitStack()
        self.ops = {e: [] for e in ENGS}
        g = GLOBAL.get(id(nc))
        if g is None:
            ges = ExitStack()
            g = dict(es=ges, sem={e: ges.enter_context(nc.semaphore(f"g_{e}")) for e in ENGS}, cnt={e: 0 for e in ENGS}, pool=[])
            GLOBAL[id(nc)] = g
        self.g = g
        self.sem = g["sem"]
        self.cnt = dict(g["cnt"])
        self.dsems = {}
        self.ndsem = 0
        self.bufs = []
        self.semkey = {}

    def sb(self, name, shape, dt):
        t = self.es.enter_context(self.nc.sbuf_tensor(f"{self.name}_{name}", list(shape), dt))
        return self.track(Buf(t))

    def ps(self, name, shape=(128, 512), dt=F32):
        t = self.es.enter_context(self.nc.psum_tensor(f"{self.name}_{name}", list(shape), dt))
        return self.track(Buf(t))

    def track(self, b):
        b.w = {}
        b.r = {}
        b.dsem = None
        self.bufs.append(b)
        return b

    def _deps(self, reads, writes):
        deps = {}
        for b in reads:
            _merge(deps, b.w)
        for b in writes:
            _merge(deps, b.r)
            if not b.multi:
                _merge(deps, b.w)
        return deps

    def _commit(self, tok, reads, writes):
        for b in reads:
            _merge(b.r, tok)
        for b in writes:
            if b.multi:
                _merge(b.w, tok)
            else:
                b.w = dict(tok)
                b.r = {}

    def op(self, eng, fn, reads=(), writes=()):
        deps = self._deps(reads, writes)
        self.cnt[eng] += 1
        s = self.sem[eng]
        tok = {id(s): self.cnt[eng]}
        self.semkey[id(s)] = s
        self.ops[eng].append((deps, fn, s, 1))
        self._commit(tok, reads, writes)

    def dma(self, eng, out, in_, reads=(), writes=()):
        deps = self._deps(reads, writes)
        b = writes[0]
        if b.dsem is None:
            pool = self.g["pool"]
            if self.ndsem >= len(pool):
                pool.append([self.g["es"].enter_context(self.nc.semaphore(f"g_d{len(pool)}")), 0])
            b.dsem = pool[self.ndsem][0]
            self.dsems[id(b.dsem)] = pool[self.ndsem][1]
            self.semkey[id(b.dsem)] = b.dsem
            self.ndsem += 1
        self.dsems[id(b.dsem)] += 16
        tok = {id(b.dsem): self.dsems[id(b.dsem)]}
        self.ops[eng].append((deps, lambda e, o=out, i=in_: e.dma_start(out=o, in_=i), b.dsem, 16))
        self._commit(tok, reads, writes)

    def run(self):
        nc = self.nc
        final = {}
        for e in ENGS:
            if self.cnt[e] > self.g["cnt"][e]:
                final[id(self.sem[e])] = self.cnt[e]
                self.semkey[id(self.sem[e])] = self.sem[e]
        final.update({k: v for k, v in self.dsems.items()})

        def emit(engname, eng):
            waited = {}
            own = id(self.sem[engname])
            for deps, fn, s, n in self.ops[engname]:
                for k, v in deps.items():
                    if engname == "pe" and k == own:
                        continue
                    if waited.get(k, 0) < v:
                        eng.wait_ge(self.semkey[k], v)
                        waited[k] = v
                fn(eng).then_inc(s, n)
            for k, v in final.items():
                if waited.get(k, 0) < v:
                    eng.wait_ge(self.semkey[k], v)

        with nc.Block() as block:
            @block.tensor
            def _(e):
                emit("pe", e)

            @block.scalar
            def _(e):
                emit("act", e)

            @block.vector
            def _(e):
                emit("dve", e)

            @block.gpsimd
            def _(e):
                emit("pool", e)

            @block.sync
            def _(e):
                emit("sp", e)
        for e in ENGS:
            self.g["cnt"][e] = self.cnt[e]
        for p in self.g["pool"]:
            if id(p[0]) in self.dsems:
                p[1] = self.dsems[id(p[0])]
        for b in self.bufs:
            b.w = {}
            b.r = {}
            b.dsem = None
        self.es.close()

import math, os
T = 8192

def conv_stage(nc, QKT, QKC, convT_d):
    st = Stage(nc, "sB1"); st.track(QKT); st.track(QKC)
    cw = st.sb("cw", [128, 4, 4], F32)
    st.dma("sp", cw[:], convT_d.rearrange("(k p) j -> p k j", p=128), writes=[cw])
    win = [st.sb(f"win{i}", [128, 4, 515], F32) for i in range(2)]
    acc = [st.sb(f"acc{i}", [128, 4, 512], F32) for i in range(2)]
    out = [st.sb(f"out{i}", [128, 4, 512], F32) for i in range(2)]
    for tt in range(T // 512):
        w = win[tt % 2]; a = acc[tt % 2]; o = out[tt % 2]
        if tt == 0:
            st.op("pool", lambda e, w=w: e.memset(w[:, :, 0:3], 0.0), writes=[w])
            st.dma("sp", w[:, :, 3:515], QKT[:, 0:512].rearrange("(k p) t -> p k t", p=128), reads=[QKT], writes=[w])
        else:
            st.dma("sp", w[:], QKT[:, tt*512-3:(tt+1)*512].rearrange("(k p) t -> p k t", p=128), reads=[QKT], writes=[w])
        for k in range(4):
            eng = "dve"
            st.op(eng, lambda e, w=w, a=a, k=k: e.tensor_scalar(a[:, k, :], w[:, k, 0:512], cw[:, k, 0:1], None, ALU.mult), reads=[w, cw], writes=[a])
            for j in range(1, 4):
                st.op(eng, lambda e, w=w, a=a, k=k, j=j: e.scalar_tensor_tensor(a[:, k, :], w[:, k, j:j+512], cw[:, k, j:j+1], a[:, k, :], ALU.mult, ALU.add), reads=[w, cw, a], writes=[a])
        st.op("act", lambda e, a=a, o=o: e.activation(o[:], a[:], AF.Silu), reads=[a], writes=[o])
        st.op("dve", lambda e, o=o: e.tensor_scalar(o[:, 2:4, :], o[:, 2:4, :], 0.125, None, ALU.mult), reads=[o], writes=[o])
        st.dma("sp", QKC[:, tt*512:(tt+1)*512].rearrange("(k p) t -> p k t", p=128), o[:], reads=[o], writes=[QKC])
    st.run()

def layer_norm(st, r, ones, gcol, bcol, out, pss, tmp):
    sq, mean, rstd = tmp["sq"], tmp["mean"], tmp["rstd"]
    ps_s, ps_q = pss
    st.op("act", lambda e: e.activation(sq[:], r[:], AF.Square), reads=[r], writes=[sq])
    for k in range(8):
        st.op("pe", lambda e, k=k: e.matmul(ps_s[:], ones[:], r[:, k, :], start=(k == 0), stop=(k == 7)), reads=[ones, r], writes=[ps_s])
    for k in range(8):
        st.op("pe", lambda e, k=k: e.matmul(ps_q[:], ones[:], sq[:, k, :], start=(k == 0), stop=(k == 7)), reads=[ones, sq], writes=[ps_q])
    st.op("dve", lambda e: e.tensor_scalar(mean[:], ps_s[:], 1.0 / 1024, None, ALU.mult), reads=[ps_s], writes=[mean])
    st.op("dve", lambda e: e.tensor_tensor(rstd[:], mean[:], mean[:], ALU.mult), reads=[mean], writes=[rstd])
    st.op("dve", lambda e: e.scalar_tensor_tensor(rstd[:], ps_q[:], 1.0 / 1024, rstd[:], ALU.mult, ALU.subtract), reads=[ps_q, rstd], writes=[rstd])
    st.op("dve", lambda e: e.tensor_scalar(rstd[:], rstd[:], 1e-5, None, ALU.add), reads=[rstd], writes=[rstd])
    st.op("act", lambda e: e.activation(rstd[:], rstd[:], AF.Ln), reads=[rstd], writes=[rstd])
    st.op("act", lambda e: e.activation(rstd[:], rstd[:], AF.Exp, scale=-0.5), reads=[rstd], writes=[rstd])
    st.op("dve", lambda e: e.tensor_tensor(sq[:], r[:], mean[:].unsqueeze(1).to_broadcast([128, 8, 512]), ALU.subtract), reads=[r, mean], writes=[sq])
    st.op("pool", lambda e: e.tensor_tensor(sq[:], sq[:], rstd[:].unsqueeze(1).to_broadcast([128, 8, 512]), ALU.mult), reads=[sq, rstd], writes=[sq])
    for k in range(8):
        eng = "dve" if k % 2 == 0 else "pool"
        st.op(eng, lambda e, k=k: e.tensor_scalar(out[:, k, :], sq[:, k, :], gcol[:, k:k+1], bcol[:, k:k+1], ALU.mult, ALU.add), reads=[sq, gcol, bcol], writes=[out])

def mlstm_stage(nc, QKC, VAO, YAT, gb_d, ng_d, U_d, ident_d):
    st = Stage(nc, "sB2"); st.track(QKC); st.track(VAO); st.track(YAT)
    U = st.sb("U", [64, 64], F32); st.dma("sp", U[:], U_d[:, :], writes=[U])
    ident = st.sb("ident", [128, 128], F32); st.dma("sp", ident[:], ident_d[:, :], writes=[ident])
    gb = st.sb("gb", [64, 8], F32); st.dma("sp", gb[:], gb_d.partition_broadcast(64), writes=[gb])
    ng = st.sb("ng", [64, 512], F32); st.dma("sp", ng[:], ng_d.partition_broadcast(64), writes=[ng])
    ones64 = st.sb("ones64", [64, 64], F32); st.op("pool", lambda e: e.memset(ones64[:], 1.0), writes=[ones64])
    maskb = st.sb("maskb", [64, 4, 64], F32)
    st.op("dve", lambda e: e.tensor_copy(maskb[:], U[:].unsqueeze(1).to_broadcast([64, 4, 64])), reads=[U], writes=[maskb])
    C = st.sb("C", [64, 4, 129], F32); st.op("pool", lambda e: e.memset(C[:], 0.0), writes=[C])
    q4s = [st.sb(f"q4{i}", [64, 8, 512], F32) for i in range(2)]
    vas = [st.sb(f"va{i}", [64, 8, 1032], F32) for i in range(2)]
    yos = [st.sb(f"yo{i}", [128, 4, 512], F32) for i in range(2)]
    def mk(i):
        d = {}
        for n, s in dict(g=[64, 8], e1=[64, 4], nl=[64, 4], nlb=[64, 4, 64], col=[64, 4], Dt=[64, 4, 64], Ebc=[64, 4, 64], wtmp=[64, 4], wcol=[64, 4],
                         dec=[64, 4], nbt=[64, 8], Wt=[64, 4, 64], qt=[64, 4, 64], kt=[64, 4, 64], vaug=[64, 4, 129], den=[64, 4], rden=[64, 4], hn=[64, 4, 128],
                         sm=[64, 4], xc=[64, 4, 128], sq=[64, 4, 128], ssq=[64, 4], rstd=[64, 4], sig=[64, 512], gs=[64, 512], y=[64, 512]).items():
            d[n] = st.sb(f"{n}{i}", s, F32)
        st.op("pool", lambda e, v=d["vaug"]: e.memset(v[:], 1.0), writes=[d["vaug"]])
        return d
    tmps = [mk(0), mk(1)]
    psA = st.ps("psA", [64, 512]); psB = st.ps("psB", [64, 512])
    pSK = st.ps("pSK", [64, 512])
    psN = [st.ps(f"psN{j}", [64, 512]) for j in range(2)]; psU = [st.ps(f"psU{j}", [64, 512]) for j in range(2)]
    psY = st.ps("psY", [128, 512])
    for tt in range(T // 512):
        q4 = q4s[tt % 2]; va = vas[tt % 2]; yo = yos[tt % 2]
        st.dma("sp", q4[:], QKC[:, tt*512:(tt+1)*512].rearrange("(g d) t -> d g t", d=64), reads=[QKC], writes=[q4])
        st.dma("sp", va[:], VAO[tt*512:(tt+1)*512, :].rearrange("(c p) n -> p c n", p=64), reads=[VAO], writes=[va])
        for c in range(8):
            d = tmps[c % 2]; cs = slice(c*64, (c+1)*64)
            g, e1, nl, nlb, col, Dt, Ebc, wtmp, wcol, dec, Wt, qt, kt, vaug, den, rden, hn, sm, xc, sq, ssq, rstd, sig, gs, y = [d[n] for n in
                ("g", "e1", "nl", "nlb", "col", "Dt", "Ebc", "wtmp", "wcol", "dec", "Wt", "qt", "kt", "vaug", "den", "rden", "hn", "sm", "xc", "sq", "ssq", "rstd", "sig", "gs", "y")]
            st.op("dve", lambda e, g=g, va=va, c=c: e.tensor_tensor(g[:], va[:, c, 1024:1032], gb[:], ALU.add), reads=[va, gb], writes=[g])
            st.op("act", lambda e, g=g, e1=e1: e.activation(e1[:], g[:, 4:8], AF.Exp, scale=-1.0), reads=[g], writes=[e1])
            st.op("dve", lambda e, e1=e1: e.tensor_scalar(e1[:], e1[:], 1.0, None, ALU.add), reads=[e1], writes=[e1])
            st.op("act", lambda e, nl=nl, e1=e1: e.activation(nl[:], e1[:], AF.Ln), reads=[e1], writes=[nl])
            st.op("dve", lambda e, nl=nl, nlb=nlb: e.tensor_copy(nlb[:], nl[:].unsqueeze(2).to_broadcast([64, 4, 64])), reads=[nl], writes=[nlb])
            st.op("pe", lambda e, nl=nl: e.matmul(psA[:, 0:4], U[:], nl[:], start=True, stop=True), reads=[U, nl], writes=[psA])
            st.op("pe", lambda e, nl=nl: e.matmul(psA[:, 4:8], ones64[:], nl[:], start=True, stop=True), reads=[ones64, nl], writes=[psA])
            for h in range(4):
                st.op("pe", lambda e, nlb=nlb, h=h: e.matmul(psB[:, h*64:(h+1)*64], nlb[:, h, :], U[:], start=True, stop=True), reads=[nlb, U], writes=[psB])
            nbt = d["nbt"]
            st.op("act", lambda e, nbt=nbt: e.copy(nbt[:], psA[:, 0:8]), reads=[psA], writes=[nbt])
            st.op("dve", lambda e, col=col, g=g, nbt=nbt: e.tensor_tensor(col[:], nbt[:, 0:4], g[:, 0:4], ALU.add), reads=[nbt, g], writes=[col])
            for h in range(4):
                st.op("act", lambda e, Dt=Dt, col=col, h=h: e.activation(Dt[:, h, :], psB[:, h*64:(h+1)*64], AF.Exp, bias=col[:, h:h+1], scale=-1.0), reads=[psB, col], writes=[Dt])
            st.op("act", lambda e, Ebc=Ebc: e.activation(Ebc[:].rearrange("p h t -> p (h t)"), psB[:, 0:256], AF.Exp, scale=-1.0), reads=[psB], writes=[Ebc])
            st.op("dve", lambda e, wtmp=wtmp, col=col, nbt=nbt: e.tensor_tensor(wtmp[:], col[:], nbt[:, 4:8], ALU.subtract), reads=[col, nbt], writes=[wtmp])
            st.op("act", lambda e, wcol=wcol, wtmp=wtmp: e.activation(wcol[:], wtmp[:], AF.Exp), reads=[wtmp], writes=[wcol])
            st.op("act", lambda e, dec=dec, nbt=nbt: e.activation(dec[:], nbt[:, 4:8], AF.Exp, scale=-1.0), reads=[nbt], writes=[dec])
            for h in range(4):
                st.op("pe", lambda e, q4=q4, h=h, cs=cs: e.matmul(pSK[:, h*64:(h+1)*64], q4[:, 4+h, cs], q4[:, h, cs], start=True, stop=True), reads=[q4], writes=[pSK])
            st.op("dve", lambda e, Wt=Wt, Dt=Dt: e.tensor_tensor(Wt[:], Dt[:], maskb[:], ALU.mult), reads=[Dt, maskb], writes=[Wt])
            st.op("dve", lambda e, Wt=Wt: e.tensor_tensor(Wt[:], Wt[:], pSK[:, 0:256].rearrange("p (h t) -> p h t", h=4), ALU.mult), reads=[Wt, pSK], writes=[Wt])
            st.op("pool", lambda e, qt=qt, q4=q4, Ebc=Ebc, cs=cs: e.tensor_tensor(qt[:], q4[:, 0:4, cs], Ebc[:], ALU.mult), reads=[q4, Ebc], writes=[qt])
            for h in range(4):
                st.op("pe", lambda e, q4=q4, h=h, cs=cs: e.transpose(pSK[:, 256+h*64:256+(h+1)*64], q4[:, 4+h, cs], ident[0:64, 0:64]), reads=[q4, ident], writes=[pSK])
            st.op("dve", lambda e, kt=kt, wcol=wcol: e.tensor_tensor(kt[:], pSK[:, 256:512].rearrange("p (h t) -> p h t", h=4), wcol[:].unsqueeze(2).to_broadcast([64, 4, 64]), ALU.mult), reads=[pSK, wcol], writes=[kt])
            st.op("pool", lambda e, vaug=vaug, va=va, c=c: e.tensor_copy(vaug[:, :, 0:128], va[:, c, 0:512].rearrange("p (h v) -> p h v", h=4)), reads=[va], writes=[vaug])
            for h in range(4):
                pn = psN[h // 2]; o = (h % 2) * 129
                st.op("pe", lambda e, pn=pn, o=o, Wt=Wt, vaug=vaug, h=h: e.matmul(pn[:, o:o+129], Wt[:, h, :], vaug[:, h, :], start=True, stop=False), reads=[Wt, vaug], writes=[pn])
                st.op("pe", lambda e, pn=pn, o=o, qt=qt, h=h: e.matmul(pn[:, o:o+129], qt[:, h, :], C[:, h, :], start=False, stop=True), reads=[qt, C], writes=[pn])
            for h in range(4):
                pu = psU[h // 2]; o = (h % 2) * 129
                st.op("pe", lambda e, pu=pu, o=o, kt=kt, vaug=vaug, h=h: e.matmul(pu[:, o:o+129], kt[:, h, :], vaug[:, h, :], start=True, stop=True), reads=[kt, vaug], writes=[pu])
            for h in range(4):
                pu = psU[h // 2]; o = (h % 2) * 129
                st.op("dve", lambda e, pu=pu, o=o, dec=dec, h=h: e.scalar_tensor_tensor(C[:, h, :], C[:, h, :], dec[:, h:h+1], pu[:, o:o+129], ALU.mult, ALU.add), reads=[C, dec, pu], writes=[C])
            for j in range(2):
                pv = psN[j]
                st.op("act", lambda e, pv=pv, den=den, j=j: e.activation(den[:, 2*j:2*j+2], pv[:, 0:258].rearrange("p (i n) -> p i n", n=129)[:, :, 128], AF.Abs), reads=[pv], writes=[den])
            st.op("dve", lambda e, den=den: e.tensor_scalar(den[:], den[:], 1.0, None, ALU.max), reads=[den], writes=[den])
            st.op("dve", lambda e, den=den, rden=rden: e.reciprocal(rden[:], den[:]), reads=[den], writes=[rden])
            for j in range(2):
                pv = psN[j]
                st.op("dve", lambda e, pv=pv, hn=hn, rden=rden, j=j: e.tensor_tensor(hn[:, 2*j:2*j+2, :], pv[:, 0:258].rearrange("p (i n) -> p i n", n=129)[:, :, 0:128], rden[:, 2*j:2*j+2].unsqueeze(2).to_broadcast([64, 2, 128]), ALU.mult), reads=[pv, rden], writes=[hn])
            st.op("dve", lambda e, sm=sm, hn=hn: e.tensor_reduce(sm[:], hn[:], AX.X, ALU.add), reads=[hn], writes=[sm])
            st.op("dve", lambda e, sm=sm: e.tensor_scalar(sm[:], sm[:], -1.0 / 128, None, ALU.mult), reads=[sm], writes=[sm])
            st.op("dve", lambda e, xc=xc, hn=hn, sm=sm: e.tensor_tensor(xc[:], hn[:], sm[:].unsqueeze(2).to_broadcast([64, 4, 128]), ALU.add), reads=[hn, sm], writes=[xc])
            st.op("pool", lambda e, sq=sq, xc=xc: e.tensor_tensor(sq[:], xc[:], xc[:], ALU.mult), reads=[xc], writes=[sq])
            st.op("dve", lambda e, ssq=ssq, sq=sq: e.tensor_reduce(ssq[:], sq[:], AX.X, ALU.add), reads=[sq], writes=[ssq])
            st.op("dve", lambda e, ssq=ssq: e.tensor_scalar(ssq[:], ssq[:], 1.0 / 128, 1e-5, ALU.mult, ALU.add), reads=[ssq], writes=[ssq])
            st.op("act", lambda e, ssq=ssq, rstd=rstd: e.activation(rstd[:], ssq[:], AF.Ln), reads=[ssq], writes=[rstd])
            st.op("act", lambda e, rstd=rstd: e.activation(rstd[:], rstd[:], AF.Exp, scale=-0.5), reads=[rstd], writes=[rstd])
            st.op("act", lambda e, sig=sig, va=va, c=c: e.activation(sig[:], va[:, c, 512:1024], AF.Sigmoid), reads=[va], writes=[sig])
            st.op("pool", lambda e, gs=gs, sig=sig: e.tensor_tensor(gs[:], sig[:], ng[:], ALU.mult), reads=[sig, ng], writes=[gs])
            st.op("dve", lambda e, y=y, xc=xc, rstd=rstd: e.tensor_tensor(y[:].rearrange("p (h v) -> p h v", h=4), xc[:], rstd[:].unsqueeze(2).to_broadcast([64, 4, 128]), ALU.mult), reads=[xc, rstd], writes=[y])
            st.op("pool", lambda e, y=y, gs=gs: e.tensor_tensor(y[:], y[:], gs[:], ALU.mult), reads=[y, gs], writes=[y])
            for h in range(4):
                st.op("pe", lambda e, y=y, h=h: e.transpose(psY[:, h*64:(h+1)*64], y[:, h*128:(h+1)*128], ident[0:64, 0:64]), reads=[y, ident], writes=[psY])
            st.op("act", lambda e, yo=yo, cs=cs: e.copy(yo[:, :, cs], psY[:, 0:256].rearrange("p (h t) -> p h t", h=4)), reads=[psY], writes=[yo])
        st.dma("sp", YAT[:, tt*512:(tt+1)*512].rearrange("(k p) t -> p k t", p=128), yo[:], reads=[yo], writes=[YAT])
    st.run()

def t5_bucket_np(d):
    d = np.maximum(d, 0)
    lr = np.log(np.maximum(d, 1).astype(np.float32) / np.float32(16)) / np.float32(math.log(8.0))
    large = np.minimum(16 + (lr * 16).astype(np.int32), 31)
    return np.where(d < 16, d, large)

def host_consts():
    j = np.arange(512); d = j - 127
    oh = np.zeros((32, 512), np.float32)
    b = t5_bucket_np(d)
    for jj in range(511):
        if d[jj] >= 0: oh[b[jj], jj] = 1.0
    J = np.eye(128, dtype=np.float32)[::-1].copy()
    negtri = np.where(np.arange(128)[None, :] <= np.arange(128)[:, None], 0.0, -1e30).astype(np.float32)
    pow2 = np.tile((2.0 ** -np.arange(NIT)).astype(np.float32)[None, :], (128, 1))
    irep = np.tile(np.eye(128, dtype=np.float32), (1, 4))
    return dict(OH1=oh, J=J, NEGTRI=negtri, POW2=pow2, IREP=irep, ident=np.eye(128, dtype=np.float32))

def bias_setup(nc, rb_d, OH1_d, J_d, BVh, BIASD):
    st = Stage(nc, "sbias")
    BV = st.track(Buf(BVh.ap(), multi=True)); BD = st.track(Buf(BIASD, multi=True))
    rb = st.sb("rb", [32, 8], F32); st.dma("sp", rb[:], rb_d[:, :], writes=[rb])
    oh = st.sb("oh", [32, 512], F32); st.dma("sp", oh[:], OH1_d[:, :], writes=[oh])
    J = st.sb("J", [128, 128], F32); st.dma("sp", J[:], J_d[:, :], writes=[J])
    ps = st.ps("ps"); bv = st.sb("bv", [8, 512], F32)
    st.op("pe", lambda e: e.matmul(ps[0:8, :], rb[:], oh[:], start=True, stop=True), reads=[rb, oh], writes=[ps])
    st.op("dve", lambda e: e.tensor_scalar(bv[:], ps[0:8, :], 8.0, None, ALU.mult), reads=[ps], writes=[bv])
    st.dma("sp", BV[:, :], bv[:], reads=[bv], writes=[BV])
    hk = st.sb("hk", [128, 8, 128], F32); hi = st.sb("hi", [128, 2, 1024], BF16); tmp = st.sb("tmp", [128, 512], F32)
    ps2 = st.ps("ps2")
    for kind in range(3):
        st.dma("sp", hk[:], bass.AP(BVh, 128 * kind, [[1, 128], [512, 8], [1, 128]]), reads=[BV], writes=[hk])
        for half in range(2):
            st.op("pe", lambda e, half=half: e.matmul(ps2[:], J[:], hk[:, half*4:(half+1)*4, :].rearrange("p h t -> p (h t)"), start=True, stop=True), reads=[J, hk], writes=[ps2])
            st.op("act", lambda e, half=half: e.copy(hi[:, 0, half*512:(half+1)*512], ps2[:]), reads=[ps2], writes=[hi])
            st.op("dve", lambda e, half=half: e.tensor_tensor(tmp[:], ps2[:], hi[:, 0, half*512:(half+1)*512], ALU.subtract), reads=[ps2, hi], writes=[tmp])
            st.op("dve", lambda e, half=half: e.tensor_copy(hi[:, 1, half*512:(half+1)*512], tmp[:]), reads=[tmp], writes=[hi])
        st.dma("sp", BD[kind], hi[:], reads=[hi], writes=[BD])
    st.run()

def dsa_prep(nc, BQT, BC, wuk_d, kvg_d, ident_d, CKT, CKV, QLT):
    st = Stage(nc, "sC1")
    for b in (BQT, BC, CKT, CKV, QLT): st.track(b)
    wuk = st.sb("wuk", [64, 8, 128], BF16); st.dma("pool", wuk[:], wuk_d.rearrange("h d c -> d h c"), writes=[wuk])
    kvg = st.sb("kvg", [128, 128], F32); st.dma("sp", kvg[:], kvg_d.partition_broadcast(128), writes=[kvg])
    ident = st.sb("ident", [128, 128], F32); st.dma("sp", ident[:], ident_d[:, :], writes=[ident])
    bqs = [st.sb(f"bq{i}", [64, 8, 512], F32) for i in range(2)]; bqb = [st.sb(f"bqb{i}", [64, 8, 512], BF16) for i in range(2)]
    qls = [st.sb(f"ql{i}", [128, 8, 512], BF16) for i in range(2)]
    bcs = [st.sb(f"bc{i}", [128, 4, 128], F32) for i in range(2)]
    sq = st.sb("sq", [128, 4, 128], F32); ssq = st.sb("ssq", [128, 4], F32); rstd = st.sb("rstd", [128, 4], F32)
    ckv = st.sb("ckv", [128, 4, 128], F32)
    ckvb = [st.sb(f"ckvb{i}", [128, 4, 129], BF16) for i in range(2)]
    for c in ckvb: st.op("pool", lambda e, c=c: e.memset(c[:], 1.0), writes=[c])
    ckt = [st.sb(f"ckt{i}", [128, 512], BF16) for i in range(2)]
    pss = [st.ps(f"ps{i}") for i in range(4)]; pst = st.ps("pst")
    pi = 0
    for tt in range(T // 512):
        bq = bqs[tt % 2]; bb = bqb[tt % 2]; ql = qls[tt % 2]; bc = bcs[tt % 2]; cb = ckvb[tt % 2]; ct = ckt[tt % 2]
        st.dma("sp", bq[:], BQT[:, tt*512:(tt+1)*512].rearrange("(g d) t -> d g t", d=64), reads=[BQT], writes=[bq])
        st.op("pool", lambda e, bq=bq, bb=bb: e.tensor_copy(bb[:], bq[:]), reads=[bq], writes=[bb])
        for h in range(8):
            ps = pss[pi % 4]; pi += 1
            st.op("pe", lambda e, ps=ps, h=h, bb=bb: e.matmul(ps[:], wuk[:, h, :], bb[:, h, :], start=True, stop=True), reads=[wuk, bb], writes=[ps])
            if h % 2 == 0:
                st.op("act", lambda e, ps=ps, h=h, ql=ql: e.copy(ql[:, h, :], ps[:]), reads=[ps], writes=[ql])
            else:
                st.op("dve", lambda e, ps=ps, h=h, ql=ql: e.tensor_copy(ql[:, h, :], ps[:]), reads=[ps], writes=[ql])
        for q in range(4):
            st.dma("sp", QLT[:, tt*4+q, :, :], ql[:, :, q*128:(q+1)*128], reads=[ql], writes=[QLT])
        st.dma("sp", bc[:], BC[tt*512:(tt+1)*512, :].rearrange("(s p) c -> p s c", p=128), reads=[BC], writes=[bc])
        st.op("pool", lambda e, bc=bc: e.tensor_tensor(sq[:], bc[:], bc[:], ALU.mult), reads=[bc], writes=[sq])
        st.op("dve", lambda e: e.tensor_reduce(ssq[:], sq[:], AX.X, ALU.add), reads=[sq], writes=[ssq])
        st.op("dve", lambda e: e.tensor_scalar(ssq[:], ssq[:], 1.0 / 128, 1e-5, ALU.mult, ALU.add), reads=[ssq], writes=[ssq])
        st.op("act", lambda e: e.activation(rstd[:], ssq[:], AF.Ln), reads=[ssq], writes=[rstd])
        st.op("act", lambda e: e.activation(rstd[:], rstd[:], AF.Exp, scale=-0.5), reads=[rstd], writes=[rstd])
        st.op("dve", lambda e, bc=bc: e.tensor_tensor(ckv[:], bc[:], rstd[:].unsqueeze(2).to_broadcast([128, 4, 128]), ALU.mult), reads=[bc, rstd], writes=[ckv])
        st.op("pool", lambda e: e.tensor_tensor(ckv[:], ckv[:], kvg[:].unsqueeze(1).to_broadcast([128, 4, 128]), ALU.mult), reads=[ckv, kvg], writes=[ckv])
        st.op("pool", lambda e, cb=cb: e.tensor_copy(cb[:, :, 0:128], ckv[:]), reads=[ckv], writes=[cb])
        st.dma("sp", CKV[tt*512:(tt+1)*512, :].rearrange("(s p) c -> p s c", p=128), cb[:], reads=[cb], writes=[CKV])
        for s in range(4):
            st.op("pe", lambda e, s=s: e.transpose(pst[:, s*128:(s+1)*128], ckv[:, s, :], ident[:]), reads=[ckv, ident], writes=[pst])
        st.op("act", lambda e, ct=ct: e.copy(ct[:], pst[:]), reads=[pst], writes=[ct])
        st.dma("sp", CKT[:, tt*512:(tt+1)*512], ct[:], reads=[ct], writes=[CKT])
    st.run()

def dsa_attn(nc, name, q0, q1, IQT, IKT, IW, CKT, CKV, QLT, BIASD, OLT, C):
    st = Stage(nc, name)
    for b in (IQT, IKT, IW, CKT, CKV, QLT, OLT): st.track(b)
    BD = st.track(Buf(BIASD, multi=True))
    nkmax = q1 * 128
    cktS = st.sb("ckt", [128, nkmax], BF16); st.dma("sp", cktS[:], CKT[:, 0:nkmax], reads=[CKT], writes=[cktS])
    ckvS = st.sb("ckv", [128, q1, 129], BF16)
    for k0 in range(0, q1, 8):
        k1 = min(q1, k0 + 8)
        st.dma("sp", ckvS[:, k0:k1, :], CKV[k0*128:k1*128, :].rearrange("(k p) c -> p k c", p=128), reads=[CKV], writes=[ckvS])
    kiT = st.sb("kiT", [32, nkmax], F32); st.dma("sp", kiT[:], IKT[:, 0:nkmax], reads=[IKT], writes=[kiT])
    bias = st.sb("bias", [128, 3, 2, 1024], BF16)
    for kind in range(3): st.dma("sp", bias[:, kind], BD[kind], reads=[BD], writes=[bias])
    identb = st.sb("identb", [128, 128], BF16); st.dma("pool", identb[:], C["ident"][:, :], writes=[identb])
    ident = st.sb("ident", [128, 128], F32); st.dma("sp", ident[:], C["ident"][:, :], writes=[ident])
    irep = st.sb("irep", [128, 512], BF16); st.dma("pool", irep[:], C["IREP"][:, :], writes=[irep])
    negtri = st.sb("negtri", [128, 128], F32); st.dma("sp", negtri[:], C["NEGTRI"][:, :], writes=[negtri])
    pow2 = st.sb("pow2", [128, NIT], F32); st.dma("sp", pow2[:], C["POW2"][:, :], writes=[pow2])
    accs = [st.sb(f"acc{i}", [128, nkmax], F32) for i in range(2)]
    NMs = [st.sb(f"NM{i}", [128, nkmax], BF16) for i in range(2)]
    qis = [st.sb(f"qi{i}", [32, 8, 128], F32) for i in range(2)]; ws = [st.sb(f"w{i}", [128, 8], F32) for i in range(2)]
    qls = [st.sb(f"ql{i}", [128, 8, 128], BF16) for i in range(2)]
    rs = [st.sb(f"r{i}", [128, 512], F32) for i in range(4)]
    PTs = [st.sb(f"PT{i}", [128, 512], BF16) for i in range(3)]
    sm = {n: st.sb(n, s, F32) for n, s in dict(den=[128, 8], rden=[128, 8]).items()}
    ol = st.sb("ol", [128, 8, 128], F32); olT = [st.sb(f"olT{i}", [128, 8, 128], BF16) for i in range(2)]
    psL = [st.ps(f"psL{i}") for i in range(3)]; psO = [st.ps(f"psO{i}") for i in range(3)]; psI = [st.ps(f"psI{i}") for i in range(2)]
    OH = [(0, 0), (0, 1), (0, 2), (1, 0), (1, 1), (1, 2), (2, 0), (2, 1)]
    sms = [{n: st.sb(f"{n}{i}", s, F32) for n, s in dict(A=[128, 1], lo=[128, 1], steps=[128, NIT], mid=[128, 1], cnt=[128, 1], ge=[128, 1]).items()} for i in range(2)]
    state = dict(li=0, ii=0)

    def score(qt):
        nk = (qt + 1) * 128; qs = slice(qt*128, (qt+1)*128)
        qi = qis[qt % 2]; w = ws[qt % 2]; ql = qls[qt % 2]; acc = accs[qt % 2]
        st.dma("sp", qi[:], IQT[:, qs].rearrange("(g d) t -> d g t", d=32), reads=[IQT], writes=[qi])
        st.dma("sp", w[:], IW[qs, :], reads=[IW], writes=[w])
        st.dma("sp", ql[:], QLT[:, qt, :, :], reads=[QLT], writes=[ql])
        st.op("pool", lambda e, w=w: e.tensor_scalar(w[:], w[:], 1.0 / 16, None, ALU.mult), reads=[w], writes=[w])
        for kb in range((nk + 511) // 512):
            n = min(512, nk - kb*512); ks = slice(kb*512, kb*512 + n)
            for h in range(8):
                ii = state["ii"]; ps = psI[ii % 2]; r = rs[ii % 4]; state["ii"] += 1
                st.op("pe", lambda e, ps=ps, qi=qi, h=h, ks=ks, n=n: e.matmul(ps[:, 0:n], qi[:, h, :], kiT[:, ks], start=True, stop=True), reads=[qi, kiT], writes=[ps])
                st.op("act", lambda e, ps=ps, r=r, n=n: e.activation(r[:, 0:n], ps[:, 0:n], AF.Relu), reads=[ps], writes=[r])
                if h == 0:
                    st.op("dve", lambda e, r=r, w=w, ks=ks, n=n, acc=acc: e.tensor_scalar(acc[:, ks], r[:, 0:n], w[:, 0:1], None, ALU.mult), reads=[r, w], writes=[acc])
                else:
                    st.op("dve", lambda e, r=r, w=w, ks=ks, n=n, h=h, acc=acc: e.scalar_tensor_tensor(acc[:, ks], r[:, 0:n], w[:, h:h+1], acc[:, ks], ALU.mult, ALU.add), reads=[r, w, acc], writes=[acc])

    def bisect(qt):
        nk = (qt + 1) * 128; acc = accs[qt % 2]; NM = NMs[qt % 2]
        A, lo, steps, mid, cnt, ge = [sms[qt % 2][n] for n in ("A", "lo", "steps", "mid", "cnt", "ge")]
        st.op("dve", lambda e: e.reduce_max(A[:], acc[:, 0:nk], AX.X, apply_absolute_value=True), reads=[acc], writes=[A])
        st.op("dve", lambda e: e.tensor_tensor(acc[:, nk-128:nk], acc[:, nk-128:nk], negtri[:], ALU.add), reads=[acc, negtri], writes=[acc])
        st.op("dve", lambda e: e.tensor_scalar(A[:], A[:], 1.0, None, ALU.add), reads=[A], writes=[A])
        st.op("dve", lambda e: e.tensor_scalar(lo[:], A[:], -1.0, None, ALU.mult), reads=[A], writes=[lo])
        st.op("dve", lambda e: e.tensor_scalar(steps[:], pow2[:], A[:, 0:1], None, ALU.mult), reads=[pow2, A], writes=[steps])
        for k in range(NIT):
            st.op("dve", lambda e, k=k: e.tensor_tensor(mid[:], lo[:], steps[:, k:k+1], ALU.add), reads=[lo, steps], writes=[mid])
            st.op("dve", lambda e: e.tensor_scalar(NM[:, 0:nk], acc[:, 0:nk], mid[:, 0:1], 0.0, ALU.is_ge, ALU.add, accum_out=cnt[:, 0:1]), reads=[acc, mid], writes=[NM, cnt])
            st.op("dve", lambda e: e.tensor_scalar(ge[:], cnt[:], 255.5, None, ALU.is_ge), reads=[cnt], writes=[ge])
            st.op("dve", lambda e, k=k: e.scalar_tensor_tensor(lo[:], ge[:], steps[:, k:k+1], lo[:], ALU.mult, ALU.add), reads=[ge, steps, lo], writes=[lo])
        st.op("dve", lambda e: e.tensor_scalar(NM[:, 0:nk], acc[:, 0:nk], lo[:, 0:1], -30000.0, ALU.is_lt, ALU.mult), reads=[acc, lo], writes=[NM])

    def attn(qt):
        qs = slice(qt*128, (qt+1)*128)
        ql = qls[qt % 2]; NM = NMs[qt % 2]; oT = olT[qt % 2]
        den, rden = sm["den"], sm["rden"]
        for kt in range(qt + 1):
            kind = min(qt - kt, 2); kts = slice(kt*128, (kt+1)*128)
            for half in range(2):
                li = state["li"]; ps = psL[li % 3]; PT = PTs[li % 3]; state["li"] += 1
                hs = slice(half*512, (half+1)*512)
                st.op("pe", lambda e, ps=ps, kts=kts, ql=ql, half=half: e.matmul(ps[:], cktS[:, kts], ql[:, half*4:(half+1)*4, :].rearrange("p h t -> p (h t)"), start=True, stop=False), reads=[cktS, ql], writes=[ps])
                st.op("pe", lambda e, ps=ps, kind=kind, hs=hs: e.matmul(ps[:], identb[:], bias[:, kind, 0, hs], start=False, stop=False), reads=[identb, bias], writes=[ps])
                st.op("pe", lambda e, ps=ps, kind=kind, hs=hs: e.matmul(ps[:], identb[:], bias[:, kind, 1, hs], start=False, stop=False), reads=[identb, bias], writes=[ps])
                st.op("pe", lambda e, ps=ps, NM=NM, kts=kts: e.matmul(ps[:], NM[:, kts], irep[:], start=False, stop=True), reads=[NM, irep], writes=[ps])
                st.op("act", lambda e, ps=ps, PT=PT: e.activation(PT[:], ps[:], AF.Exp, scale=0.125), reads=[ps], writes=[PT])
                for hh in range(4):
                    h = half*4 + hh; bnk, slot = OH[h]; po = psO[bnk]
                    st.op("pe", lambda e, po=po, slot=slot, PT=PT, hh=hh, kt=kt, qt=qt: e.matmul(po[:, slot*129:(slot+1)*129], PT[:, hh*128:(hh+1)*128], ckvS[:, kt, :], start=(kt == 0 and slot == 0), stop=(kt == qt), skip_group_check=True),
                          reads=[PT, ckvS], writes=[po])
        for bnk in range(3):
            nh = 3 if bnk < 2 else 2; h0 = bnk*3; po = psO[bnk]
            st.op("act", lambda e, po=po, nh=nh, h0=h0: e.copy(den[:, h0:h0+nh], po[:, 0:nh*129].rearrange("p (i n) -> p i n", n=129)[:, :, 128]), reads=[po], writes=[den])
        st.op("dve", lambda e: e.reciprocal(rden[:], den[:]), reads=[den], writes=[rden])
        for bnk in range(3):
            nh = 3 if bnk < 2 else 2; h0 = bnk*3; po = psO[bnk]
            st.op("dve", lambda e, po=po, nh=nh, h0=h0: e.tensor_tensor(ol[:, h0:h0+nh, :], po[:, 0:nh*129].rearrange("p (i n) -> p i n", n=129)[:, :, 0:128], rden[:, h0:h0+nh].unsqueeze(2).to_broadcast([128, nh, 128]), ALU.mult), reads=[po, rden], writes=[ol])
        for half in range(2):
            li = state["li"]; ps = psL[li % 3]; state["li"] += 1
            for hh in range(4):
                st.op("pe", lambda e, ps=ps, hh=hh, half=half: e.transpose(ps[:, hh*128:(hh+1)*128], ol[:, half*4+hh, :], ident[:]), reads=[ol, ident], writes=[ps])
            st.op("act", lambda e, ps=ps, oT=oT, half=half: e.copy(oT[:, half*4:(half+1)*4, :].rearrange("p h t -> p (h t)"), ps[:]), reads=[ps], writes=[oT])
        st.dma("sp", OLT[:, :, qs], oT[:], reads=[oT], writes=[OLT])

    score(q0)
    for qt in range(q0, q1):
        if qt + 1 < q1:
            score(qt + 1)
        bisect(qt)
        attn(qt)
    st.run()


OFF = dict(a_qk=0, a_v=512, a_o=1024, a_if=1536, b_q=1544, b_c=2056, i_q=2184, i_k=2440, i_w=2472, g_a=2480, g_b=3504)
ALPHA = 8 ** 0.25
D = 1024


def transpose_in_stage(nc, x_in, XT0, ident_d):
    st = Stage(nc, "s0"); st.track(XT0)
    ident = st.sb("ident", [128, 128], F32)
    st.dma("sp", ident[:], ident_d[:, :], writes=[ident])
    xin = [st.sb(f"xin{i}", [128, 4, D], F32) for i in range(2)]
    xo = [st.sb(f"xo{i}", [128, 8, 512], F32) for i in range(2)]
    pss = [st.ps(f"ps{i}") for i in range(4)]
    pi = 0
    for tt in range(T // 512):
        xi = xin[tt % 2]; xot = xo[tt % 2]
        st.dma("sp", xi[:], x_in[tt*512:(tt+1)*512, :].rearrange("(s p) d -> p s d", p=128), writes=[xi])
        for k in range(8):
            ps = pss[pi % 4]; pi += 1
            for s in range(4):
                st.op("pe", lambda e, ps=ps, xi=xi, s=s, k=k: e.transpose(ps[:, s*128:(s+1)*128], xi[:, s, k*128:(k+1)*128], ident[:]),
                      reads=[xi, ident], writes=[ps])
            if k % 2 == 0:
                st.op("dve", lambda e, ps=ps, xot=xot, k=k: e.tensor_copy(xot[:, k, :], ps[:]), reads=[ps], writes=[xot])
            else:
                st.op("act", lambda e, ps=ps, xot=xot, k=k: e.copy(xot[:, k, :], ps[:]), reads=[ps], writes=[xot])
        st.dma("sp", XT0[:, tt*512:(tt+1)*512].rearrange("(k p) t -> p k t", p=128), xot[:], reads=[xot], writes=[XT0])
    st.run()


def transpose_out_stage(nc, XO, y_out, ident_d):
    st = Stage(nc, "s9"); st.track(XO)
    Y = st.track(Buf(y_out, multi=True))
    ident = st.sb("ident", [128, 128], F32)
    st.dma("sp", ident[:], ident_d[:, :], writes=[ident])
    xin = [st.sb(f"xin{i}", [128, 8, 512], F32) for i in range(2)]
    yo = [st.sb(f"yo{i}", [128, 4, D], F32) for i in range(2)]
    pss = [st.ps(f"ps{i}") for i in range(4)]
    pi = 0
    for tt in range(T // 512):
        xi = xin[tt % 2]; yt = yo[tt % 2]
        st.dma("sp", xi[:], XO[:, tt*512:(tt+1)*512].rearrange("(k p) t -> p k t", p=128), reads=[XO], writes=[xi])
        for s in range(4):
            for half in range(2):
                ps = pss[pi % 4]; pi += 1
                for kk in range(4):
                    k = half * 4 + kk
                    st.op("pe", lambda e, ps=ps, xi=xi, s=s, k=k, kk=kk: e.transpose(ps[:, kk*128:(kk+1)*128], xi[:, k, s*128:(s+1)*128], ident[:]),
                          reads=[xi, ident], writes=[ps])
                if half == 0:
                    st.op("dve", lambda e, ps=ps, yt=yt, s=s: e.tensor_copy(yt[:, s, 0:512], ps[:]), reads=[ps], writes=[yt])
                else:
                    st.op("act", lambda e, ps=ps, yt=yt, s=s: e.copy(yt[:, s, 512:1024], ps[:]), reads=[ps], writes=[yt])
        st.dma("sp", Y[tt*512:(tt+1)*512, :].rearrange("(s p) d -> p s d", p=128), yt[:], reads=[yt], writes=[Y])
    st.run()


def inproj_stage(nc, XT0, w_in, S):
    st = Stage(nc, "sA"); st.track(XT0)
    for b in S.values(): st.track(b)
    wb = st.sb("wb", [128, 8, 4528], BF16)
    wf = st.sb("wf", [128, 8, 296], F32)
    for k in range(8):
        st.dma("pool", wb[:, k, :], w_in[k*128:(k+1)*128, :], writes=[wb])
    st.dma("sp", wf[:], w_in[:, 2184:2480].rearrange("(k p) n -> p k n", p=128), writes=[wf])
    xf = [st.sb(f"xf{i}", [128, 8, 512], F32) for i in range(2)]
    xb = [st.sb(f"xb{i}", [128, 8, 512], BF16) for i in range(2)]
    ofm = [st.sb(f"ofm{i}", [128, 8, 512], F32) for i in range(2)]
    otm = [st.sb(f"otm{i}", [128, 4, 1032], F32) for i in range(2)]
    otb = [st.sb(f"otb{i}", [128, 4, 136], F32) for i in range(2)]
    pss = [st.ps(f"ps{i}") for i in range(6)]
    pi = [0]; oi = [0]

    def evac(ps_ap, out_ap, ps, ob, func=None):
        if func is not None or pi[0] % 2 == 1:
            st.op("act", lambda e: e.activation(out_ap, ps_ap, func if func is not None else AF.Copy), reads=[ps], writes=[ob])
        else:
            st.op("dve", lambda e: e.tensor_copy(out_ap, ps_ap), reads=[ps], writes=[ob])

    def fm_group(xt_b, wt, col0, ncols, dst, tt, func=None):
        M = min(128, ncols); nch = ncols // M
        ob = ofm[oi[0] % 2]; oi[0] += 1
        for c in range(nch):
            ps = pss[pi[0] % 6]; pi[0] += 1
            for k in range(8):
                st.op("pe", lambda e, ps=ps, c=c, k=k: e.matmul(ps[0:M, :], wt[:, k, col0+c*M:col0+(c+1)*M], xt_b[:, k, :], start=(k == 0), stop=(k == 7)),
                      reads=[wt, xt_b], writes=[ps])
            evac(ps[0:M, :], ob[0:M, c, :], ps, ob, func)
        if M == 128:
            st.dma("sp", dst[:, tt*512:(tt+1)*512].rearrange("(k p) t -> p k t", p=128), ob[:, 0:nch, :], reads=[ob], writes=[dst])
        else:
            st.dma("sp", dst[:, tt*512:(tt+1)*512], ob[0:M, 0, :], reads=[ob], writes=[dst])

    for tt in range(T // 512):
        xft = xf[tt % 2]; xbt = xb[tt % 2]
        st.dma("sp", xft[:], XT0[:, tt*512:(tt+1)*512].rearrange("(k p) t -> p k t", p=128), reads=[XT0], writes=[xft])
        st.op("pool", lambda e, xft=xft, xbt=xbt: e.tensor_copy(xbt[:], xft[:]), reads=[xft], writes=[xbt])
        fm_group(xbt, wb, OFF["a_qk"], 512, S["QKT"], tt)
        fm_group(xbt, wb, OFF["b_q"], 512, S["BQT"], tt)
        fm_group(xbt, wb, OFF["g_a"], 1024, S["GAT"], tt, func=AF.Sigmoid)
        fm_group(xbt, wb, OFF["g_b"], 1024, S["GBT"], tt, func=AF.Sigmoid)
        fm_group(xft, wf, 0, 256, S["IQT"], tt)
        fm_group(xft, wf, 256, 32, S["IKT"], tt)
        ob = otm[tt % 2]; ob2 = otb[tt % 2]
        for s in range(4):
            for (c0, n) in ((0, 512), (512, 512), (1024, 8)):
                ps = pss[pi[0] % 6]; pi[0] += 1
                for k in range(8):
                    st.op("pe", lambda e, ps=ps, s=s, k=k, c0=c0, n=n, xbt=xbt: e.matmul(ps[:, 0:n], xbt[:, k, s*128:(s+1)*128], wb[:, k, 512+c0:512+c0+n], start=(k == 0), stop=(k == 7)),
                          reads=[wb, xbt], writes=[ps])
                evac(ps[:, 0:n], ob[:, s, c0:c0+n], ps, ob)
            ps = pss[pi[0] % 6]; pi[0] += 1
            for k in range(8):
                st.op("pe", lambda e, ps=ps, s=s, k=k, xbt=xbt: e.matmul(ps[:, 0:128], xbt[:, k, s*128:(s+1)*128], wb[:, k, OFF["b_c"]:OFF["b_c"]+128], start=(k == 0), stop=(k == 7)),
                      reads=[wb, xbt], writes=[ps])
            evac(ps[:, 0:128], ob2[:, s, 0:128], ps, ob2)
            ps = pss[pi[0] % 6]; pi[0] += 1
            for k in range(8):
                st.op("pe", lambda e, ps=ps, s=s, k=k, xft=xft: e.matmul(ps[:, 0:8], xft[:, k, s*128:(s+1)*128], wf[:, k, 288:296], start=(k == 0), stop=(k == 7)),
                      reads=[wf, xft], writes=[ps])
            evac(ps[:, 0:8], ob2[:, s, 128:136], ps, ob2)
        st.dma("sp", S["VAO"][tt*512:(tt+1)*512, :].rearrange("(s p) n -> p s n", p=128), ob[:], reads=[ob], writes=[S["VAO"]])
        st.dma("sp", S["BC"][tt*512:(tt+1)*512, :].rearrange("(s p) n -> p s n", p=128), ob2[:, :, 0:128], reads=[ob2], writes=[S["BC"]])
        st.dma("sp", S["IW"][tt*512:(tt+1)*512, :].rearrange("(s p) n -> p s n", p=128), ob2[:, :, 128:136], reads=[ob2], writes=[S["IW"]])
    st.run()


def wc_stage(nc, wuvT_d, wbb_d, WC):
    st = Stage(nc, "sWc"); st.track(WC)
    wuvT = st.sb("wuvT", [64, 8, 128], BF16); st.dma("pool", wuvT[:], wuvT_d.rearrange("h d c -> d h c"), writes=[wuvT])
    wbb = st.sb("wbb", [64, 8, 1024], BF16); st.dma("pool", wbb[:], wbb_d.rearrange("(h d) n -> d h n", d=64), writes=[wbb])
    wc = st.sb("wc", [128, 8, 1024], BF16)
    pss = [st.ps(f"ps{i}") for i in range(4)]
    pi = 0
    for h in range(8):
        for half in range(2):
            ps = pss[pi % 4]; pi += 1
            st.op("pe", lambda e, ps=ps, h=h, half=half: e.matmul(ps[:], wuvT[:, h, :], wbb[:, h, half*512:(half+1)*512], start=True, stop=True), reads=[wuvT, wbb], writes=[ps])
            if pi % 2 == 0:
                st.op("act", lambda e, ps=ps, h=h, half=half: e.copy(wc[:, h, half*512:(half+1)*512], ps[:]), reads=[ps], writes=[wc])
            else:
                st.op("dve", lambda e, ps=ps, h=h, half=half: e.tensor_copy(wc[:, h, half*512:(half+1)*512], ps[:]), reads=[ps], writes=[wc])
    st.dma("sp", WC[:, :, :], wc[:], reads=[wc], writes=[WC])
    st.run()


def merge_stage(nc, XI, XO, YAT, OLT, GAT, GBT, WC, wa_d, wo_d, g_d, b_d, ones_d):
    st = Stage(nc, "sD")
    for b in (XI, XO, YAT, OLT, GAT, GBT, WC): st.track(b)
    ones = st.sb("ones", [128, 128], F32); st.dma("sp", ones[:], ones_d[:, :], writes=[ones])
    gcol = st.sb("gcol", [128, 8], F32); st.dma("sp", gcol[:], g_d, writes=[gcol])
    bcol = st.sb("bcol", [128, 8], F32); st.dma("sp", bcol[:], b_d, writes=[bcol])
    wa = st.sb("wa", [128, 4, 1024], BF16); st.dma("pool", wa[:], wa_d.rearrange("(k p) n -> p k n", p=128), writes=[wa])
    wo = st.sb("wo", [128, 8, 1024], BF16); st.dma("pool", wo[:], wo_d.rearrange("(k p) n -> p k n", p=128), writes=[wo])
    wc = st.sb("wc", [128, 8, 1024], BF16); st.dma("sp", wc[:], WC[:, :, :], reads=[WC], writes=[wc])
    yab = st.sb("yab", [128, 4, 512], BF16); ol = st.sb("ol", [128, 8, 512], BF16)
    ga = st.sb("ga", [128, 8, 512], F32); gb = st.sb("gb", [128, 8, 512], F32); x = st.sb("x", [128, 8, 512], F32)
    mg = st.sb("mg", [128, 8, 512], BF16)
    t1 = [st.sb(f"t1{i}", [128, 512], F32) for i in range(2)]; t2 = [st.sb(f"t2{i}", [128, 512], F32) for i in range(2)]
    tmp = dict(sq=st.sb("sq", [128, 8, 512], F32), mean=st.sb("mean", [128, 512], F32), rstd=st.sb("rstd", [128, 512], F32))
    psA = [st.ps(f"psA{i}") for i in range(2)]; psB = [st.ps(f"psB{i}") for i in range(2)]; psO = [st.ps(f"psO{i}") for i in range(2)]
    psS = st.ps("psS"); psQ = st.ps("psQ")
    for tt in range(T // 512):
        ts = slice(tt*512, (tt+1)*512)
        st.dma("pool", yab[:], YAT[:, ts].rearrange("(k p) t -> p k t", p=128), reads=[YAT], writes=[yab])
        st.dma("sp", ol[:], OLT[:, :, ts], reads=[OLT], writes=[ol])
        st.dma("sp", ga[:], GAT[:, ts].rearrange("(k p) t -> p k t", p=128), reads=[GAT], writes=[ga])
        st.dma("sp", gb[:], GBT[:, ts].rearrange("(k p) t -> p k t", p=128), reads=[GBT], writes=[gb])
        st.dma("sp", x[:], XI[:, ts].rearrange("(k p) t -> p k t", p=128), reads=[XI], writes=[x])
        for c in range(8):
            pa = psA[c % 2]; pb = psB[c % 2]; a1 = t1[c % 2]; a2 = t2[c % 2]; cs = slice(c*128, (c+1)*128)
            for k in range(4):
                st.op("pe", lambda e, pa=pa, k=k, cs=cs: e.matmul(pa[:], wa[:, k, cs], yab[:, k, :], start=(k == 0), stop=(k == 3)), reads=[wa, yab], writes=[pa])
            for h in range(8):
                st.op("pe", lambda e, pb=pb, h=h, cs=cs: e.matmul(pb[:], wc[:, h, cs], ol[:, h, :], start=(h == 0), stop=(h == 7)), reads=[wc, ol], writes=[pb])
            st.op("dve", lambda e, pa=pa, a1=a1, c=c: e.tensor_tensor(a1[:], pa[:], ga[:, c, :], ALU.mult), reads=[pa, ga], writes=[a1])
            st.op("dve", lambda e, pb=pb, a2=a2, c=c: e.tensor_tensor(a2[:], pb[:], gb[:, c, :], ALU.mult), reads=[pb, gb], writes=[a2])
            st.op("pool", lambda e, a1=a1, a2=a2, c=c: e.tensor_tensor(mg[:, c, :], a1[:], a2[:], ALU.add), reads=[a1, a2], writes=[mg])
        for c in range(8):
            po = psO[c % 2]; cs = slice(c*128, (c+1)*128)
            for k in range(8):
                st.op("pe", lambda e, po=po, k=k, cs=cs: e.matmul(po[:], wo[:, k, cs], mg[:, k, :], start=(k == 0), stop=(k == 7)), reads=[wo, mg], writes=[po])
            st.op("dve", lambda e, po=po, c=c: e.scalar_tensor_tensor(x[:, c, :], x[:, c, :], ALPHA, po[:], ALU.mult, ALU.add), reads=[x, po], writes=[x])
        layer_norm(st, x, ones, gcol, bcol, tmp["sq"], (psS, psQ), tmp)
        st.dma("sp", XO[:, ts].rearrange("(k p) t -> p k t", p=128), tmp["sq"][:], reads=[tmp["sq"]], writes=[XO])
    st.run()


def ffn_stage(nc, name, tb0, tb1, XI, XO, wg_d, wu_d, wd_d, g_d, b_d, C, nexp, dff, G, rw_d=None):
    TB = 1024; NF = dff // 128; NG = NF // G
    st = Stage(nc, name); st.track(XI); st.track(XO)
    ones = st.sb("ones", [128, 128], F32); st.dma("sp", ones[:], C["ones"][:, :], writes=[ones])
    gcol = st.sb("gcol", [128, 8], F32); st.dma("sp", gcol[:], g_d, writes=[gcol])
    bcol = st.sb("bcol", [128, 8], F32); st.dma("sp", bcol[:], b_d, writes=[bcol])
    moe = rw_d is not None
    xb = st.sb("xb", [128, 8, TB], BF16); y = st.sb("y", [128, 8, TB], F32)
    wgg = st.sb("wgg", [128, 8, G*128], BF16); wug = st.sb("wug", [128, 8, G*128], BF16); wdg = st.sb("wdg", [128, G, 1024], BF16)
    hg = st.sb("hg", [128, G, 2, 512], BF16)
    sg = [st.sb(f"sg{i}", [128, 512], F32) for i in range(2)]
    psg = [st.ps(f"psg{i}") for i in range(2)]; psu = [st.ps(f"psu{i}") for i in range(2)]
    psd = [st.ps(f"psd{i}") for i in range(4)]
    tmp = dict(sq=st.sb("sq", [128, 8, 512], F32), mean=st.sb("mean", [128, 512], F32), rstd=st.sb("rstd", [128, 512], F32))
    xr = st.sb("xr", [128, 8, 512], F32)
    if moe:
        ident = st.sb("ident", [128, 128], F32); st.dma("sp", ident[:], C["ident"][:, :], writes=[ident])
        rw = st.sb("rw", [128, 8, 8], F32); st.dma("sp", rw[:], rw_d.rearrange("(k p) e -> p k e", p=128), writes=[rw])
        sel = st.sb("sel", [8, 8, 128], F32); st.dma("sp", sel[:], C["SEL"][:, :, :], writes=[sel])
        xs = [st.sb(f"xs{i}", [128, 8, 128], F32) for i in range(2)]
        GT = st.sb("GT", [8, TB], F32); gbc = st.sb("gbc", [128, TB], F32)
        t2 = [st.sb(f"t2{i}", [128, 512], F32) for i in range(2)]
        sm = {n: st.sb(n, s, F32) for n, s in dict(lg=[128, 8], m8=[128, 8], dl=[128, 1], g1=[128, 1], g2=[128, 1], e1=[128, 8], e2=[128, 8]).items()}
    it = 0
    for tb in range(tb0, tb1):
        t0 = tb * TB
        st.dma("pool", xb[:], XI[:, t0:t0+TB].rearrange("(k p) t -> p k t", p=128), reads=[XI], writes=[xb])
        if moe:
            lg, m8, dl, g1, g2, e1, e2 = [sm[n] for n in ("lg", "m8", "dl", "g1", "g2", "e1", "e2")]
            for s in range(TB // 128):
                xst = xs[s % 2]; pr = psd[s % 4]
                st.dma("sp", xst[:], XI[:, t0+s*128:t0+(s+1)*128].rearrange("(k p) t -> p k t", p=128), reads=[XI], writes=[xst])
                for k in range(8):
                    st.op("pe", lambda e, pr=pr, xst=xst, k=k: e.matmul(pr[:, 0:8], xst[:, k, :], rw[:, k, :], start=(k == 0), stop=(k == 7)), reads=[xst, rw], writes=[pr])
                st.op("act", lambda e, pr=pr: e.copy(lg[:], pr[:, 0:8]), reads=[pr], writes=[lg])
                st.op("dve", lambda e: e.max(m8[:], lg[:]), reads=[lg], writes=[m8])
                st.op("dve", lambda e: e.tensor_tensor(dl[:], m8[:, 0:1], m8[:, 1:2], ALU.subtract), reads=[m8], writes=[dl])
                st.op("act", lambda e: e.activation(g1[:], dl[:], AF.Sigmoid), reads=[dl], writes=[g1])
                st.op("act", lambda e: e.activation(g2[:], dl[:], AF.Sigmoid, scale=-1.0), reads=[dl], writes=[g2])
                st.op("dve", lambda e: e.tensor_scalar(e1[:], lg[:], m8[:, 0:1], g1[:, 0:1], ALU.is_equal, ALU.mult), reads=[lg, m8, g1], writes=[e1])
                st.op("dve", lambda e: e.tensor_scalar(e2[:], lg[:], m8[:, 1:2], g2[:, 0:1], ALU.is_equal, ALU.mult), reads=[lg, m8, g2], writes=[e2])
                st.op("dve", lambda e: e.tensor_tensor(e1[:], e1[:], e2[:], ALU.add), reads=[e1, e2], writes=[e1])
                st.op("pe", lambda e, pr=pr: e.transpose(pr[0:8, 128:256], e1[:], ident[:]), reads=[e1, ident], writes=[pr])
                st.op("act", lambda e, pr=pr, s=s: e.copy(GT[:, s*128:(s+1)*128], pr[0:8, 128:256]), reads=[pr], writes=[GT])
        first = True
        for ex in range(nexp):
            if moe:
                for sub in range(2):
                    pr = psd[sub]
                    st.op("pe", lambda e, pr=pr, ex=ex, sub=sub: e.matmul(pr[:], sel[:, ex, :], GT[:, sub*512:(sub+1)*512], start=True, stop=True), reads=[sel, GT], writes=[pr])
                    st.op("act", lambda e, pr=pr, sub=sub: e.copy(gbc[:, sub*512:(sub+1)*512], pr[:]), reads=[pr], writes=[gbc])
            for grp in range(NG):
                f0 = grp * G * 128
                st.dma("pool", wgg[:], wg_d[ex, :, f0:f0+G*128].rearrange("(k p) n -> p k n", p=128), writes=[wgg])
                st.dma("pool", wug[:], wu_d[ex, :, f0:f0+G*128].rearrange("(k p) n -> p k n", p=128), writes=[wug])
                st.dma("pool", wdg[:], wd_d[ex, f0:f0+G*128, :].rearrange("(g p) n -> p g n", p=128), writes=[wdg])
                for fi in range(G):
                    fs = slice(fi*128, (fi+1)*128)
                    for sub in range(2):
                        pg, pu, sgt = psg[it % 2], psu[it % 2], sg[it % 2]
                        ss = slice(sub*512, (sub+1)*512)
                        for k in range(8):
                            st.op("pe", lambda e, pg=pg, k=k, fs=fs, ss=ss: e.matmul(pg[:], wgg[:, k, fs], xb[:, k, ss], start=(k == 0), stop=(k == 7)), reads=[wgg, xb], writes=[pg])
                        for k in range(8):
                            st.op("pe", lambda e, pu=pu, k=k, fs=fs, ss=ss: e.matmul(pu[:], wug[:, k, fs], xb[:, k, ss], start=(k == 0), stop=(k == 7)), reads=[wug, xb], writes=[pu])
                        st.op("act", lambda e, pg=pg, sgt=sgt: e.activation(sgt[:], pg[:], AF.Silu), reads=[pg], writes=[sgt])
                        if moe:
                            tt2 = t2[it % 2]
                            st.op("dve", lambda e, pu=pu, tt2=tt2, ss=ss: e.tensor_tensor(tt2[:], pu[:], gbc[:, ss], ALU.mult), reads=[pu, gbc], writes=[tt2])
                            st.op("pool", lambda e, sgt=sgt, tt2=tt2, fi=fi, sub=sub: e.tensor_tensor(hg[:, fi, sub, :], sgt[:], tt2[:], ALU.mult), reads=[sgt, tt2], writes=[hg])
                        else:
                            st.op("dve", lambda e, pu=pu, sgt=sgt, fi=fi, sub=sub: e.tensor_tensor(hg[:, fi, sub, :], sgt[:], pu[:], ALU.mult), reads=[pu, sgt], writes=[hg])
                        it += 1
                for sub in range(2):
                    ss = slice(sub*512, (sub+1)*512)
                    for c in range(8):
                        pd = psd[c % 4]; cs = slice(c*128, (c+1)*128)
                        for fi in range(G):
                            st.op("pe", lambda e, pd=pd, fi=fi, cs=cs, sub=sub: e.matmul(pd[:], wdg[:, fi, cs], hg[:, fi, sub, :], start=(fi == 0), stop=(fi == G-1)), reads=[wdg, hg], writes=[pd])
                        if first:
                            st.op("dve", lambda e, pd=pd, c=c, ss=ss: e.tensor_copy(y[:, c, ss], pd[:]), reads=[pd], writes=[y])
                        else:
                            st.op("dve", lambda e, pd=pd, c=c, ss=ss: e.tensor_tensor(y[:, c, ss], y[:, c, ss], pd[:], ALU.add), reads=[pd, y], writes=[y])
                first = False
        for sub in range(2):
            ss = slice(sub*512, (sub+1)*512); ts = slice(t0+sub*512, t0+(sub+1)*512)
            st.dma("sp", xr[:], XI[:, ts].rearrange("(k p) t -> p k t", p=128), reads=[XI], writes=[xr])
            st.op("dve", lambda e, ss=ss: e.scalar_tensor_tensor(xr[:], xr[:], ALPHA, y[:, :, ss], ALU.mult, ALU.add), reads=[xr, y], writes=[xr])
            layer_norm(st, xr, ones, gcol, bcol, tmp["sq"], (psg[0], psu[0]), tmp)
            st.dma("sp", XO[:, ts].rearrange("(k p) t -> p k t", p=128), tmp["sq"][:], reads=[tmp["sq"]], writes=[XO])
    st.run()

NIT = 22


def dump_stage(nc, name, SRC, dst_ap):
    st = Stage(nc, name); st.track(SRC)
    Dst = st.track(Buf(dst_ap, multi=True))
    t = [st.sb(f"t{i}", [128, 8, 512], F32) for i in range(2)]
    for tt in range(T // 512):
        ts = slice(tt*512, (tt+1)*512)
        st.dma("sp", t[tt % 2][:], SRC[:, ts].rearrange("(k p) t -> p k t", p=128), reads=[SRC], writes=[t[tt % 2]])
        st.dma("sp", Dst[:, ts].rearrange("(k p) t -> p k t", p=128), t[tt % 2][:], reads=[t[tt % 2]], writes=[Dst])
    st.run()


def build_full(NL, debug=False):
    nc = bass.Bass("TRN2", target_bir_lowering=False)
    NQ = T // 128; ND = (NL + 1) // 2; NM = NL // 2
    dr = lambda n, s, k="Internal", dt=F32: nc.dram_tensor(n, list(s), dt, kind=k).ap()
    ein = lambda n, s: dr(n, s, "ExternalInput")
    x_in = ein("x", [T, D]); w_in = ein("w_in", [NL, D, 4528]); convT = ein("convT", [NL, 512, 4])
    gbias = ein("gbias", [NL, 1, 8]); ng = ein("ng", [NL, 1, 512]); kvg = ein("kvg", [NL, 1, 128])
    wuk = ein("wuk", [NL, 8, 64, 128]); wuvT = ein("wuvT", [NL, 8, 64, 128]); rb = ein("rb", [32, 8])
    wba = ein("wba", [NL, 512, 1024]); wbb = ein("wbb", [NL, 512, 1024]); wout = ein("wout", [NL, 1024, 1024])
    lng = ein("lng", [NL, 2, 128, 8]); lnb = ein("lnb", [NL, 2, 128, 8])
    dwg = ein("dwg", [ND, 1, D, 2816]); dwu = ein("dwu", [ND, 1, D, 2816]); dwd = ein("dwd", [ND, 1, 2816, D])
    if NM:
        rw = ein("rw", [NM, D, 8]); ewg = ein("ewg", [NM, 8, D, 3584]); ewu = ein("ewu", [NM, 8, D, 3584]); ewd = ein("ewd", [NM, 8, 3584, D])
    C = {n: ein(n, s) for n, s in dict(OH1=[32, 512], J=[128, 128], NEGTRI=[128, 128], POW2=[128, NIT], IREP=[128, 512], ident=[128, 128],
                                       ones=[128, 128], U=[64, 64], SEL=[8, 8, 128]).items()}
    y = dr("y", [T, D], "ExternalOutput")
    dbg = [dr(f"dbg{l}", [D, T], "ExternalOutput") for l in range(NL)] if debug else None
    mb = lambda n, s, dt=F32: Buf(dr(n, s, dt=dt), multi=True)
    XTa = mb("XTa", [D, T]); XTb = mb("XTb", [D, T])
    S = dict(QKT=mb("QKT", [512, T]), BQT=mb("BQT", [512, T]), IQT=mb("IQT", [256, T]), IKT=mb("IKT", [32, T]),
             GAT=mb("GAT", [1024, T]), GBT=mb("GBT", [1024, T]), VAO=mb("VAO", [T, 1032]), BC=mb("BC", [T, 128]), IW=mb("IW", [T, 8]))
    QKC = mb("QKC", [512, T]); YAT = mb("YAT", [512, T])
    CKT = mb("CKT", [128, T], BF16); CKV = mb("CKV", [T, 129], BF16); QLT = mb("QLT", [128, NQ, 8, 128], BF16); OLT = mb("OLT", [128, 8, T], BF16)
    WC = mb("WC", [128, 8, 1024], BF16)
    BVh = nc.dram_tensor("BV", [8, 512], F32); BIASD = dr("BIASD", [3, 128, 2, 1024], dt=BF16)
    SFX[0] = ""
    transpose_in_stage(nc, x_in, XTa, C["ident"])
    bias_setup(nc, rb, C["OH1"], C["J"], BVh, BIASD)
    bounds = [0]
    while bounds[-1] < NQ:
        a = bounds[-1]; b = a; pairs = 0
        while b < NQ and (pairs + b + 1 <= 1100 or b == a):
            pairs += b + 1; b += 1
        bounds.append(b)
    for l in range(NL):
        SFX[0] = f"_L{l}"
        inproj_stage(nc, XTa, w_in[l], S)
        conv_stage(nc, S["QKT"], QKC, convT[l])
        mlstm_stage(nc, QKC, S["VAO"], YAT, gbias[l], ng[l], C["U"], C["ident"])
        dsa_prep(nc, S["BQT"], S["BC"], wuk[l], kvg[l], C["ident"], CKT, CKV, QLT)
        for i in range(len(bounds) - 1):
            dsa_attn(nc, f"sC2_{i}", bounds[i], bounds[i+1], S["IQT"], S["IKT"], S["IW"], CKT, CKV, QLT, BIASD, OLT, C)
        wc_stage(nc, wuvT[l], wbb[l], WC)
        merge_stage(nc, XTa, XTb, YAT, OLT, S["GAT"], S["GBT"], WC, wba[l], wout[l], lng[l, 0], lnb[l, 0], C["ones"])
        NTB = T // 1024
        if l % 2 == 0:
            ffn_stage(nc, "sE", 0, NTB, XTb, XTa, dwg[l // 2], dwu[l // 2], dwd[l // 2], lng[l, 1], lnb[l, 1], C, 1, 2816, 11)
        else:
            j = l // 2
            for tb in range(0, NTB, 2):
                ffn_stage(nc, f"sF{tb}", tb, min(tb + 2, NTB), XTb, XTa, ewg[j], ewu[j], ewd[j], lng[l, 1], lnb[l, 1], C, 8, 3584, 7, rw_d=rw[j])
        if debug:
            dump_stage(nc, "sdbg", XTa, dbg[l])
    SFX[0] = "_fin"
    transpose_out_stage(nc, XTa, y, C["ident"])
    return nc


def host_inputs(inputs, NL):
    f = lambda a: np.ascontiguousarray(np.asarray(a, dtype=np.float32))
    ND = (NL + 1) // 2; NM = NL // 2
    colz = lambda v: f(np.asarray(v)[:NL].reshape(NL, 2, 8, 128).transpose(0, 1, 3, 2))
    d = dict(
        w_in=f(inputs["w_in"][:NL]), convT=f(np.asarray(inputs["mlstm_conv_w"])[:NL].transpose(0, 2, 1)),
        gbias=f(np.asarray(inputs["mlstm_gate_bias"])[:NL].reshape(NL, 1, 8)), ng=f(np.asarray(inputs["mlstm_norm_g"])[:NL].reshape(NL, 1, 512)),
        kvg=f(np.asarray(inputs["dsa_kv_norm_g"])[:NL].reshape(NL, 1, 128)), wuk=f(inputs["dsa_w_uk"][:NL]),
        wuvT=f(np.asarray(inputs["dsa_w_uv"])[:NL].transpose(0, 1, 3, 2)), rb=f(inputs["rel_bias"]),
        wba=f(inputs["w_branch_a"][:NL]), wbb=f(inputs["w_branch_b"][:NL]), wout=f(inputs["w_out"][:NL]),
        lng=colz(inputs["ln_g"]), lnb=colz(inputs["ln_b"]),
        dwg=f(np.asarray(inputs["dense_w_gate"])[:ND, None]), dwu=f(np.asarray(inputs["dense_w_up"])[:ND, None]), dwd=f(np.asarray(inputs["dense_w_down"])[:ND, None]),
    )
    if NM:
        d.update(rw=f(inputs["router_w"][:NM]), ewg=f(inputs["expert_w_gate"][:NM]), ewu=f(inputs["expert_w_up"][:NM]), ewd=f(inputs["expert_w_down"][:NM]))
    hc = host_consts()
    hc["ones"] = np.ones((128, 128), np.float32); hc["U"] = np.triu(np.ones((64, 64), np.float32))
    sel = np.zeros((8, 8, 128), np.float32)
    for e in range(8): sel[e, e, :] = 1.0
    hc["SEL"] = sel
    d.update(hc)
    return d


def kernel(**inputs):
    x = np.asarray(inputs["x"], dtype=np.float32)
    n = x.shape[0]
    NL = 4
    shared = host_inputs(inputs, NL)
    nc = build_full(NL, debug=False)
    in_maps = [dict(shared, x=np.ascontiguousarray(x[c])) for c in range(n)]
    res = run_bass_kernel_spmd(nc, in_maps, core_ids=list(range(n)))
    return np.stack([np.asarray(res.results[c]["y"], dtype=np.float32) for c in range(n)], axis=0)
```

```python
import numpy as np
from contextlib import ExitStack
import concourse.bass as bass
import concourse.mybir as mybir
from concourse.bass_utils import run_bass_kernel_spmd

F32, BF16 = mybir.dt.float32, mybir.dt.bfloat16
ALU = mybir.AluOpType
AF = mybir.ActivationFunctionType
AX = mybir.AxisListType
ENGS = ("pe", "act", "dve", "pool", "sp")
SFX = [""]
SCOUNT = [0]
GLOBAL = {}


def _merge(d, s):
    for k, v in s.items():
        if d.get(k, 0) < v:
            d[k] = v


class Buf:
    def __init__(self, t, multi=False):
        self.t = t
        self.multi = multi
        self.w = {}
        self.r = {}
        self.dsem = None

    def __getitem__(self, k):
        return self.t[k]


class Stage:
    def __init__(self, nc, name):
        self.nc = nc
        name = name + SFX[0]
        self.name = name
        self.es = ExitStack()
        self.ops = {e: [] for e in ENGS}
        g = GLOBAL.get(id(nc))
        if g is None:
            ges = ExitStack()
            g = dict(es=ges, sem={e: ges.enter_context(nc.semaphore(f"g_{e}")) for e in ENGS}, cnt={e: 0 for e in ENGS}, pool=[])
            GLOBAL[id(nc)] = g
        self.g = g
        self.sem = g["sem"]
        self.cnt = dict(g["cnt"])
        self.dsems = {}
        self.ndsem = 0
        self.bufs = []
        self.semkey = {}

    def sb(self, name, shape, dt):
        t = self.es.enter_context(self.nc.sbuf_tensor(f"{self.name}_{name}", list(shape), dt))
        return self.track(Buf(t))

    def ps(self, name, shape=(128, 512), dt=F32):
        t = self.es.enter_context(self.nc.psum_tensor(f"{self.name}_{name}", list(shape), dt))
        return self.track(Buf(t))

    def track(self, b):
        b.w = {}
        b.r = {}
        b.dsem = None
        self.bufs.append(b)
        return b

    def _deps(self, reads, writes):
        deps = {}
        for b in reads:
            _merge(deps, b.w)
        for b in writes:
            _merge(deps, b.r)
            if not b.multi:
                _merge(deps, b.w)
        return deps

    def _commit(self, tok, reads, writes):
        for b in reads:
            _merge(b.r, tok)
        for b in writes:
            if b.multi:
                _merge(b.w, tok)
            else:
                b.w = dict(tok)
                b.r = {}

    def op(self, eng, fn, reads=(), writes=()):
        deps = self._deps(reads, writes)
        self.cnt[eng] += 1
        s = self.sem[eng]
        tok = {id(s): self.cnt[eng]}
        self.semkey[id(s)] = s
        self.ops[eng].append((deps, fn, s, 1))
        self._commit(tok, reads, writes)

    def dma(self, eng, out, in_, reads=(), writes=()):
        deps = self._deps(reads, writes)
        b = writes[0]
        if b.dsem is None:
            pool = self.g["pool"]
            if self.ndsem >= len(pool):
                pool.append([self.g["es"].enter_context(self.nc.semaphore(f"g_d{len(pool)}")), 0])
            b.dsem = pool[self.ndsem][0]
            self.dsems[id(b.dsem)] = pool[self.ndsem][1]
            self.semkey[id(b.dsem)] = b.dsem
            self.ndsem += 1
        self.dsems[id(b.dsem)] += 16
        tok = {id(b.dsem): self.dsems[id(b.dsem)]}
        self.ops[eng].append((deps, lambda e, o=out, i=in_: e.dma_start(out=o, in_=i), b.dsem, 16))
        self._commit(tok, reads, writes)

    def run(self):
        nc = self.nc
        final = {}
        for e in ENGS:
            if self.cnt[e] > self.g["cnt"][e]:
                final[id(self.sem[e])] = self.cnt[e]
                self.semkey[id(self.sem[e])] = self.sem[e]
        final.update({k: v for k, v in self.dsems.items()})

        def emit(engname, eng):
            waited = {}
            own = id(self.sem[engname])
            for deps, fn, s, n in self.ops[engname]:
                for k, v in deps.items():
                    if engname == "pe" and k == own:
                        continue
                    if waited.get(k, 0) < v:
                        eng.wait_ge(self.semkey[k], v)
                        waited[k] = v
                fn(eng).then_inc(s, n)
            for k, v in final.items():
                if waited.get(k, 0) < v:
                    eng.wait_ge(self.semkey[k], v)

        with nc.Block() as block:
            @block.tensor
            def _(e):
                emit("pe", e)

            @block.scalar
            def _(e):
                emit("act", e)

            @block.vector
            def _(e):
                emit("dve", e)

            @block.gpsimd
            def _(e):
                emit("pool", e)

            @block.sync
            def _(e):
                emit("sp", e)
        for e in ENGS:
            self.g["cnt"][e] = self.cnt[e]
        for p in self.g["pool"]:
            if id(p[0]) in self.dsems:
                p[1] = self.dsems[id(p[0])]
        for b in self.bufs:
            b.w = {}
            b.r = {}
            b.dsem = None
        self.es.close()

import math, os
T = 8192

def conv_stage(nc, QKT, QKC, convT_d):
    st = Stage(nc, "sB1"); st.track(QKT); st.track(QKC)
    cw = st.sb("cw", [128, 4, 4], F32)
    st.dma("sp", cw[:], convT_d.rearrange("(k p) j -> p k j", p=128), writes=[cw])
    win = [st.sb(f"win{i}", [128, 4, 515], F32) for i in range(2)]
    acc = [st.sb(f"acc{i}", [128, 4, 512], F32) for i in range(2)]
    out = [st.sb(f"out{i}", [128, 4, 512], F32) for i in range(2)]
    for tt in range(T // 512):
        w = win[tt % 2]; a = acc[tt % 2]; o = out[tt % 2]
        if tt == 0:
            st.op("pool", lambda e, w=w: e.memset(w[:, :, 0:3], 0.0), writes=[w])
            st.dma("sp", w[:, :, 3:515], QKT[:, 0:512].rearrange("(k p) t -> p k t", p=128), reads=[QKT], writes=[w])
        else:
            st.dma("sp", w[:], QKT[:, tt*512-3:(tt+1)*512].rearrange("(k p) t -> p k t", p=128), reads=[QKT], writes=[w])
        for k in range(4):
            eng = "dve"
            st.op(eng, lambda e, w=w, a=a, k=k: e.tensor_scalar(a[:, k, :], w[:, k, 0:512], cw[:, k, 0:1], None, ALU.mult), reads=[w, cw], writes=[a])
            for j in range(1, 4):
                st.op(eng, lambda e, w=w, a=a, k=k, j=j: e.scalar_tensor_tensor(a[:, k, :], w[:, k, j:j+512], cw[:, k, j:j+1], a[:, k, :], ALU.mult, ALU.add), reads=[w, cw, a], writes=[a])
        st.op("act", lambda e, a=a, o=o: e.activation(o[:], a[:], AF.Silu), reads=[a], writes=[o])
        st.op("dve", lambda e, o=o: e.tensor_scalar(o[:, 2:4, :], o[:, 2:4, :], 0.125, None, ALU.mult), reads=[o], writes=[o])
        st.dma("sp", QKC[:, tt*512:(tt+1)*512].rearrange("(k p) t -> p k t", p=128), o[:], reads=[o], writes=[QKC])
    st.run()

def layer_norm(st, r, ones, gcol, bcol, out, pss, tmp):
    sq, mean, rstd = tmp["sq"], tmp["mean"], tmp["rstd"]
    ps_s, ps_q = pss
    st.op("act", lambda e: e.activation(sq[:], r[:], AF.Square), reads=[r], writes=[sq])
    for k in range(8):
        st.op("pe", lambda e, k=k: e.matmul(ps_s[:], ones[:], r[:, k, :], start=(k == 0), stop=(k == 7)), reads=[ones, r], writes=[ps_s])
    for k in range(8):
        st.op("pe", lambda e, k=k: e.matmul(ps_q[:], ones[:], sq[:, k, :], start=(k == 0), stop=(k == 7)), reads=[ones, sq], writes=[ps_q])
    st.op("dve", lambda e: e.tensor_scalar(mean[:], ps_s[:], 1.0 / 1024, None, ALU.mult), reads=[ps_s], writes=[mean])
    st.op("dve", lambda e: e.tensor_tensor(rstd[:], mean[:], mean[:], ALU.mult), reads=[mean], writes=[rstd])
    st.op("dve", lambda e: e.scalar_tensor_tensor(rstd[:], ps_q[:], 1.0 / 1024, rstd[:], ALU.mult, ALU.subtract), reads=[ps_q, rstd], writes=[rstd])
    st.op("dve", lambda e: e.tensor_scalar(rstd[:], rstd[:], 1e-5, None, ALU.add), reads=[rstd], writes=[rstd])
    st.op("act", lambda e: e.activation(rstd[:], rstd[:], AF.Ln), reads=[rstd], writes=[rstd])
    st.op("act", lambda e: e.activation(rstd[:], rstd[:], AF.Exp, scale=-0.5), reads=[rstd], writes=[rstd])
    st.op("dve", lambda e: e.tensor_tensor(sq[:], r[:], mean[:].unsqueeze(1).to_broadcast([128, 8, 512]), ALU.subtract), reads=[r, mean], writes=[sq])
    st.op("pool", lambda e: e.tensor_tensor(sq[:], sq[:], rstd[:].unsqueeze(1).to_broadcast([128, 8, 512]), ALU.mult), reads=[sq, rstd], writes=[sq])
    for k in range(8):
        eng = "dve" if k % 2 == 0 else "pool"
        st.op(eng, lambda e, k=k: e.tensor_scalar(out[:, k, :], sq[:, k, :], gcol[:, k:k+1], bcol[:, k:k+1], ALU.mult, ALU.add), reads=[sq, gcol, bcol], writes=[out])

def mlstm_stage(nc, QKC, VAO, YAT, gb_d, ng_d, U_d, ident_d):
    st = Stage(nc, "sB2"); st.track(QKC); st.track(VAO); st.track(YAT)
    U = st.sb("U", [64, 64], F32); st.dma("sp", U[:], U_d[:, :], writes=[U])
    ident = st.sb("ident", [128, 128], F32); st.dma("sp", ident[:], ident_d[:, :], writes=[ident])
    gb = st.sb("gb", [64, 8], F32); st.dma("sp", gb[:], gb_d.partition_broadcast(64), writes=[gb])
    ng = st.sb("ng", [64, 512], F32); st.dma("sp", ng[:], ng_d.partition_broadcast(64), writes=[ng])
    ones64 = st.sb("ones64", [64, 64], F32); st.op("pool", lambda e: e.memset(ones64[:], 1.0), writes=[ones64])
    maskb = st.sb("maskb", [64, 4, 64], F32)
    st.op("dve", lambda e: e.tensor_copy(maskb[:], U[:].unsqueeze(1).to_broadcast([64, 4, 64])), reads=[U], writes=[maskb])
    C = st.sb("C", [64, 4, 129], F32); st.op("pool", lambda e: e.memset(C[:], 0.0), writes=[C])
    q4s = [st.sb(f"q4{i}", [64, 8, 512], F32) for i in range(2)]
    vas = [st.sb(f"va{i}", [64, 8, 1032], F32) for i in range(2)]
    yos = [st.sb(f"yo{i}", [128, 4, 512], F32) for i in range(2)]
    def mk(i):
        d = {}
        for n, s in dict(g=[64, 8], e1=[64, 4], nl=[64, 4], nlb=[64, 4, 64], col=[64, 4], Dt=[64, 4, 64], Ebc=[64, 4, 64], wtmp=[64, 4], wcol=[64, 4],
                         dec=[64, 4], nbt=[64, 8], Wt=[64, 4, 64], qt=[64, 4, 64], kt=[64, 4, 64], vaug=[64, 4, 129], den=[64, 4], rden=[64, 4], hn=[64, 4, 128],
                         sm=[64, 4], xc=[64, 4, 128], sq=[64, 4, 128], ssq=[64, 4], rstd=[64, 4], sig=[64, 512], gs=[64, 512], y=[64, 512]).items():
            d[n] = st.sb(f"{n}{i}", s, F32)
        st.op("pool", lambda e, v=d["vaug"]: e.memset(v[:], 1.0), writes=[d["vaug"]])
        return d
    tmps = [mk(0), mk(1)]
    psA = st.ps("psA", [64, 512]); psB = st.ps("psB", [64, 512])
    pSK = st.ps("pSK", [64, 512])
    psN = [st.ps(f"psN{j}", [64, 512]) for j in range(2)]; psU = [st.ps(f"psU{j}", [64, 512]) for j in range(2)]
    psY = st.ps("psY", [128, 512])
    for tt in range(T // 512):
        q4 = q4s[tt % 2]; va = vas[tt % 2]; yo = yos[tt % 2]
        st.dma("sp", q4[:], QKC[:, tt*512:(tt+1)*512].rearrange("(g d) t -> d g t", d=64), reads=[QKC], writes=[q4])
        st.dma("sp", va[:], VAO[tt*512:(tt+1)*512, :].rearrange("(c p) n -> p c n", p=64), reads=[VAO], writes=[va])
        for c in range(8):
            d = tmps[c % 2]; cs = slice(c*64, (c+1)*64)
            g, e1, nl, nlb, col, Dt, Ebc, wtmp, wcol, dec, Wt, qt, kt, vaug, den, rden, hn, sm, xc, sq, ssq, rstd, sig, gs, y = [d[n] for n in
                ("g", "e1", "nl", "nlb", "col", "Dt", "Ebc", "wtmp", "wcol", "dec", "Wt", "qt", "kt", "vaug", "den", "rden", "hn", "sm", "xc", "sq", "ssq", "rstd", "sig", "gs", "y")]
            st.op("dve", lambda e, g=g, va=va, c=c: e.tensor_tensor(g[:], va[:, c, 1024:1032], gb[:], ALU.add), reads=[va, gb], writes=[g])
            st.op("act", lambda e, g=g, e1=e1: e.activation(e1[:], g[:, 4:8], AF.Exp, scale=-1.0), reads=[g], writes=[e1])
            st.op("dve", lambda e, e1=e1: e.tensor_scalar(e1[:], e1[:], 1.0, None, ALU.add), reads=[e1], writes=[e1])
            st.op("act", lambda e, nl=nl, e1=e1: e.activation(nl[:], e1[:], AF.Ln), reads=[e1], writes=[nl])
            st.op("dve", lambda e, nl=nl, nlb=nlb: e.tensor_copy(nlb[:], nl[:].unsqueeze(2).to_broadcast([64, 4, 64])), reads=[nl], writes=[nlb])
            st.op("pe", lambda e, nl=nl: e.matmul(psA[:, 0:4], U[:], nl[:], start=True, stop=True), reads=[U, nl], writes=[psA])
            st.op("pe", lambda e, nl=nl: e.matmul(psA[:, 4:8], ones64[:], nl[:], start=True, stop=True), reads=[ones64, nl], writes=[psA])
            for h in range(4):
                st.op("pe", lambda e, nlb=nlb, h=h: e.matmul(psB[:, h*64:(h+1)*64], nlb[:, h, :], U[:], start=True, stop=True), reads=[nlb, U], writes=[psB])
            nbt = d["nbt"]
            st.op("act", lambda e, nbt=nbt: e.copy(nbt[:], psA[:, 0:8]), reads=[psA], writes=[nbt])
            st.op("dve", lambda e, col=col, g=g, nbt=nbt: e.tensor_tensor(col[:], nbt[:, 0:4], g[:, 0:4], ALU.add), reads=[nbt, g], writes=[col])
            for h in range(4):
                st.op("act", lambda e, Dt=Dt, col=col, h=h: e.activation(Dt[:, h, :], psB[:, h*64:(h+1)*64], AF.Exp, bias=col[:, h:h+1], scale=-1.0), reads=[psB, col], writes=[Dt])
            st.op("act", lambda e, Ebc=Ebc: e.activation(Ebc[:].rearrange("p h t -> p (h t)"), psB[:, 0:256], AF.Exp, scale=-1.0), reads=[psB], writes=[Ebc])
            st.op("dve", lambda e, wtmp=wtmp, col=col, nbt=nbt: e.tensor_tensor(wtmp[:], col[:], nbt[:, 4:8], ALU.subtract), reads=[col, nbt], writes=[wtmp])
            st.op("act", lambda e, wcol=wcol, wtmp=wtmp: e.activation(wcol[:], wtmp[:], AF.Exp), reads=[wtmp], writes=[wcol])
            st.op("act", lambda e, dec=dec, nbt=nbt: e.activation(dec[:], nbt[:, 4:8], AF.Exp, scale=-1.0), reads=[nbt], writes=[dec])
            for h in range(4):
                st.op("pe", lambda e, q4=q4, h=h, cs=cs: e.matmul(pSK[:, h*64:(h+1)*64], q4[:, 4+h, cs], q4[:, h, cs], start=True, stop=True), reads=[q4], writes=[pSK])
            st.op("dve", lambda e, Wt=Wt, Dt=Dt: e.tensor_tensor(Wt[:], Dt[:], maskb[:], ALU.mult), reads=[Dt, maskb], writes=[Wt])
            st.op("dve", lambda e, Wt=Wt: e.tensor_tensor(Wt[:], Wt[:], pSK[:, 0:256].rearrange("p (h t) -> p h t", h=4), ALU.mult), reads=[Wt, pSK], writes=[Wt])
            st.op("pool", lambda e, qt=qt, q4=q4, Ebc=Ebc, cs=cs: e.tensor_tensor(qt[:], q4[:, 0:4, cs], Ebc[:], ALU.mult), reads=[q4, Ebc], writes=[qt])
            for h in range(4):
                st.op("pe", lambda e, q4=q4, h=h, cs=cs: e.transpose(pSK[:, 256+h*64:256+(h+1)*64], q4[:, 4+h, cs], ident[0:64, 0:64]), reads=[q4, ident], writes=[pSK])
            st.op("dve", lambda e, kt=kt, wcol=wcol: e.tensor_tensor(kt[:], pSK[:, 256:512].rearrange("p (h t) -> p h t", h=4), wcol[:].unsqueeze(2).to_broadcast([64, 4, 64]), ALU.mult), reads=[pSK, wcol], writes=[kt])
            st.op("pool", lambda e, vaug=vaug, va=va, c=c: e.tensor_copy(vaug[:, :, 0:128], va[:, c, 0:512].rearrange("p (h v) -> p h v", h=4)), reads=[va], writes=[vaug])
            for h in range(4):
                pn = psN[h // 2]; o = (h % 2) * 129
                st.op("pe", lambda e, pn=pn, o=o, Wt=Wt, vaug=vaug, h=h: e.matmul(pn[:, o:o+129], Wt[:, h, :], vaug[:, h, :], start=True, stop=False), reads=[Wt, vaug], writes=[pn])
                st.op("pe", lambda e, pn=pn, o=o, qt=qt, h=h: e.matmul(pn[:, o:o+129], qt[:, h, :], C[:, h, :], start=False, stop=True), reads=[qt, C], writes=[pn])
            for h in range(4):
                pu = psU[h // 2]; o = (h % 2) * 129
                st.op("pe", lambda e, pu=pu, o=o, kt=kt, vaug=vaug, h=h: e.matmul(pu[:, o:o+129], kt[:, h, :], vaug[:, h, :], start=True, stop=True), reads=[kt, vaug], writes=[pu])
            for h in range(4):
                pu = psU[h // 2]; o = (h % 2) * 129
                st.op("dve", lambda e, pu=pu, o=o, dec=dec, h=h: e.scalar_tensor_tensor(C[:, h, :], C[:, h, :], dec[:, h:h+1], pu[:, o:o+129], ALU.mult, ALU.add), reads=[C, dec, pu], writes=[C])
            for j in range(2):
                pv = psN[j]
                st.op("act", lambda e, pv=pv, den=den, j=j: e.activation(den[:, 2*j:2*j+2], pv[:, 0:258].rearrange("p (i n) -> p i n", n=129)[:, :, 128], AF.Abs), reads=[pv], writes=[den])
            st.op("dve", lambda e, den=den: e.tensor_scalar(den[:], den[:], 1.0, None, ALU.max), reads=[den], writes=[den])
            st.op("dve", lambda e, den=den, rden=rden: e.reciprocal(rden[:], den[:]), reads=[den], writes=[rden])
            for j in range(2):
                pv = psN[j]
                st.op("dve", lambda e, pv=pv, hn=hn, rden=rden, j=j: e.tensor_tensor(hn[:, 2*j:2*j+2, :], pv[:, 0:258].rearrange("p (i n) -> p i n", n=129)[:, :, 0:128], rden[:, 2*j:2*j+2].unsqueeze(2).to_broadcast([64, 2, 128]), ALU.mult), reads=[pv, rden], writes=[hn])
            st.op("dve", lambda e, sm=sm, hn=hn: e.tensor_reduce(sm[:], hn[:], AX.X, ALU.add), reads=[hn], writes=[sm])
            st.op("dve", lambda e, sm=sm: e.tensor_scalar(sm[:], sm[:], -1.0 / 128, None, ALU.mult), reads=[sm], writes=[sm])
            st.op("dve", lambda e, xc=xc, hn=hn, sm=sm: e.tensor_tensor(xc[:], hn[:], sm[:].unsqueeze(2).to_broadcast([64, 4, 128]), ALU.add), reads=[hn, sm], writes=[xc])
            st.op("pool", lambda e, sq=sq, xc=xc: e.tensor_tensor(sq[:], xc[:], xc[:], ALU.mult), reads=[xc], writes=[sq])
            st.op("dve", lambda e, ssq=ssq, sq=sq: e.tensor_reduce(ssq[:], sq[:], AX.X, ALU.add), reads=[sq], writes=[ssq])
            st.op("dve", lambda e, ssq=ssq: e.tensor_scalar(ssq[:], ssq[:], 1.0 / 128, 1e-5, ALU.mult, ALU.add), reads=[ssq], writes=[ssq])
            st.op("act", lambda e, ssq=ssq, rstd=rstd: e.activation(rstd[:], ssq[:], AF.Ln), reads=[ssq], writes=[rstd])
            st.op("act", lambda e, rstd=rstd: e.activation(rstd[:], rstd[:], AF.Exp, scale=-0.5), reads=[rstd], writes=[rstd])
            st.op("act", lambda e, sig=sig, va=va, c=c: e.activation(sig[:], va[:, c, 512:1024], AF.Sigmoid), reads=[va], writes=[sig])
            st.op("pool", lambda e, gs=gs, sig=sig: e.tensor_tensor(gs[:], sig[:], ng[:], ALU.mult), reads=[sig, ng], writes=[gs])
            st.op("dve", lambda e, y=y, xc=xc, rstd=rstd: e.tensor_tensor(y[:].rearrange("p (h v) -> p h v", h=4), xc[:], rstd[:].unsqueeze(2).to_broadcast([64, 4, 128]), ALU.mult), reads=[xc, rstd], writes=[y])
            st.op("pool", lambda e, y=y, gs=gs: e.tensor_tensor(y[:], y[:], gs[:], ALU.mult), reads=[y, gs], writes=[y])
            for h in range(4):
                st.op("pe", lambda e, y=y, h=h: e.transpose(psY[:, h*64:(h+1)*64], y[:, h*128:(h+1)*128], ident[0:64, 0:64]), reads=[y, ident], writes=[psY])
            st.op("act", lambda e, yo=yo, cs=cs: e.copy(yo[:, :, cs], psY[:, 0:256].rearrange("p (h t) -> p h t", h=4)), reads=[psY], writes=[yo])
        st.dma("sp", YAT[:, tt*512:(tt+1)*512].rearrange("(k p) t -> p k t", p=128), yo[:], reads=[yo], writes=[YAT])
    st.run()

def t5_bucket_np(d):
    d = np.maximum(d, 0)
    lr = np.log(np.maximum(d, 1).astype(np.float32) / np.float32(16)) / np.float32(math.log(8.0))
    large = np.minimum(16 + (lr * 16).astype(np.int32), 31)
    return np.where(d < 16, d, large)

def host_consts():
    j = np.arange(512); d = j - 127
    oh = np.zeros((32, 512), np.float32)
    b = t5_bucket_np(d)
    for jj in range(511):
        if d[jj] >= 0: oh[b[jj], jj] = 1.0
    J = np.eye(128, dtype=np.float32)[::-1].copy()
    negtri = np.where(np.arange(128)[None, :] <= np.arange(128)[:, None], 0.0, -1e30).astype(np.float32)
    pow2 = np.tile((2.0 ** -np.arange(NIT)).astype(np.float32)[None, :], (128, 1))
    irep = np.tile(np.eye(128, dtype=np.float32), (1, 4))
    return dict(OH1=oh, J=J, NEGTRI=negtri, POW2=pow2, IREP=irep, ident=np.eye(128, dtype=np.float32))

def bias_setup(nc, rb_d, OH1_d, J_d, BVh, BIASD):
    st = Stage(nc, "sbias")
    BV = st.track(Buf(BVh.ap(), multi=True)); BD = st.track(Buf(BIASD, multi=True))
    rb = st.sb("rb", [32, 8], F32); st.dma("sp", rb[:], rb_d[:, :], writes=[rb])
    oh = st.sb("oh", [32, 512], F32); st.dma("sp", oh[:], OH1_d[:, :], writes=[oh])
    J = st.sb("J", [128, 128], F32); st.dma("sp", J[:], J_d[:, :], writes=[J])
    ps = st.ps("ps"); bv = st.sb("bv", [8, 512], F32)
    st.op("pe", lambda e: e.matmul(ps[0:8, :], rb[:], oh[:], start=True, stop=True), reads=[rb, oh], writes=[ps])
    st.op("dve", lambda e: e.tensor_scalar(bv[:], ps[0:8, :], 8.0, None, ALU.mult), reads=[ps], writes=[bv])
    st.dma("sp", BV[:, :], bv[:], reads=[bv], writes=[BV])
    hk = st.sb("hk", [128, 8, 128], F32); hi = st.sb("hi", [128, 2, 1024], BF16); tmp = st.sb("tmp", [128, 512], F32)
    ps2 = st.ps("ps2")
    for kind in range(3):
        st.dma("sp", hk[:], bass.AP(BVh, 128 * kind, [[1, 128], [512, 8], [1, 128]]), reads=[BV], writes=[hk])
        for half in range(2):
            st.op("pe", lambda e, half=half: e.matmul(ps2[:], J[:], hk[:, half*4:(half+1)*4, :].rearrange("p h t -> p (h t)"), start=True, stop=True), reads=[J, hk], writes=[ps2])
            st.op("act", lambda e, half=half: e.copy(hi[:, 0, half*512:(half+1)*512], ps2[:]), reads=[ps2], writes=[hi])
            st.op("dve", lambda e, half=half: e.tensor_tensor(tmp[:], ps2[:], hi[:, 0, half*512:(half+1)*512], ALU.subtract), reads=[ps2, hi], writes=[tmp])
            st.op("dve", lambda e, half=half: e.tensor_copy(hi[:, 1, half*512:(half+1)*512], tmp[:]), reads=[tmp], writes=[hi])
        st.dma("sp", BD[kind], hi[:], reads=[hi], writes=[BD])
    st.run()

def dsa_prep(nc, BQT, BC, wuk_d, kvg_d, ident_d, CKT, CKV, QLT):
    st = Stage(nc, "sC1")
    for b in (BQT, BC, CKT, CKV, QLT): st.track(b)
    wuk = st.sb("wuk", [64, 8, 128], BF16); st.dma("pool", wuk[:], wuk_d.rearrange("h d c -> d h c"), writes=[wuk])
    kvg = st.sb("kvg", [128, 128], F32); st.dma("sp", kvg[:], kvg_d.partition_broadcast(128), writes=[kvg])
    ident = st.sb("ident", [128, 128], F32); st.dma("sp", ident[:], ident_d[:, :], writes=[ident])
    bqs = [st.sb(f"bq{i}", [64, 8, 512], F32) for i in range(2)]; bqb = [st.sb(f"bqb{i}", [64, 8, 512], BF16) for i in range(2)]
    qls = [st.sb(f"ql{i}", [128, 8, 512], BF16) for i in range(2)]
    bcs = [st.sb(f"bc{i}", [128, 4, 128], F32) for i in range(2)]
    sq = st.sb("sq", [128, 4, 128], F32); ssq = st.sb("ssq", [128, 4], F32); rstd = st.sb("rstd", [128, 4], F32)
    ckv = st.sb("ckv", [128, 4, 128], F32)
    ckvb = [st.sb(f"ckvb{i}", [128, 4, 129], BF16) for i in range(2)]
    for c in ckvb: st.op("pool", lambda e, c=c: e.memset(c[:], 1.0), writes=[c])
    ckt = [st.sb(f"ckt{i}", [128, 512], BF16) for i in range(2)]
    pss = [st.ps(f"ps{i}") for i in range(4)]; pst = st.ps("pst")
    pi = 0
    for tt in range(T // 512):
        bq = bqs[tt % 2]; bb = bqb[tt % 2]; ql = qls[tt % 2]; bc = bcs[tt % 2]; cb = ckvb[tt % 2]; ct = ckt[tt % 2]
        st.dma("sp", bq[:], BQT[:, tt*512:(tt+1)*512].rearrange("(g d) t -> d g t", d=64), reads=[BQT], writes=[bq])
        st.op("pool", lambda e, bq=bq, bb=bb: e.tensor_copy(bb[:], bq[:]), reads=[bq], writes=[bb])
        for h in range(8):
            ps = pss[pi % 4]; pi += 1
            st.op("pe", lambda e, ps=ps, h=h, bb=bb: e.matmul(ps[:], wuk[:, h, :], bb[:, h, :], start=True, stop=True), reads=[wuk, bb], writes=[ps])
            if h % 2 == 0:
                st.op("act", lambda e, ps=ps, h=h, ql=ql: e.copy(ql[:, h, :], ps[:]), reads=[ps], writes=[ql])
            else:
                st.op("dve", lambda e, ps=ps, h=h, ql=ql: e.tensor_copy(ql[:, h, :], ps[:]), reads=[ps], writes=[ql])
        for q in range(4):
            st.dma("sp", QLT[:, tt*4+q, :, :], ql[:, :, q*128:(q+1)*128], reads=[ql], writes=[QLT])
        st.dma("sp", bc[:], BC[tt*512:(tt+1)*512, :].rearrange("(s p) c -> p s c", p=128), reads=[BC], writes=[bc])
        st.op("pool", lambda e, bc=bc: e.tensor_tensor(sq[:], bc[:], bc[:], ALU.mult), reads=[bc], writes=[sq])
        st.op("dve", lambda e: e.tensor_reduce(ssq[:], sq[:], AX.X, ALU.add), reads=[sq], writes=[ssq])
        st.op("dve", lambda e: e.tensor_scalar(ssq[:], ssq[:], 1.0 / 128, 1e-5, ALU.mult, ALU.add), reads=[ssq], writes=[ssq])
        st.op("act", lambda e: e.activation(rstd[:], ssq[:], AF.Ln), reads=[ssq], writes=[rstd])
        st.op("act", lambda e: e.activation(rstd[:], rstd[:], AF.Exp, scale=-0.5), reads=[rstd], writes=[rstd])
        st.op("dve", lambda e, bc=bc: e.tensor_tensor(ckv[:], bc[:], rstd[:].unsqueeze(2).to_broadcast([128, 4, 128]), ALU.mult), reads=[bc, rstd], writes=[ckv])
        st.op("pool", lambda e: e.tensor_tensor(ckv[:], ckv[:], kvg[:].unsqueeze(1).to_broadcast([128, 4, 128]), ALU.mult), reads=[ckv, kvg], writes=[ckv])
        st.op("pool", lambda e, cb=cb: e.tensor_copy(cb[:, :, 0:128], ckv[:]), reads=[ckv], writes=[cb])
        st.dma("sp", CKV[tt*512:(tt+1)*512, :].rearrange("(s p) c -> p s c", p=128), cb[:], reads=[cb], writes=[CKV])
        for s in range(4):
            st.op("pe", lambda e, s=s: e.transpose(pst[:, s*128:(s+1)*128], ckv[:, s, :], ident[:]), reads=[ckv, ident], writes=[pst])
        st.op("act", lambda e, ct=ct: e.copy(ct[:], pst[:]), reads=[pst], writes=[ct])
        st.dma("sp", CKT[:, tt*512:(tt+1)*512], ct[:], reads=[ct], writes=[CKT])
    st.run()

def dsa_attn(nc, name, q0, q1, IQT, IKT, IW, CKT, CKV, QLT, BIASD, OLT, C):
    st = Stage(nc, name)
    for b in (IQT, IKT, IW, CKT, CKV, QLT, OLT): st.track(b)
    BD = st.track(Buf(BIASD, multi=True))
    nkmax = q1 * 128
    cktS = st.sb("ckt", [128, nkmax], BF16); st.dma("sp", cktS[:], CKT[:, 0:nkmax], reads=[CKT], writes=[cktS])
    ckvS = st.sb("ckv", [128, q1, 129], BF16)
    for k0 in range(0, q1, 8):
        k1 = min(q1, k0 + 8)
        st.dma("sp", ckvS[:, k0:k1, :], CKV[k0*128:k1*128, :].rearrange("(k p) c -> p k c", p=128), reads=[CKV], writes=[ckvS])
    kiT = st.sb("kiT", [32, nkmax], F32); st.dma("sp", kiT[:], IKT[:, 0:nkmax], reads=[IKT], writes=[kiT])
    bias = st.sb("bias", [128, 3, 2, 1024], BF16)
    for kind in range(3): st.dma("sp", bias[:, kind], BD[kind], reads=[BD], writes=[bias])
    identb = st.sb("identb", [128, 128], BF16); st.dma("pool", identb[:], C["ident"][:, :], writes=[identb])
    ident = st.sb("ident", [128, 128], F32); st.dma("sp", ident[:], C["ident"][:, :], writes=[ident])
    irep = st.sb("irep", [128, 512], BF16); st.dma("pool", irep[:], C["IREP"][:, :], writes=[irep])
    negtri = st.sb("negtri", [128, 128], F32); st.dma("sp", negtri[:], C["NEGTRI"][:, :], writes=[negtri])
    pow2 = st.sb("pow2", [128, NIT], F32); st.dma("sp", pow2[:], C["POW2"][:, :], writes=[pow2])
    accs = [st.sb(f"acc{i}", [128, nkmax], F32) for i in range(2)]
    NMs = [st.sb(f"NM{i}", [128, nkmax], BF16) for i in range(2)]
    qis = [st.sb(f"qi{i}", [32, 8, 128], F32) for i in range(2)]; ws = [st.sb(f"w{i}", [128, 8], F32) for i in range(2)]
    qls = [st.sb(f"ql{i}", [128, 8, 128], BF16) for i in range(2)]
    rs = [st.sb(f"r{i}", [128, 512], F32) for i in range(4)]
    PTs = [st.sb(f"PT{i}", [128, 512], BF16) for i in range(3)]
    sm = {n: st.sb(n, s, F32) for n, s in dict(den=[128, 8], rden=[128, 8]).items()}
    ol = st.sb("ol", [128, 8, 128], F32); olT = [st.sb(f"olT{i}", [128, 8, 128], BF16) for i in range(2)]
    psL = [st.ps(f"psL{i}") for i in range(3)]; psO = [st.ps(f"psO{i}") for i in range(3)]; psI = [st.ps(f"psI{i}") for i in range(2)]
    OH = [(0, 0), (0, 1), (0, 2), (1, 0), (1, 1), (1, 2), (2, 0), (2, 1)]
    sms = [{n: st.sb(f"{n}{i}", s, F32) for n, s in dict(A=[128, 1], lo=[128, 1], steps=[128, NIT], mid=[128, 1], cnt=[128, 1], ge=[128, 1]).items()} for i in range(2)]
    state = dict(li=0, ii=0)

    def score(qt):
        nk = (qt + 1) * 128; qs = slice(qt*128, (qt+1)*128)
        qi = qis[qt % 2]; w = ws[qt % 2]; ql = qls[qt % 2]; acc = accs[qt % 2]
        st.dma("sp", qi[:], IQT[:, qs].rearrange("(g d) t -> d g t", d=32), reads=[IQT], writes=[qi])
        st.dma("sp", w[:], IW[qs, :], reads=[IW], writes=[w])
        st.dma("sp", ql[:], QLT[:, qt, :, :], reads=[QLT], writes=[ql])
        st.op("pool", lambda e, w=w: e.tensor_scalar(w[:], w[:], 1.0 / 16, None, ALU.mult), reads=[w], writes=[w])
        for kb in range((nk + 511) // 512):
            n = min(512, nk - kb*512); ks = slice(kb*512, kb*512 + n)
            for h in range(8):
                ii = state["ii"]; ps = psI[ii % 2]; r = rs[ii % 4]; state["ii"] += 1
                st.op("pe", lambda e, ps=ps, qi=qi, h=h, ks=ks, n=n: e.matmul(ps[:, 0:n], qi[:, h, :], kiT[:, ks], start=True, stop=True), reads=[qi, kiT], writes=[ps])
                st.op("act", lambda e, ps=ps, r=r, n=n: e.activation(r[:, 0:n], ps[:, 0:n], AF.Relu), reads=[ps], writes=[r])
                if h == 0:
                    st.op("dve", lambda e, r=r, w=w, ks=ks, n=n, acc=acc: e.tensor_scalar(acc[:, ks], r[:, 0:n], w[:, 0:1], None, ALU.mult), reads=[r, w], writes=[acc])
                else:
                    st.op("dve", lambda e, r=r, w=w, ks=ks, n=n, h=h, acc=acc: e.scalar_tensor_tensor(acc[:, ks], r[:, 0:n], w[:, h:h+1], acc[:, ks], ALU.mult, ALU.add), reads=[r, w, acc], writes=[acc])

    def bisect(qt):
        nk = (qt + 1) * 128; acc = accs[qt % 2]; NM = NMs[qt % 2]
        A, lo, steps, mid, cnt, ge = [sms[qt % 2][n] for n in ("A", "lo", "steps", "mid", "cnt", "ge")]
        st.op("dve", lambda e: e.reduce_max(A[:], acc[:, 0:nk], AX.X, apply_absolute_value=True), reads=[acc], writes=[A])
        st.op("dve", lambda e: e.tensor_tensor(acc[:, nk-128:nk], acc[:, nk-128:nk], negtri[:], ALU.add), reads=[acc, negtri], writes=[acc])
        st.op("dve", lambda e: e.tensor_scalar(A[:], A[:], 1.0, None, ALU.add), reads=[A], writes=[A])
        st.op("dve", lambda e: e.tensor_scalar(lo[:], A[:], -1.0, None, ALU.mult), reads=[A], writes=[lo])
        st.op("dve", lambda e: e.tensor_scalar(steps[:], pow2[:], A[:, 0:1], None, ALU.mult), reads=[pow2, A], writes=[steps])
        for k in range(NIT):
            st.op("dve", lambda e, k=k: e.tensor_tensor(mid[:], lo[:], steps[:, k:k+1], ALU.add), reads=[lo, steps], writes=[mid])
            st.op("dve", lambda e: e.tensor_scalar(NM[:, 0:nk], acc[:, 0:nk], mid[:, 0:1], 0.0, ALU.is_ge, ALU.add, accum_out=cnt[:, 0:1]), reads=[acc, mid], writes=[NM, cnt])
            st.op("dve", lambda e: e.tensor_scalar(ge[:], cnt[:], 255.5, None, ALU.is_ge), reads=[cnt], writes=[ge])
            st.op("dve", lambda e, k=k: e.scalar_tensor_tensor(lo[:], ge[:], steps[:, k:k+1], lo[:], ALU.mult, ALU.add), reads=[ge, steps, lo], writes=[lo])
        st.op("dve", lambda e: e.tensor_scalar(NM[:, 0:nk], acc[:, 0:nk], lo[:, 0:1], -30000.0, ALU.is_lt, ALU.mult), reads=[acc, lo], writes=[NM])

    def attn(qt):
        qs = slice(qt*128, (qt+1)*128)
        ql = qls[qt % 2]; NM = NMs[qt % 2]; oT = olT[qt % 2]
        den, rden = sm["den"], sm["rden"]
        its = [(kt, half) for kt in range(qt + 1) for half in range(2)]
        slots = []

        def qk(i):
            kt, half = its[i]
            kind = min(qt - kt, 2); kts = slice(kt*128, (kt+1)*128)
            li = state["li"]; ps = psL[li % 3]; PT = PTs[li % 3]; state["li"] += 1
            slots.append((ps, PT))
            hs = slice(half*512, (half+1)*512)
            st.op("pe", lambda e: e.matmul(ps[:], cktS[:, kts], ql[:, half*4:(half+1)*4, :].rearrange("p h t -> p (h t)"), start=True, stop=False), reads=[cktS, ql], writes=[ps])
            st.op("pe", lambda e: e.matmul(ps[:], identb[:], bias[:, kind, 0, hs], start=False, stop=False), reads=[identb, bias], writes=[ps])
            st.op("pe", lambda e: e.matmul(ps[:], identb[:], bias[:, kind, 1, hs], start=False, stop=False), reads=[identb, bias], writes=[ps])
            st.op("pe", lambda e: e.matmul(ps[:], NM[:, kts], irep[:], start=False, stop=True), reads=[NM, irep], writes=[ps])

        def pv(i):
            kt, half = its[i]; ps, PT = slots[i]
            st.op("act", lambda e: e.activation(PT[:], ps[:], AF.Exp, scale=0.125), reads=[ps], writes=[PT])
            for hh in range(4):
                h = half*4 + hh; bnk, slot = OH[h]; po = psO[bnk]
                st.op("pe", lambda e, po=po, slot=slot, hh=hh: e.matmul(po[:, slot*129:(slot+1)*129], PT[:, hh*128:(hh+1)*128], ckvS[:, kt, :], start=(kt == 0 and slot == 0), stop=(kt == qt), skip_group_check=True),
                      reads=[PT, ckvS], writes=[po])

        qk(0)
        for i in range(len(its)):
            if i + 1 < len(its):
                qk(i + 1)
            pv(i)
        for bnk in range(3):
            nh = 3 if bnk < 2 else 2; h0 = bnk*3; po = psO[bnk]
            st.op("act", lambda e, po=po, nh=nh, h0=h0: e.copy(den[:, h0:h0+nh], po[:, 0:nh*129].rearrange("p (i n) -> p i n", n=129)[:, :, 128]), reads=[po], writes=[den])
        st.op("dve", lambda e: e.reciprocal(rden[:], den[:]), reads=[den], writes=[rden])
        for bnk in range(3):
            nh = 3 if bnk < 2 else 2; h0 = bnk*3; po = psO[bnk]
            st.op("dve", lambda e, po=po, nh=nh, h0=h0: e.tensor_tensor(ol[:, h0:h0+nh, :], po[:, 0:nh*129].rearrange("p (i n) -> p i n", n=129)[:, :, 0:128], rden[:, h0:h0+nh].unsqueeze(2).to_broadcast([128, nh, 128]), ALU.mult), reads=[po, rden], writes=[ol])
        for half in range(2):
            li = state["li"]; ps = psL[li % 3]; state["li"] += 1
            for hh in range(4):
                st.op("pe", lambda e, ps=ps, hh=hh, half=half: e.transpose(ps[:, hh*128:(hh+1)*128], ol[:, half*4+hh, :], ident[:]), reads=[ol, ident], writes=[ps])
            st.op("act", lambda e, ps=ps, oT=oT, half=half: e.copy(oT[:, half*4:(half+1)*4, :].rearrange("p h t -> p (h t)"), ps[:]), reads=[ps], writes=[oT])
        st.dma("sp", OLT[:, :, qs], oT[:], reads=[oT], writes=[OLT])

    score(q0)
    bisect(q0)
    for qt in range(q0, q1):
        if qt + 1 < q1:
            score(qt + 1)
        attn(qt)
        if qt + 1 < q1:
            bisect(qt + 1)
    st.run()


OFF = dict(a_qk=0, a_v=512, a_o=1024, a_if=1536, b_q=1544, b_c=2056, i_q=2184, i_k=2440, i_w=2472, g_a=2480, g_b=3504)
ALPHA = 8 ** 0.25
D = 1024


def transpose_in_stage(nc, x_in, XT0, ident_d):
    st = Stage(nc, "s0"); st.track(XT0)
    ident = st.sb("ident", [128, 128], F32)
    st.dma("sp", ident[:], ident_d[:, :], writes=[ident])
    xin = [st.sb(f"xin{i}", [128, 4, D], F32) for i in range(2)]
    xo = [st.sb(f"xo{i}", [128, 8, 512], F32) for i in range(2)]
    pss = [st.ps(f"ps{i}") for i in range(4)]
    pi = 0
    for tt in range(T // 512):
        xi = xin[tt % 2]; xot = xo[tt % 2]
        st.dma("sp", xi[:], x_in[tt*512:(tt+1)*512, :].rearrange("(s p) d -> p s d", p=128), writes=[xi])
        for k in range(8):
            ps = pss[pi % 4]; pi += 1
            for s in range(4):
                st.op("pe", lambda e, ps=ps, xi=xi, s=s, k=k: e.transpose(ps[:, s*128:(s+1)*128], xi[:, s, k*128:(k+1)*128], ident[:]),
                      reads=[xi, ident], writes=[ps])
            if k % 2 == 0:
                st.op("dve", lambda e, ps=ps, xot=xot, k=k: e.tensor_copy(xot[:, k, :], ps[:]), reads=[ps], writes=[xot])
            else:
                st.op("act", lambda e, ps=ps, xot=xot, k=k: e.copy(xot[:, k, :], ps[:]), reads=[ps], writes=[xot])
        st.dma("sp", XT0[:, tt*512:(tt+1)*512].rearrange("(k p) t -> p k t", p=128), xot[:], reads=[xot], writes=[XT0])
    st.run()


def transpose_out_stage(nc, XO, y_out, ident_d):
    st = Stage(nc, "s9"); st.track(XO)
    Y = st.track(Buf(y_out, multi=True))
    ident = st.sb("ident", [128, 128], F32)
    st.dma("sp", ident[:], ident_d[:, :], writes=[ident])
    xin = [st.sb(f"xin{i}", [128, 8, 512], F32) for i in range(2)]
    yo = [st.sb(f"yo{i}", [128, 4, D], F32) for i in range(2)]
    pss = [st.ps(f"ps{i}") for i in range(4)]
    pi = 0
    for tt in range(T // 512):
        xi = xin[tt % 2]; yt = yo[tt % 2]
        st.dma("sp", xi[:], XO[:, tt*512:(tt+1)*512].rearrange("(k p) t -> p k t", p=128), reads=[XO], writes=[xi])
        for s in range(4):
            for half in range(2):
                ps = pss[pi % 4]; pi += 1
                for kk in range(4):
                    k = half * 4 + kk
                    st.op("pe", lambda e, ps=ps, xi=xi, s=s, k=k, kk=kk: e.transpose(ps[:, kk*128:(kk+1)*128], xi[:, k, s*128:(s+1)*128], ident[:]),
                          reads=[xi, ident], writes=[ps])
                if half == 0:
                    st.op("dve", lambda e, ps=ps, yt=yt, s=s: e.tensor_copy(yt[:, s, 0:512], ps[:]), reads=[ps], writes=[yt])
                else:
                    st.op("act", lambda e, ps=ps, yt=yt, s=s: e.copy(yt[:, s, 512:1024], ps[:]), reads=[ps], writes=[yt])
        st.dma("sp", Y[tt*512:(tt+1)*512, :].rearrange("(s p) d -> p s d", p=128), yt[:], reads=[yt], writes=[Y])
    st.run()


def inproj_stage(nc, XT0, w_in, S):
    st = Stage(nc, "sA"); st.track(XT0)
    for b in S.values(): st.track(b)
    wb = st.sb("wb", [128, 8, 4528], BF16)
    wf = st.sb("wf", [128, 8, 296], F32)
    for k in range(8):
        st.dma("pool", wb[:, k, :], w_in[k*128:(k+1)*128, :], writes=[wb])
    st.dma("sp", wf[:], w_in[:, 2184:2480].rearrange("(k p) n -> p k n", p=128), writes=[wf])
    xf = [st.sb(f"xf{i}", [128, 8, 512], F32) for i in range(2)]
    xb = [st.sb(f"xb{i}", [128, 8, 512], BF16) for i in range(2)]
    ofm = [st.sb(f"ofm{i}", [128, 8, 512], F32) for i in range(2)]
    otm = [st.sb(f"otm{i}", [128, 4, 1032], F32) for i in range(2)]
    otb = [st.sb(f"otb{i}", [128, 4, 136], F32) for i in range(2)]
    pss = [st.ps(f"ps{i}") for i in range(6)]
    pi = [0]; oi = [0]

    def evac(ps_ap, out_ap, ps, ob, func=None):
        if func is not None or pi[0] % 2 == 1:
            st.op("act", lambda e: e.activation(out_ap, ps_ap, func if func is not None else AF.Copy), reads=[ps], writes=[ob])
        else:
            st.op("dve", lambda e: e.tensor_copy(out_ap, ps_ap), reads=[ps], writes=[ob])

    def fm_group(xt_b, wt, col0, ncols, dst, tt, func=None):
        M = min(128, ncols); nch = ncols // M
        ob = ofm[oi[0] % 2]; oi[0] += 1
        for c in range(nch):
            ps = pss[pi[0] % 6]; pi[0] += 1
            for k in range(8):
                st.op("pe", lambda e, ps=ps, c=c, k=k: e.matmul(ps[0:M, :], wt[:, k, col0+c*M:col0+(c+1)*M], xt_b[:, k, :], start=(k == 0), stop=(k == 7)),
                      reads=[wt, xt_b], writes=[ps])
            evac(ps[0:M, :], ob[0:M, c, :], ps, ob, func)
        if M == 128:
            st.dma("sp", dst[:, tt*512:(tt+1)*512].rearrange("(k p) t -> p k t", p=128), ob[:, 0:nch, :], reads=[ob], writes=[dst])
        else:
            st.dma("sp", dst[:, tt*512:(tt+1)*512], ob[0:M, 0, :], reads=[ob], writes=[dst])

    for tt in range(T // 512):
        xft = xf[tt % 2]; xbt = xb[tt % 2]
        st.dma("sp", xft[:], XT0[:, tt*512:(tt+1)*512].rearrange("(k p) t -> p k t", p=128), reads=[XT0], writes=[xft])
        st.op("pool", lambda e, xft=xft, xbt=xbt: e.tensor_copy(xbt[:], xft[:]), reads=[xft], writes=[xbt])
        fm_group(xbt, wb, OFF["a_qk"], 512, S["QKT"], tt)
        fm_group(xbt, wb, OFF["b_q"], 512, S["BQT"], tt)
        fm_group(xbt, wb, OFF["g_a"], 1024, S["GAT"], tt, func=AF.Sigmoid)
        fm_group(xbt, wb, OFF["g_b"], 1024, S["GBT"], tt, func=AF.Sigmoid)
        fm_group(xft, wf, 0, 256, S["IQT"], tt)
        fm_group(xft, wf, 256, 32, S["IKT"], tt)
        ob = otm[tt % 2]; ob2 = otb[tt % 2]
        for s in range(4):
            for (c0, n) in ((0, 512), (512, 512), (1024, 8)):
                ps = pss[pi[0] % 6]; pi[0] += 1
                for k in range(8):
                    st.op("pe", lambda e, ps=ps, s=s, k=k, c0=c0, n=n, xbt=xbt: e.matmul(ps[:, 0:n], xbt[:, k, s*128:(s+1)*128], wb[:, k, 512+c0:512+c0+n], start=(k == 0), stop=(k == 7)),
                          reads=[wb, xbt], writes=[ps])
                evac(ps[:, 0:n], ob[:, s, c0:c0+n], ps, ob)
            ps = pss[pi[0] % 6]; pi[0] += 1
            for k in range(8):
                st.op("pe", lambda e, ps=ps, s=s, k=k, xbt=xbt: e.matmul(ps[:, 0:128], xbt[:, k, s*128:(s+1)*128], wb[:, k, OFF["b_c"]:OFF["b_c"]+128], start=(k == 0), stop=(k == 7)),
                      reads=[wb, xbt], writes=[ps])
            evac(ps[:, 0:128], ob2[:, s, 0:128], ps, ob2)
            ps = pss[pi[0] % 6]; pi[0] += 1
            for k in range(8):
                st.op("pe", lambda e, ps=ps, s=s, k=k, xft=xft: e.matmul(ps[:, 0:8], xft[:, k, s*128:(s+1)*128], wf[:, k, 288:296], start=(k == 0), stop=(k == 7)),
                      reads=[wf, xft], writes=[ps])
            evac(ps[:, 0:8], ob2[:, s, 128:136], ps, ob2)
        st.dma("sp", S["VAO"][tt*512:(tt+1)*512, :].rearrange("(s p) n -> p s n", p=128), ob[:], reads=[ob], writes=[S["VAO"]])
        st.dma("sp", S["BC"][tt*512:(tt+1)*512, :].rearrange("(s p) n -> p s n", p=128), ob2[:, :, 0:128], reads=[ob2], writes=[S["BC"]])
        st.dma("sp", S["IW"][tt*512:(tt+1)*512, :].rearrange("(s p) n -> p s n", p=128), ob2[:, :, 128:136], reads=[ob2], writes=[S["IW"]])
    st.run()


def wc_stage(nc, wuvT_d, wbb_d, WC):
    st = Stage(nc, "sWc"); st.track(WC)
    wuvT = st.sb("wuvT", [64, 8, 128], BF16); st.dma("pool", wuvT[:], wuvT_d.rearrange("h d c -> d h c"), writes=[wuvT])
    wbb = st.sb("wbb", [64, 8, 1024], BF16); st.dma("pool", wbb[:], wbb_d.rearrange("(h d) n -> d h n", d=64), writes=[wbb])
    wc = st.sb("wc", [128, 8, 1024], BF16)
    pss = [st.ps(f"ps{i}") for i in range(4)]
    pi = 0
    for h in range(8):
        for half in range(2):
            ps = pss[pi % 4]; pi += 1
            st.op("pe", lambda e, ps=ps, h=h, half=half: e.matmul(ps[:], wuvT[:, h, :], wbb[:, h, half*512:(half+1)*512], start=True, stop=True), reads=[wuvT, wbb], writes=[ps])
            if pi % 2 == 0:
                st.op("act", lambda e, ps=ps, h=h, half=half: e.copy(wc[:, h, half*512:(half+1)*512], ps[:]), reads=[ps], writes=[wc])
            else:
                st.op("dve", lambda e, ps=ps, h=h, half=half: e.tensor_copy(wc[:, h, half*512:(half+1)*512], ps[:]), reads=[ps], writes=[wc])
    st.dma("sp", WC[:, :, :], wc[:], reads=[wc], writes=[WC])
    st.run()


def merge_stage(nc, XI, XO, YAT, OLT, GAT, GBT, WC, wa_d, wo_d, g_d, b_d, ones_d):
    st = Stage(nc, "sD")
    for b in (XI, XO, YAT, OLT, GAT, GBT, WC): st.track(b)
    ones = st.sb("ones", [128, 128], F32); st.dma("sp", ones[:], ones_d[:, :], writes=[ones])
    gcol = st.sb("gcol", [128, 8], F32); st.dma("sp", gcol[:], g_d, writes=[gcol])
    bcol = st.sb("bcol", [128, 8], F32); st.dma("sp", bcol[:], b_d, writes=[bcol])
    wa = st.sb("wa", [128, 4, 1024], BF16); st.dma("pool", wa[:], wa_d.rearrange("(k p) n -> p k n", p=128), writes=[wa])
    wo = st.sb("wo", [128, 8, 1024], BF16); st.dma("pool", wo[:], wo_d.rearrange("(k p) n -> p k n", p=128), writes=[wo])
    wc = st.sb("wc", [128, 8, 1024], BF16); st.dma("sp", wc[:], WC[:, :, :], reads=[WC], writes=[wc])
    yab = st.sb("yab", [128, 4, 512], BF16); ol = st.sb("ol", [128, 8, 512], BF16)
    ga = st.sb("ga", [128, 8, 512], F32); gb = st.sb("gb", [128, 8, 512], F32); x = st.sb("x", [128, 8, 512], F32)
    mg = st.sb("mg", [128, 8, 512], BF16)
    t1 = [st.sb(f"t1{i}", [128, 512], F32) for i in range(2)]; t2 = [st.sb(f"t2{i}", [128, 512], F32) for i in range(2)]
    tmp = dict(sq=st.sb("sq", [128, 8, 512], F32), mean=st.sb("mean", [128, 512], F32), rstd=st.sb("rstd", [128, 512], F32))
    psA = [st.ps(f"psA{i}") for i in range(2)]; psB = [st.ps(f"psB{i}") for i in range(2)]; psO = [st.ps(f"psO{i}") for i in range(2)]
    psS = st.ps("psS"); psQ = st.ps("psQ")
    for tt in range(T // 512):
        ts = slice(tt*512, (tt+1)*512)
        st.dma("pool", yab[:], YAT[:, ts].rearrange("(k p) t -> p k t", p=128), reads=[YAT], writes=[yab])
        st.dma("sp", ol[:], OLT[:, :, ts], reads=[OLT], writes=[ol])
        st.dma("sp", ga[:], GAT[:, ts].rearrange("(k p) t -> p k t", p=128), reads=[GAT], writes=[ga])
        st.dma("sp", gb[:], GBT[:, ts].rearrange("(k p) t -> p k t", p=128), reads=[GBT], writes=[gb])
        st.dma("sp", x[:], XI[:, ts].rearrange("(k p) t -> p k t", p=128), reads=[XI], writes=[x])
        for c in range(8):
            pa = psA[c % 2]; pb = psB[c % 2]; a1 = t1[c % 2]; a2 = t2[c % 2]; cs = slice(c*128, (c+1)*128)
            for k in range(4):
                st.op("pe", lambda e, pa=pa, k=k, cs=cs: e.matmul(pa[:], wa[:, k, cs], yab[:, k, :], start=(k == 0), stop=(k == 3)), reads=[wa, yab], writes=[pa])
            for h in range(8):
                st.op("pe", lambda e, pb=pb, h=h, cs=cs: e.matmul(pb[:], wc[:, h, cs], ol[:, h, :], start=(h == 0), stop=(h == 7)), reads=[wc, ol], writes=[pb])
            st.op("dve", lambda e, pa=pa, a1=a1, c=c: e.tensor_tensor(a1[:], pa[:], ga[:, c, :], ALU.mult), reads=[pa, ga], writes=[a1])
            st.op("dve", lambda e, pb=pb, a2=a2, c=c: e.tensor_tensor(a2[:], pb[:], gb[:, c, :], ALU.mult), reads=[pb, gb], writes=[a2])
            st.op("pool", lambda e, a1=a1, a2=a2, c=c: e.tensor_tensor(mg[:, c, :], a1[:], a2[:], ALU.add), reads=[a1, a2], writes=[mg])
        for c in range(8):
            po = psO[c % 2]; cs = slice(c*128, (c+1)*128)
            for k in range(8):
                st.op("pe", lambda e, po=po, k=k, cs=cs: e.matmul(po[:], wo[:, k, cs], mg[:, k, :], start=(k == 0), stop=(k == 7)), reads=[wo, mg], writes=[po])
            st.op("dve", lambda e, po=po, c=c: e.scalar_tensor_tensor(x[:, c, :], x[:, c, :], ALPHA, po[:], ALU.mult, ALU.add), reads=[x, po], writes=[x])
        layer_norm(st, x, ones, gcol, bcol, tmp["sq"], (psS, psQ), tmp)
        st.dma("sp", XO[:, ts].rearrange("(k p) t -> p k t", p=128), tmp["sq"][:], reads=[tmp["sq"]], writes=[XO])
    st.run()


def ffn_stage(nc, name, tb0, tb1, XI, XO, wg_d, wu_d, wd_d, g_d, b_d, C, nexp, dff, G, rw_d=None):
    TB = 1024; NF = dff // 128; NG = NF // G
    st = Stage(nc, name); st.track(XI); st.track(XO)
    ones = st.sb("ones", [128, 128], F32); st.dma("sp", ones[:], C["ones"][:, :], writes=[ones])
    gcol = st.sb("gcol", [128, 8], F32); st.dma("sp", gcol[:], g_d, writes=[gcol])
    bcol = st.sb("bcol", [128, 8], F32); st.dma("sp", bcol[:], b_d, writes=[bcol])
    moe = rw_d is not None
    xb = st.sb("xb", [128, 8, TB], BF16); y = st.sb("y", [128, 8, TB], F32)
    wgg = st.sb("wgg", [128, 8, G*128], BF16); wug = st.sb("wug", [128, 8, G*128], BF16); wdg = st.sb("wdg", [128, G, 1024], BF16)
    hg = st.sb("hg", [128, G, 2, 512], BF16)
    sg = [st.sb(f"sg{i}", [128, 512], F32) for i in range(2)]
    psg = [st.ps(f"psg{i}") for i in range(2)]; psu = [st.ps(f"psu{i}") for i in range(2)]
    psd = [st.ps(f"psd{i}") for i in range(4)]
    tmp = dict(sq=st.sb("sq", [128, 8, 512], F32), mean=st.sb("mean", [128, 512], F32), rstd=st.sb("rstd", [128, 512], F32))
    xr = st.sb("xr", [128, 8, 512], F32)
    if moe:
        ident = st.sb("ident", [128, 128], F32); st.dma("sp", ident[:], C["ident"][:, :], writes=[ident])
        rw = st.sb("rw", [128, 8, 8], F32); st.dma("sp", rw[:], rw_d.rearrange("(k p) e -> p k e", p=128), writes=[rw])
        sel = st.sb("sel", [8, 8, 128], F32); st.dma("sp", sel[:], C["SEL"][:, :, :], writes=[sel])
        xs = [st.sb(f"xs{i}", [128, 8, 128], F32) for i in range(2)]
        GT = st.sb("GT", [8, TB], F32); gbc = st.sb("gbc", [128, TB], F32)
        t2 = [st.sb(f"t2{i}", [128, 512], F32) for i in range(2)]
        sm = {n: st.sb(n, s, F32) for n, s in dict(lg=[128, 8], m8=[128, 8], dl=[128, 1], g1=[128, 1], g2=[128, 1], e1=[128, 8], e2=[128, 8]).items()}
    it = 0
    for tb in range(tb0, tb1):
        t0 = tb * TB
        st.dma("pool", xb[:], XI[:, t0:t0+TB].rearrange("(k p) t -> p k t", p=128), reads=[XI], writes=[xb])
        if moe:
            lg, m8, dl, g1, g2, e1, e2 = [sm[n] for n in ("lg", "m8", "dl", "g1", "g2", "e1", "e2")]
            for s in range(TB // 128):
                xst = xs[s % 2]; pr = psd[s % 4]
                st.dma("sp", xst[:], XI[:, t0+s*128:t0+(s+1)*128].rearrange("(k p) t -> p k t", p=128), reads=[XI], writes=[xst])
                for k in range(8):
                    st.op("pe", lambda e, pr=pr, xst=xst, k=k: e.matmul(pr[:, 0:8], xst[:, k, :], rw[:, k, :], start=(k == 0), stop=(k == 7)), reads=[xst, rw], writes=[pr])
                st.op("act", lambda e, pr=pr: e.copy(lg[:], pr[:, 0:8]), reads=[pr], writes=[lg])
                st.op("dve", lambda e: e.max(m8[:], lg[:]), reads=[lg], writes=[m8])
                st.op("dve", lambda e: e.tensor_tensor(dl[:], m8[:, 0:1], m8[:, 1:2], ALU.subtract), reads=[m8], writes=[dl])
                st.op("act", lambda e: e.activation(g1[:], dl[:], AF.Sigmoid), reads=[dl], writes=[g1])
                st.op("act", lambda e: e.activation(g2[:], dl[:], AF.Sigmoid, scale=-1.0), reads=[dl], writes=[g2])
                st.op("dve", lambda e: e.tensor_scalar(e1[:], lg[:], m8[:, 0:1], g1[:, 0:1], ALU.is_equal, ALU.mult), reads=[lg, m8, g1], writes=[e1])
                st.op("dve", lambda e: e.tensor_scalar(e2[:], lg[:], m8[:, 1:2], g2[:, 0:1], ALU.is_equal, ALU.mult), reads=[lg, m8, g2], writes=[e2])
                st.op("dve", lambda e: e.tensor_tensor(e1[:], e1[:], e2[:], ALU.add), reads=[e1, e2], writes=[e1])
                st.op("pe", lambda e, pr=pr: e.transpose(pr[0:8, 128:256], e1[:], ident[:]), reads=[e1, ident], writes=[pr])
                st.op("act", lambda e, pr=pr, s=s: e.copy(GT[:, s*128:(s+1)*128], pr[0:8, 128:256]), reads=[pr], writes=[GT])
        first = True
        for ex in range(nexp):
            if moe:
                for sub in range(2):
                    pr = psd[sub]
                    st.op("pe", lambda e, pr=pr, ex=ex, sub=sub: e.matmul(pr[:], sel[:, ex, :], GT[:, sub*512:(sub+1)*512], start=True, stop=True), reads=[sel, GT], writes=[pr])
                    st.op("act", lambda e, pr=pr, sub=sub: e.copy(gbc[:, sub*512:(sub+1)*512], pr[:]), reads=[pr], writes=[gbc])
            for grp in range(NG):
                f0 = grp * G * 128
                st.dma("pool", wgg[:], wg_d[ex, :, f0:f0+G*128].rearrange("(k p) n -> p k n", p=128), writes=[wgg])
                st.dma("pool", wug[:], wu_d[ex, :, f0:f0+G*128].rearrange("(k p) n -> p k n", p=128), writes=[wug])
                st.dma("pool", wdg[:], wd_d[ex, f0:f0+G*128, :].rearrange("(g p) n -> p g n", p=128), writes=[wdg])
                for fi in range(G):
                    fs = slice(fi*128, (fi+1)*128)
                    for sub in range(2):
                        pg, pu, sgt = psg[it % 2], psu[it % 2], sg[it % 2]
                        ss = slice(sub*512, (sub+1)*512)
                        for k in range(8):
                            st.op("pe", lambda e, pg=pg, k=k, fs=fs, ss=ss: e.matmul(pg[:], wgg[:, k, fs], xb[:, k, ss], start=(k == 0), stop=(k == 7)), reads=[wgg, xb], writes=[pg])
                        for k in range(8):
                            st.op("pe", lambda e, pu=pu, k=k, fs=fs, ss=ss: e.matmul(pu[:], wug[:, k, fs], xb[:, k, ss], start=(k == 0), stop=(k == 7)), reads=[wug, xb], writes=[pu])
                        st.op("act", lambda e, pg=pg, sgt=sgt: e.activation(sgt[:], pg[:], AF.Silu), reads=[pg], writes=[sgt])
                        if moe:
                            tt2 = t2[it % 2]
                            st.op("dve", lambda e, pu=pu, tt2=tt2, ss=ss: e.tensor_tensor(tt2[:], pu[:], gbc[:, ss], ALU.mult), reads=[pu, gbc], writes=[tt2])
                            st.op("pool", lambda e, sgt=sgt, tt2=tt2, fi=fi, sub=sub: e.tensor_tensor(hg[:, fi, sub, :], sgt[:], tt2[:], ALU.mult), reads=[sgt, tt2], writes=[hg])
                        else:
                            st.op("dve", lambda e, pu=pu, sgt=sgt, fi=fi, sub=sub: e.tensor_tensor(hg[:, fi, sub, :], sgt[:], pu[:], ALU.mult), reads=[pu, sgt], writes=[hg])
                        it += 1
                for sub in range(2):
                    ss = slice(sub*512, (sub+1)*512)
                    for c in range(8):
                        pd = psd[c % 4]; cs = slice(c*128, (c+1)*128)
                        for fi in range(G):
                            st.op("pe", lambda e, pd=pd, fi=fi, cs=cs, sub=sub: e.matmul(pd[:], wdg[:, fi, cs], hg[:, fi, sub, :], start=(fi == 0), stop=(fi == G-1)), reads=[wdg, hg], writes=[pd])
                        if first:
                            st.op("dve", lambda e, pd=pd, c=c, ss=ss: e.tensor_copy(y[:, c, ss], pd[:]), reads=[pd], writes=[y])
                        else:
                            st.op("dve", lambda e, pd=pd, c=c, ss=ss: e.tensor_tensor(y[:, c, ss], y[:, c, ss], pd[:], ALU.add), reads=[pd, y], writes=[y])
                first = False
        for sub in range(2):
            ss = slice(sub*512, (sub+1)*512); ts = slice(t0+sub*512, t0+(sub+1)*512)
            st.dma("sp", xr[:], XI[:, ts].rearrange("(k p) t -> p k t", p=128), reads=[XI], writes=[xr])
            st.op("dve", lambda e, ss=ss: e.scalar_tensor_tensor(xr[:], xr[:], ALPHA, y[:, :, ss], ALU.mult, ALU.add), reads=[xr, y], writes=[xr])
            layer_norm(st, xr, ones, gcol, bcol, tmp["sq"], (psg[0], psu[0]), tmp)
            st.dma("sp", XO[:, ts].rearrange("(k p) t -> p k t", p=128), tmp["sq"][:], reads=[tmp["sq"]], writes=[XO])
    st.run()

NIT = 22


def dump_stage(nc, name, SRC, dst_ap):
    st = Stage(nc, name); st.track(SRC)
    Dst = st.track(Buf(dst_ap, multi=True))
    t = [st.sb(f"t{i}", [128, 8, 512], F32) for i in range(2)]
    for tt in range(T // 512):
        ts = slice(tt*512, (tt+1)*512)
        st.dma("sp", t[tt % 2][:], SRC[:, ts].rearrange("(k p) t -> p k t", p=128), reads=[SRC], writes=[t[tt % 2]])
        st.dma("sp", Dst[:, ts].rearrange("(k p) t -> p k t", p=128), t[tt % 2][:], reads=[t[tt % 2]], writes=[Dst])
    st.run()


def build_full(NL, debug=False):
    nc = bass.Bass("TRN2", target_bir_lowering=False)
    NQ = T // 128; ND = (NL + 1) // 2; NM = NL // 2
    dr = lambda n, s, k="Internal", dt=F32: nc.dram_tensor(n, list(s), dt, kind=k).ap()
    ein = lambda n, s: dr(n, s, "ExternalInput")
    x_in = ein("x", [T, D]); w_in = ein("w_in", [NL, D, 4528]); convT = ein("convT", [NL, 512, 4])
    gbias = ein("gbias", [NL, 1, 8]); ng = ein("ng", [NL, 1, 512]); kvg = ein("kvg", [NL, 1, 128])
    wuk = ein("wuk", [NL, 8, 64, 128]); wuvT = ein("wuvT", [NL, 8, 64, 128]); rb = ein("rb", [32, 8])
    wba = ein("wba", [NL, 512, 1024]); wbb = ein("wbb", [NL, 512, 1024]); wout = ein("wout", [NL, 1024, 1024])
    lng = ein("lng", [NL, 2, 128, 8]); lnb = ein("lnb", [NL, 2, 128, 8])
    dwg = ein("dwg", [ND, 1, D, 2816]); dwu = ein("dwu", [ND, 1, D, 2816]); dwd = ein("dwd", [ND, 1, 2816, D])
    if NM:
        rw = ein("rw", [NM, D, 8]); ewg = ein("ewg", [NM, 8, D, 3584]); ewu = ein("ewu", [NM, 8, D, 3584]); ewd = ein("ewd", [NM, 8, 3584, D])
    C = {n: ein(n, s) for n, s in dict(OH1=[32, 512], J=[128, 128], NEGTRI=[128, 128], POW2=[128, NIT], IREP=[128, 512], ident=[128, 128],
                                       ones=[128, 128], U=[64, 64], SEL=[8, 8, 128]).items()}
    y = dr("y", [T, D], "ExternalOutput")
    dbg = [dr(f"dbg{l}", [D, T], "ExternalOutput") for l in range(NL)] if debug else None
    mb = lambda n, s, dt=F32: Buf(dr(n, s, dt=dt), multi=True)
    XTa = mb("XTa", [D, T]); XTb = mb("XTb", [D, T])
    S = dict(QKT=mb("QKT", [512, T]), BQT=mb("BQT", [512, T]), IQT=mb("IQT", [256, T]), IKT=mb("IKT", [32, T]),
             GAT=mb("GAT", [1024, T]), GBT=mb("GBT", [1024, T]), VAO=mb("VAO", [T, 1032]), BC=mb("BC", [T, 128]), IW=mb("IW", [T, 8]))
    QKC = mb("QKC", [512, T]); YAT = mb("YAT", [512, T])
    CKT = mb("CKT", [128, T], BF16); CKV = mb("CKV", [T, 129], BF16); QLT = mb("QLT", [128, NQ, 8, 128], BF16); OLT = mb("OLT", [128, 8, T], BF16)
    WC = mb("WC", [128, 8, 1024], BF16)
    BVh = nc.dram_tensor("BV", [8, 512], F32); BIASD = dr("BIASD", [3, 128, 2, 1024], dt=BF16)
    SFX[0] = ""
    transpose_in_stage(nc, x_in, XTa, C["ident"])
    bias_setup(nc, rb, C["OH1"], C["J"], BVh, BIASD)
    bounds = [0]
    while bounds[-1] < NQ:
        a = bounds[-1]; b = a; pairs = 0
        while b < NQ and (pairs + b + 1 <= 1100 or b == a):
            pairs += b + 1; b += 1
        bounds.append(b)
    for l in range(NL):
        SFX[0] = f"_L{l}"
        inproj_stage(nc, XTa, w_in[l], S)
        conv_stage(nc, S["QKT"], QKC, convT[l])
        mlstm_stage(nc, QKC, S["VAO"], YAT, gbias[l], ng[l], C["U"], C["ident"])
        dsa_prep(nc, S["BQT"], S["BC"], wuk[l], kvg[l], C["ident"], CKT, CKV, QLT)
        for i in range(len(bounds) - 1):
            dsa_attn(nc, f"sC2_{i}", bounds[i], bounds[i+1], S["IQT"], S["IKT"], S["IW"], CKT, CKV, QLT, BIASD, OLT, C)
        wc_stage(nc, wuvT[l], wbb[l], WC)
        merge_stage(nc, XTa, XTb, YAT, OLT, S["GAT"], S["GBT"], WC, wba[l], wout[l], lng[l, 0], lnb[l, 0], C["ones"])
        NTB = T // 1024
        if l % 2 == 0:
            ffn_stage(nc, "sE", 0, NTB, XTb, XTa, dwg[l // 2], dwu[l // 2], dwd[l // 2], lng[l, 1], lnb[l, 1], C, 1, 2816, 11)
        else:
            j = l // 2
            for tb in range(0, NTB, 2):
                ffn_stage(nc, f"sF{tb}", tb, min(tb + 2, NTB), XTb, XTa, ewg[j], ewu[j], ewd[j], lng[l, 1], lnb[l, 1], C, 8, 3584, 7, rw_d=rw[j])
        if debug:
            dump_stage(nc, "sdbg", XTa, dbg[l])
    SFX[0] = "_fin"
    transpose_out_stage(nc, XTa, y, C["ident"])
    return nc


def host_inputs(inputs, NL):
    f = lambda a: np.ascontiguousarray(np.asarray(a, dtype=np.float32))
    ND = (NL + 1) // 2; NM = NL // 2
    colz = lambda v: f(np.asarray(v)[:NL].reshape(NL, 2, 8, 128).transpose(0, 1, 3, 2))
    d = dict(
        w_in=f(inputs["w_in"][:NL]), convT=f(np.asarray(inputs["mlstm_conv_w"])[:NL].transpose(0, 2, 1)),
        gbias=f(np.asarray(inputs["mlstm_gate_bias"])[:NL].reshape(NL, 1, 8)), ng=f(np.asarray(inputs["mlstm_norm_g"])[:NL].reshape(NL, 1, 512)),
        kvg=f(np.asarray(inputs["dsa_kv_norm_g"])[:NL].reshape(NL, 1, 128)), wuk=f(inputs["dsa_w_uk"][:NL]),
        wuvT=f(np.asarray(inputs["dsa_w_uv"])[:NL].transpose(0, 1, 3, 2)), rb=f(inputs["rel_bias"]),
        wba=f(inputs["w_branch_a"][:NL]), wbb=f(inputs["w_branch_b"][:NL]), wout=f(inputs["w_out"][:NL]),
        lng=colz(inputs["ln_g"]), lnb=colz(inputs["ln_b"]),
        dwg=f(np.asarray(inputs["dense_w_gate"])[:ND, None]), dwu=f(np.asarray(inputs["dense_w_up"])[:ND, None]), dwd=f(np.asarray(inputs["dense_w_down"])[:ND, None]),
    )
    if NM:
        d.update(rw=f(inputs["router_w"][:NM]), ewg=f(inputs["expert_w_gate"][:NM]), ewu=f(inputs["expert_w_up"][:NM]), ewd=f(inputs["expert_w_down"][:NM]))
    hc = host_consts()
    hc["ones"] = np.ones((128, 128), np.float32); hc["U"] = np.triu(np.ones((64, 64), np.float32))
    sel = np.zeros((8, 8, 128), np.float32)
    for e in range(8): sel[e, e, :] = 1.0
    hc["SEL"] = sel
    d.update(hc)
    return d


def kernel(**inputs):
    x = np.asarray(inputs["x"], dtype=np.float32)
    n = x.shape[0]
    NL = 4
    shared = host_inputs(inputs, NL)
    nc = build_full(NL, debug=False)
    in_maps = [dict(shared, x=np.ascontiguousarray(x[c])) for c in range(n)]
    res = run_bass_kernel_spmd(nc, in_maps, core_ids=list(range(n)))
    return np.stack([np.asarray(res.results[c]["y"], dtype=np.float32) for c in range(n)], axis=0)
```

```python
import numpy as np
from contextlib import ExitStack
import concourse.bass as bass
import concourse.mybir as mybir
from concourse.bass_utils import run_bass_kernel_spmd

F32, BF16 = mybir.dt.float32, mybir.dt.bfloat16
ALU = mybir.AluOpType
AF = mybir.ActivationFunctionType
AX = mybir.AxisListType
ENGS = ("pe", "act", "dve", "pool", "sp")
SFX = [""]
SCOUNT = [0]
GLOBAL = {}


def _merge(d, s):
    for k, v in s.items():
        if d.get(k, 0) < v:
            d[k] = v


class Buf:
    def __init__(self, t, multi=False):
        self.t = t
        self.multi = multi
        self.w = {}
        self.r = {}
        self.dsem = None

    def __getitem__(self, k):
        return self.t[k]


class Stage:
    def __init__(self, nc, name):
        self.nc = nc
        name = name + SFX[0]
        self.name = name
        self.es = ExitStack()
        self.ops = {e: [] for e in ENGS}
        g = GLOBAL.get(id(nc))
        if g is None:
            ges = ExitStack()
            g = dict(es=ges, sem={e: ges.enter_context(nc.semaphore(f"g_{e}")) for e in ENGS}, cnt={e: 0 for e in ENGS}, pool=[])
            GLOBAL[id(nc)] = g
        self.g = g
        self.sem = g["sem"]
        self.cnt = dict(g["cnt"])
        self.dsems = {}
        self.ndsem = 0
        self.bufs = []
        self.semkey = {}

    def sb(self, name, shape, dt):
        t = self.es.enter_context(self.nc.sbuf_tensor(f"{self.name}_{name}", list(shape), dt))
        return self.track(Buf(t))

    def ps(self, name, shape=(128, 512), dt=F32):
        t = self.es.enter_context(self.nc.psum_tensor(f"{self.name}_{name}", list(shape), dt))
        return self.track(Buf(t))

    def track(self, b):
        b.w = {}
        b.r = {}
        b.dsem = None
        self.bufs.append(b)
        return b

    def _deps(self, reads, writes):
        deps = {}
        for b in reads:
            _merge(deps, b.w)
        for b in writes:
            _merge(deps, b.r)
            if not b.multi:
                _merge(deps, b.w)
        return deps

    def _commit(self, tok, reads, writes):
        for b in reads:
            _merge(b.r, tok)
        for b in writes:
            if b.multi:
                _merge(b.w, tok)
            else:
                b.w = dict(tok)
                b.r = {}

    def op(self, eng, fn, reads=(), writes=()):
        deps = self._deps(reads, writes)
        self.cnt[eng] += 1
        s = self.sem[eng]
        tok = {id(s): self.cnt[eng]}
        self.semkey[id(s)] = s
        self.ops[eng].append((deps, fn, s, 1))
        self._commit(tok, reads, writes)

    def dma(self, eng, out, in_, reads=(), writes=()):
        deps = self._deps(reads, writes)
        b = writes[0]
        if b.dsem is None:
            pool = self.g["pool"]
            if self.ndsem >= len(pool):
                pool.append([self.g["es"].enter_context(self.nc.semaphore(f"g_d{len(pool)}")), 0])
            b.dsem = pool[self.ndsem][0]
            self.dsems[id(b.dsem)] = pool[self.ndsem][1]
            self.semkey[id(b.dsem)] = b.dsem
            self.ndsem += 1
        self.dsems[id(b.dsem)] += 16
        tok = {id(b.dsem): self.dsems[id(b.dsem)]}
        self.ops[eng].append((deps, lambda e, o=out, i=in_: e.dma_start(out=o, in_=i), b.dsem, 16))
        self._commit(tok, reads, writes)

    def run(self):
        nc = self.nc
        final = {}
        for e in ENGS:
            if self.cnt[e] > self.g["cnt"][e]:
                final[id(self.sem[e])] = self.cnt[e]
                self.semkey[id(self.sem[e])] = self.sem[e]
        final.update({k: v for k, v in self.dsems.items()})

        def emit(engname, eng):
            waited = {}
            own = id(self.sem[engname])
            for deps, fn, s, n in self.ops[engname]:
                for k, v in deps.items():
                    if engname == "pe" and k == own:
                        continue
                    if waited.get(k, 0) < v:
                        eng.wait_ge(self.semkey[k], v)
                        waited[k] = v
                fn(eng).then_inc(s, n)
            for k, v in final.items():
                if waited.get(k, 0) < v:
                    eng.wait_ge(self.semkey[k], v)

        with nc.Block() as block:
            @block.tensor
            def _(e):
                emit("pe", e)

            @block.scalar
            def _(e):
                emit("act", e)

            @block.vector
            def _(e):
                emit("dve", e)

            @block.gpsimd
            def _(e):
                emit("pool", e)

            @block.sync
            def _(e):
                emit("sp", e)
        for e in ENGS:
            self.g["cnt"][e] = self.cnt[e]
        for p in self.g["pool"]:
            if id(p[0]) in self.dsems:
                p[1] = self.dsems[id(p[0])]
        for b in self.bufs:
            b.w = {}
            b.r = {}
            b.dsem = None
        self.es.close()

import math, os
T = 8192

def conv_stage(nc, QKT, QKC, convT_d):
    st = Stage(nc, "sB1"); st.track(QKT); st.track(QKC)
    cw = st.sb("cw", [128, 4, 4], F32)
    st.dma("sp", cw[:], convT_d.rearrange("(k p) j -> p k j", p=128), writes=[cw])
    win = [st.sb(f"win{i}", [128, 4, 515], F32) for i in range(2)]
    acc = [st.sb(f"acc{i}", [128, 4, 512], F32) for i in range(2)]
    out = [st.sb(f"out{i}", [128, 4, 512], F32) for i in range(2)]
    for tt in range(T // 512):
        w = win[tt % 2]; a = acc[tt % 2]; o = out[tt % 2]
        if tt == 0:
            st.op("pool", lambda e, w=w: e.memset(w[:, :, 0:3], 0.0), writes=[w])
            st.dma("sp", w[:, :, 3:515], QKT[:, 0:512].rearrange("(k p) t -> p k t", p=128), reads=[QKT], writes=[w])
        else:
            st.dma("sp", w[:], QKT[:, tt*512-3:(tt+1)*512].rearrange("(k p) t -> p k t", p=128), reads=[QKT], writes=[w])
        for k in range(4):
            eng = "dve"
            st.op(eng, lambda e, w=w, a=a, k=k: e.tensor_scalar(a[:, k, :], w[:, k, 0:512], cw[:, k, 0:1], None, ALU.mult), reads=[w, cw], writes=[a])
            for j in range(1, 4):
                st.op(eng, lambda e, w=w, a=a, k=k, j=j: e.scalar_tensor_tensor(a[:, k, :], w[:, k, j:j+512], cw[:, k, j:j+1], a[:, k, :], ALU.mult, ALU.add), reads=[w, cw, a], writes=[a])
        st.op("act", lambda e, a=a, o=o: e.activation(o[:], a[:], AF.Silu), reads=[a], writes=[o])
        st.op("dve", lambda e, o=o: e.tensor_scalar(o[:, 2:4, :], o[:, 2:4, :], 0.125, None, ALU.mult), reads=[o], writes=[o])
        st.dma("sp", QKC[:, tt*512:(tt+1)*512].rearrange("(k p) t -> p k t", p=128), o[:], reads=[o], writes=[QKC])
    st.run()

def layer_norm(st, r, ones, gcol, bcol, out, pss, tmp):
    sq, mean, rstd = tmp["sq"], tmp["mean"], tmp["rstd"]
    ps_s, ps_q = pss
    st.op("act", lambda e: e.activation(sq[:], r[:], AF.Square), reads=[r], writes=[sq])
    for k in range(8):
        st.op("pe", lambda e, k=k: e.matmul(ps_s[:], ones[:], r[:, k, :], start=(k == 0), stop=(k == 7)), reads=[ones, r], writes=[ps_s])
    for k in range(8):
        st.op("pe", lambda e, k=k: e.matmul(ps_q[:], ones[:], sq[:, k, :], start=(k == 0), stop=(k == 7)), reads=[ones, sq], writes=[ps_q])
    st.op("dve", lambda e: e.tensor_scalar(mean[:], ps_s[:], 1.0 / 1024, None, ALU.mult), reads=[ps_s], writes=[mean])
    st.op("dve", lambda e: e.tensor_tensor(rstd[:], mean[:], mean[:], ALU.mult), reads=[mean], writes=[rstd])
    st.op("dve", lambda e: e.scalar_tensor_tensor(rstd[:], ps_q[:], 1.0 / 1024, rstd[:], ALU.mult, ALU.subtract), reads=[ps_q, rstd], writes=[rstd])
    st.op("dve", lambda e: e.tensor_scalar(rstd[:], rstd[:], 1e-5, None, ALU.add), reads=[rstd], writes=[rstd])
    st.op("act", lambda e: e.activation(rstd[:], rstd[:], AF.Ln), reads=[rstd], writes=[rstd])
    st.op("act", lambda e: e.activation(rstd[:], rstd[:], AF.Exp, scale=-0.5), reads=[rstd], writes=[rstd])
    st.op("dve", lambda e: e.tensor_tensor(sq[:], r[:], mean[:].unsqueeze(1).to_broadcast([128, 8, 512]), ALU.subtract), reads=[r, mean], writes=[sq])
    st.op("pool", lambda e: e.tensor_tensor(sq[:], sq[:], rstd[:].unsqueeze(1).to_broadcast([128, 8, 512]), ALU.mult), reads=[sq, rstd], writes=[sq])
    for k in range(8):
        eng = "dve" if k % 2 == 0 else "pool"
        st.op(eng, lambda e, k=k: e.tensor_scalar(out[:, k, :], sq[:, k, :], gcol[:, k:k+1], bcol[:, k:k+1], ALU.mult, ALU.add), reads=[sq, gcol, bcol], writes=[out])

def mlstm_stage(nc, QKC, VAO, YAT, gb_d, ng_d, U_d, ident_d):
    st = Stage(nc, "sB2"); st.track(QKC); st.track(VAO); st.track(YAT)
    U = st.sb("U", [64, 64], F32); st.dma("sp", U[:], U_d[:, :], writes=[U])
    ident = st.sb("ident", [128, 128], F32); st.dma("sp", ident[:], ident_d[:, :], writes=[ident])
    gb = st.sb("gb", [64, 8], F32); st.dma("sp", gb[:], gb_d.partition_broadcast(64), writes=[gb])
    ng = st.sb("ng", [64, 512], F32); st.dma("sp", ng[:], ng_d.partition_broadcast(64), writes=[ng])
    ones64 = st.sb("ones64", [64, 64], F32); st.op("pool", lambda e: e.memset(ones64[:], 1.0), writes=[ones64])
    maskb = st.sb("maskb", [64, 4, 64], F32)
    st.op("dve", lambda e: e.tensor_copy(maskb[:], U[:].unsqueeze(1).to_broadcast([64, 4, 64])), reads=[U], writes=[maskb])
    C = st.sb("C", [64, 4, 129], F32); st.op("pool", lambda e: e.memset(C[:], 0.0), writes=[C])
    q4s = [st.sb(f"q4{i}", [64, 8, 512], F32) for i in range(2)]
    vas = [st.sb(f"va{i}", [64, 8, 1032], F32) for i in range(2)]
    yos = [st.sb(f"yo{i}", [128, 4, 512], F32) for i in range(2)]
    def mk(i):
        d = {}
        for n, s in dict(g=[64, 8], e1=[64, 4], nl=[64, 4], nlb=[64, 4, 64], col=[64, 4], Dt=[64, 4, 64], Ebc=[64, 4, 64], wtmp=[64, 4], wcol=[64, 4],
                         dec=[64, 4], nbt=[64, 8], Wt=[64, 4, 64], qt=[64, 4, 64], kt=[64, 4, 64], vaug=[64, 4, 129], den=[64, 4], rden=[64, 4], hn=[64, 4, 128],
                         sm=[64, 4], xc=[64, 4, 128], sq=[64, 4, 128], ssq=[64, 4], rstd=[64, 4], sig=[64, 512], gs=[64, 512], y=[64, 512]).items():
            d[n] = st.sb(f"{n}{i}", s, F32)
        st.op("pool", lambda e, v=d["vaug"]: e.memset(v[:], 1.0), writes=[d["vaug"]])
        return d
    tmps = [mk(0), mk(1)]
    psA = st.ps("psA", [64, 512]); psB = st.ps("psB", [64, 512])
    pSK = st.ps("pSK", [64, 512])
    psN = [st.ps(f"psN{j}", [64, 512]) for j in range(2)]; psU = [st.ps(f"psU{j}", [64, 512]) for j in range(2)]
    psY = st.ps("psY", [128, 512])
    for tt in range(T // 512):
        q4 = q4s[tt % 2]; va = vas[tt % 2]; yo = yos[tt % 2]
        st.dma("sp", q4[:], QKC[:, tt*512:(tt+1)*512].rearrange("(g d) t -> d g t", d=64), reads=[QKC], writes=[q4])
        st.dma("sp", va[:], VAO[tt*512:(tt+1)*512, :].rearrange("(c p) n -> p c n", p=64), reads=[VAO], writes=[va])
        for c in range(8):
            d = tmps[c % 2]; cs = slice(c*64, (c+1)*64)
            g, e1, nl, nlb, col, Dt, Ebc, wtmp, wcol, dec, Wt, qt, kt, vaug, den, rden, hn, sm, xc, sq, ssq, rstd, sig, gs, y = [d[n] for n in
                ("g", "e1", "nl", "nlb", "col", "Dt", "Ebc", "wtmp", "wcol", "dec", "Wt", "qt", "kt", "vaug", "den", "rden", "hn", "sm", "xc", "sq", "ssq", "rstd", "sig", "gs", "y")]
            st.op("dve", lambda e, g=g, va=va, c=c: e.tensor_tensor(g[:], va[:, c, 1024:1032], gb[:], ALU.add), reads=[va, gb], writes=[g])
            st.op("act", lambda e, g=g, e1=e1: e.activation(e1[:], g[:, 4:8], AF.Exp, scale=-1.0), reads=[g], writes=[e1])
            st.op("dve", lambda e, e1=e1: e.tensor_scalar(e1[:], e1[:], 1.0, None, ALU.add), reads=[e1], writes=[e1])
            st.op("act", lambda e, nl=nl, e1=e1: e.activation(nl[:], e1[:], AF.Ln), reads=[e1], writes=[nl])
            st.op("dve", lambda e, nl=nl, nlb=nlb: e.tensor_copy(nlb[:], nl[:].unsqueeze(2).to_broadcast([64, 4, 64])), reads=[nl], writes=[nlb])
            st.op("pe", lambda e, nl=nl: e.matmul(psA[:, 0:4], U[:], nl[:], start=True, stop=True), reads=[U, nl], writes=[psA])
            st.op("pe", lambda e, nl=nl: e.matmul(psA[:, 4:8], ones64[:], nl[:], start=True, stop=True), reads=[ones64, nl], writes=[psA])
            for h in range(4):
                st.op("pe", lambda e, nlb=nlb, h=h: e.matmul(psB[:, h*64:(h+1)*64], nlb[:, h, :], U[:], start=True, stop=True), reads=[nlb, U], writes=[psB])
            nbt = d["nbt"]
            st.op("act", lambda e, nbt=nbt: e.copy(nbt[:], psA[:, 0:8]), reads=[psA], writes=[nbt])
            st.op("dve", lambda e, col=col, g=g, nbt=nbt: e.tensor_tensor(col[:], nbt[:, 0:4], g[:, 0:4], ALU.add), reads=[nbt, g], writes=[col])
            for h in range(4):
                st.op("act", lambda e, Dt=Dt, col=col, h=h: e.activation(Dt[:, h, :], psB[:, h*64:(h+1)*64], AF.Exp, bias=col[:, h:h+1], scale=-1.0), reads=[psB, col], writes=[Dt])
            st.op("act", lambda e, Ebc=Ebc: e.activation(Ebc[:].rearrange("p h t -> p (h t)"), psB[:, 0:256], AF.Exp, scale=-1.0), reads=[psB], writes=[Ebc])
            st.op("dve", lambda e, wtmp=wtmp, col=col, nbt=nbt: e.tensor_tensor(wtmp[:], col[:], nbt[:, 4:8], ALU.subtract), reads=[col, nbt], writes=[wtmp])
            st.op("act", lambda e, wcol=wcol, wtmp=wtmp: e.activation(wcol[:], wtmp[:], AF.Exp), reads=[wtmp], writes=[wcol])
            st.op("act", lambda e, dec=dec, nbt=nbt: e.activation(dec[:], nbt[:, 4:8], AF.Exp, scale=-1.0), reads=[nbt], writes=[dec])
            for h in range(4):
                st.op("pe", lambda e, q4=q4, h=h, cs=cs: e.matmul(pSK[:, h*64:(h+1)*64], q4[:, 4+h, cs], q4[:, h, cs], start=True, stop=True), reads=[q4], writes=[pSK])
            st.op("dve", lambda e, Wt=Wt, Dt=Dt: e.tensor_tensor(Wt[:], Dt[:], maskb[:], ALU.mult), reads=[Dt, maskb], writes=[Wt])
            st.op("dve", lambda e, Wt=Wt: e.tensor_tensor(Wt[:], Wt[:], pSK[:, 0:256].rearrange("p (h t) -> p h t", h=4), ALU.mult), reads=[Wt, pSK], writes=[Wt])
            st.op("pool", lambda e, qt=qt, q4=q4, Ebc=Ebc, cs=cs: e.tensor_tensor(qt[:], q4[:, 0:4, cs], Ebc[:], ALU.mult), reads=[q4, Ebc], writes=[qt])
            for h in range(4):
                st.op("pe", lambda e, q4=q4, h=h, cs=cs: e.transpose(pSK[:, 256+h*64:256+(h+1)*64], q4[:, 4+h, cs], ident[0:64, 0:64]), reads=[q4, ident], writes=[pSK])
            st.op("dve", lambda e, kt=kt, wcol=wcol: e.tensor_tensor(kt[:], pSK[:, 256:512].rearrange("p (h t) -> p h t", h=4), wcol[:].unsqueeze(2).to_broadcast([64, 4, 64]), ALU.mult), reads=[pSK, wcol], writes=[kt])
            st.op("pool", lambda e, vaug=vaug, va=va, c=c: e.tensor_copy(vaug[:, :, 0:128], va[:, c, 0:512].rearrange("p (h v) -> p h v", h=4)), reads=[va], writes=[vaug])
            for h in range(4):
                pn = psN[h // 2]; o = (h % 2) * 129
                st.op("pe", lambda e, pn=pn, o=o, Wt=Wt, vaug=vaug, h=h: e.matmul(pn[:, o:o+129], Wt[:, h, :], vaug[:, h, :], start=True, stop=False), reads=[Wt, vaug], writes=[pn])
                st.op("pe", lambda e, pn=pn, o=o, qt=qt, h=h: e.matmul(pn[:, o:o+129], qt[:, h, :], C[:, h, :], start=False, stop=True), reads=[qt, C], writes=[pn])
            for h in range(4):
                pu = psU[h // 2]; o = (h % 2) * 129
                st.op("pe", lambda e, pu=pu, o=o, kt=kt, vaug=vaug, h=h: e.matmul(pu[:, o:o+129], kt[:, h, :], vaug[:, h, :], start=True, stop=True), reads=[kt, vaug], writes=[pu])
            for h in range(4):
                pu = psU[h // 2]; o = (h % 2) * 129
                st.op("dve", lambda e, pu=pu, o=o, dec=dec, h=h: e.scalar_tensor_tensor(C[:, h, :], C[:, h, :], dec[:, h:h+1], pu[:, o:o+129], ALU.mult, ALU.add), reads=[C, dec, pu], writes=[C])
            for j in range(2):
                pv = psN[j]
                st.op("act", lambda e, pv=pv, den=den, j=j: e.activation(den[:, 2*j:2*j+2], pv[:, 0:258].rearrange("p (i n) -> p i n", n=129)[:, :, 128], AF.Abs), reads=[pv], writes=[den])
            st.op("dve", lambda e, den=den: e.tensor_scalar(den[:], den[:], 1.0, None, ALU.max), reads=[den], writes=[den])
            st.op("dve", lambda e, den=den, rden=rden: e.reciprocal(rden[:], den[:]), reads=[den], writes=[rden])
            for j in range(2):
                pv = psN[j]
                st.op("dve", lambda e, pv=pv, hn=hn, rden=rden, j=j: e.tensor_tensor(hn[:, 2*j:2*j+2, :], pv[:, 0:258].rearrange("p (i n) -> p i n", n=129)[:, :, 0:128], rden[:, 2*j:2*j+2].unsqueeze(2).to_broadcast([64, 2, 128]), ALU.mult), reads=[pv, rden], writes=[hn])
            st.op("dve", lambda e, sm=sm, hn=hn: e.tensor_reduce(sm[:], hn[:], AX.X, ALU.add), reads=[hn], writes=[sm])
            st.op("dve", lambda e, sm=sm: e.tensor_scalar(sm[:], sm[:], -1.0 / 128, None, ALU.mult), reads=[sm], writes=[sm])
            st.op("dve", lambda e, xc=xc, hn=hn, sm=sm: e.tensor_tensor(xc[:], hn[:], sm[:].unsqueeze(2).to_broadcast([64, 4, 128]), ALU.add), reads=[hn, sm], writes=[xc])
            st.op("pool", lambda e, sq=sq, xc=xc: e.tensor_tensor(sq[:], xc[:], xc[:], ALU.mult), reads=[xc], writes=[sq])
            st.op("dve", lambda e, ssq=ssq, sq=sq: e.tensor_reduce(ssq[:], sq[:], AX.X, ALU.add), reads=[sq], writes=[ssq])
            st.op("dve", lambda e, ssq=ssq: e.tensor_scalar(ssq[:], ssq[:], 1.0 / 128, 1e-5, ALU.mult, ALU.add), reads=[ssq], writes=[ssq])
            st.op("act", lambda e, ssq=ssq, rstd=rstd: e.activation(rstd[:], ssq[:], AF.Ln), reads=[ssq], writes=[rstd])
            st.op("act", lambda e, rstd=rstd: e.activation(rstd[:], rstd[:], AF.Exp, scale=-0.5), reads=[rstd], writes=[rstd])
            st.op("act", lambda e, sig=sig, va=va, c=c: e.activation(sig[:], va[:, c, 512:1024], AF.Sigmoid), reads=[va], writes=[sig])
            st.op("pool", lambda e, gs=gs, sig=sig: e.tensor_tensor(gs[:], sig[:], ng[:], ALU.mult), reads=[sig, ng], writes=[gs])
            st.op("dve", lambda e, y=y, xc=xc, rstd=rstd: e.tensor_tensor(y[:].rearrange("p (h v) -> p h v", h=4), xc[:], rstd[:].unsqueeze(2).to_broadcast([64, 4, 128]), ALU.mult), reads=[xc, rstd], writes=[y])
            st.op("pool", lambda e, y=y, gs=gs: e.tensor_tensor(y[:], y[:], gs[:], ALU.mult), reads=[y, gs], writes=[y])
            for h in range(4):
                st.op("pe", lambda e, y=y, h=h: e.transpose(psY[:, h*64:(h+1)*64], y[:, h*128:(h+1)*128], ident[0:64, 0:64]), reads=[y, ident], writes=[psY])
            st.op("act", lambda e, yo=yo, cs=cs: e.copy(yo[:, :, cs], psY[:, 0:256].rearrange("p (h t) -> p h t", h=4)), reads=[psY], writes=[yo])
        st.dma("sp", YAT[:, tt*512:(tt+1)*512].rearrange("(k p) t -> p k t", p=128), yo[:], reads=[yo], writes=[YAT])
    st.run()

def t5_bucket_np(d):
    d = np.maximum(d, 0)
    lr = np.log(np.maximum(d, 1).astype(np.float32) / np.float32(16)) / np.float32(math.log(8.0))
    large = np.minimum(16 + (lr * 16).astype(np.int32), 31)
    return np.where(d < 16, d, large)

def host_consts():
    j = np.arange(512); d = j - 127
    oh = np.zeros((32, 512), np.float32)
    b = t5_bucket_np(d)
    for jj in range(511):
        if d[jj] >= 0: oh[b[jj], jj] = 1.0
    J = np.eye(128, dtype=np.float32)[::-1].copy()
    negtri = np.where(np.arange(128)[None, :] <= np.arange(128)[:, None], 0.0, -1e30).astype(np.float32)
    pow2 = np.tile((2.0 ** -np.arange(NIT)).astype(np.float32)[None, :], (128, 1))
    irep = np.tile(np.eye(128, dtype=np.float32), (1, 4))
    return dict(OH1=oh, J=J, NEGTRI=negtri, POW2=pow2, IREP=irep, ident=np.eye(128, dtype=np.float32))

def bias_setup(nc, rb_d, OH1_d, J_d, BVh, BIASD):
    st = Stage(nc, "sbias")
    BV = st.track(Buf(BVh.ap(), multi=True)); BD = st.track(Buf(BIASD, multi=True))
    rb = st.sb("rb", [32, 8], F32); st.dma("sp", rb[:], rb_d[:, :], writes=[rb])
    oh = st.sb("oh", [32, 512], F32); st.dma("sp", oh[:], OH1_d[:, :], writes=[oh])
    J = st.sb("J", [128, 128], F32); st.dma("sp", J[:], J_d[:, :], writes=[J])
    ps = st.ps("ps"); bv = st.sb("bv", [8, 512], F32)
    st.op("pe", lambda e: e.matmul(ps[0:8, :], rb[:], oh[:], start=True, stop=True), reads=[rb, oh], writes=[ps])
    st.op("dve", lambda e: e.tensor_scalar(bv[:], ps[0:8, :], 8.0, None, ALU.mult), reads=[ps], writes=[bv])
    st.dma("sp", BV[:, :], bv[:], reads=[bv], writes=[BV])
    hk = st.sb("hk", [128, 8, 128], F32); hi = st.sb("hi", [128, 2, 1024], BF16); tmp = st.sb("tmp", [128, 512], F32)
    ps2 = st.ps("ps2")
    for kind in range(3):
        st.dma("sp", hk[:], bass.AP(BVh, 128 * kind, [[1, 128], [512, 8], [1, 128]]), reads=[BV], writes=[hk])
        for half in range(2):
            st.op("pe", lambda e, half=half: e.matmul(ps2[:], J[:], hk[:, half*4:(half+1)*4, :].rearrange("p h t -> p (h t)"), start=True, stop=True), reads=[J, hk], writes=[ps2])
            st.op("act", lambda e, half=half: e.copy(hi[:, 0, half*512:(half+1)*512], ps2[:]), reads=[ps2], writes=[hi])
            st.op("dve", lambda e, half=half: e.tensor_tensor(tmp[:], ps2[:], hi[:, 0, half*512:(half+1)*512], ALU.subtract), reads=[ps2, hi], writes=[tmp])
            st.op("dve", lambda e, half=half: e.tensor_copy(hi[:, 1, half*512:(half+1)*512], tmp[:]), reads=[tmp], writes=[hi])
        st.dma("sp", BD[kind], hi[:], reads=[hi], writes=[BD])
    st.run()

def dsa_prep(nc, BQT, BC, wuk_d, kvg_d, ident_d, CKT, CKV, QLT):
    st = Stage(nc, "sC1")
    for b in (BQT, BC, CKT, CKV, QLT): st.track(b)
    wuk = st.sb("wuk", [64, 8, 128], BF16); st.dma("pool", wuk[:], wuk_d.rearrange("h d c -> d h c"), writes=[wuk])
    kvg = st.sb("kvg", [128, 128], F32); st.dma("sp", kvg[:], kvg_d.partition_broadcast(128), writes=[kvg])
    ident = st.sb("ident", [128, 128], F32); st.dma("sp", ident[:], ident_d[:, :], writes=[ident])
    bqs = [st.sb(f"bq{i}", [64, 8, 512], F32) for i in range(2)]; bqb = [st.sb(f"bqb{i}", [64, 8, 512], BF16) for i in range(2)]
    qls = [st.sb(f"ql{i}", [128, 8, 512], BF16) for i in range(2)]
    bcs = [st.sb(f"bc{i}", [128, 4, 128], F32) for i in range(2)]
    sq = st.sb("sq", [128, 4, 128], F32); ssq = st.sb("ssq", [128, 4], F32); rstd = st.sb("rstd", [128, 4], F32)
    ckv = st.sb("ckv", [128, 4, 128], F32)
    ckvb = [st.sb(f"ckvb{i}", [128, 4, 129], BF16) for i in range(2)]
    for c in ckvb: st.op("pool", lambda e, c=c: e.memset(c[:], 1.0), writes=[c])
    ckt = [st.sb(f"ckt{i}", [128, 512], BF16) for i in range(2)]
    pss = [st.ps(f"ps{i}") for i in range(4)]; pst = st.ps("pst")
    pi = 0
    for tt in range(T // 512):
        bq = bqs[tt % 2]; bb = bqb[tt % 2]; ql = qls[tt % 2]; bc = bcs[tt % 2]; cb = ckvb[tt % 2]; ct = ckt[tt % 2]
        st.dma("sp", bq[:], BQT[:, tt*512:(tt+1)*512].rearrange("(g d) t -> d g t", d=64), reads=[BQT], writes=[bq])
        st.op("pool", lambda e, bq=bq, bb=bb: e.tensor_copy(bb[:], bq[:]), reads=[bq], writes=[bb])
        for h in range(8):
            ps = pss[pi % 4]; pi += 1
            st.op("pe", lambda e, ps=ps, h=h, bb=bb: e.matmul(ps[:], wuk[:, h, :], bb[:, h, :], start=True, stop=True), reads=[wuk, bb], writes=[ps])
            if h % 2 == 0:
                st.op("act", lambda e, ps=ps, h=h, ql=ql: e.copy(ql[:, h, :], ps[:]), reads=[ps], writes=[ql])
            else:
                st.op("dve", lambda e, ps=ps, h=h, ql=ql: e.tensor_copy(ql[:, h, :], ps[:]), reads=[ps], writes=[ql])
        for q in range(4):
            st.dma("sp", QLT[:, tt*4+q, :, :], ql[:, :, q*128:(q+1)*128], reads=[ql], writes=[QLT])
        st.dma("sp", bc[:], BC[tt*512:(tt+1)*512, :].rearrange("(s p) c -> p s c", p=128), reads=[BC], writes=[bc])
        st.op("pool", lambda e, bc=bc: e.tensor_tensor(sq[:], bc[:], bc[:], ALU.mult), reads=[bc], writes=[sq])
        st.op("dve", lambda e: e.tensor_reduce(ssq[:], sq[:], AX.X, ALU.add), reads=[sq], writes=[ssq])
        st.op("dve", lambda e: e.tensor_scalar(ssq[:], ssq[:], 1.0 / 128, 1e-5, ALU.mult, ALU.add), reads=[ssq], writes=[ssq])
        st.op("act", lambda e: e.activation(rstd[:], ssq[:], AF.Ln), reads=[ssq], writes=[rstd])
        st.op("act", lambda e: e.activation(rstd[:], rstd[:], AF.Exp, scale=-0.5), reads=[rstd], writes=[rstd])
        st.op("dve", lambda e, bc=bc: e.tensor_tensor(ckv[:], bc[:], rstd[:].unsqueeze(2).to_broadcast([128, 4, 128]), ALU.mult), reads=[bc, rstd], writes=[ckv])
        st.op("pool", lambda e: e.tensor_tensor(ckv[:], ckv[:], kvg[:].unsqueeze(1).to_broadcast([128, 4, 128]), ALU.mult), reads=[ckv, kvg], writes=[ckv])
        st.op("pool", lambda e, cb=cb: e.tensor_copy(cb[:, :, 0:128], ckv[:]), reads=[ckv], writes=[cb])
        st.dma("sp", CKV[tt*512:(tt+1)*512, :].rearrange("(s p) c -> p s c", p=128), cb[:], reads=[cb], writes=[CKV])
        for s in range(4):
            st.op("pe", lambda e, s=s: e.transpose(pst[:, s*128:(s+1)*128], ckv[:, s, :], ident[:]), reads=[ckv, ident], writes=[pst])
        st.op("act", lambda e, ct=ct: e.copy(ct[:], pst[:]), reads=[pst], writes=[ct])
        st.dma("sp", CKT[:, tt*512:(tt+1)*512], ct[:], reads=[ct], writes=[CKT])
    st.run()

def dsa_attn(nc, name, q0, q1, IQT, IKT, IW, CKT, CKV, QLT, BIASD, OLT, C):
    st = Stage(nc, name)
    for b in (IQT, IKT, IW, CKT, CKV, QLT, OLT): st.track(b)
    BD = st.track(Buf(BIASD, multi=True))
    nkmax = q1 * 128
    cktS = st.sb("ckt", [128, nkmax], BF16); st.dma("sp", cktS[:], CKT[:, 0:nkmax], reads=[CKT], writes=[cktS])
    ckvS = st.sb("ckv", [128, q1, 129], BF16)
    for k0 in range(0, q1, 8):
        k1 = min(q1, k0 + 8)
        st.dma("sp", ckvS[:, k0:k1, :], CKV[k0*128:k1*128, :].rearrange("(k p) c -> p k c", p=128), reads=[CKV], writes=[ckvS])
    kiT = st.sb("kiT", [32, nkmax], F32); st.dma("sp", kiT[:], IKT[:, 0:nkmax], reads=[IKT], writes=[kiT])
    bias = st.sb("bias", [128, 3, 2, 1024], BF16)
    for kind in range(3): st.dma("sp", bias[:, kind], BD[kind], reads=[BD], writes=[bias])
    identb = st.sb("identb", [128, 128], BF16); st.dma("pool", identb[:], C["ident"][:, :], writes=[identb])
    ident = st.sb("ident", [128, 128], F32); st.dma("sp", ident[:], C["ident"][:, :], writes=[ident])
    irep = st.sb("irep", [128, 512], BF16); st.dma("pool", irep[:], C["IREP"][:, :], writes=[irep])
    negtri = st.sb("negtri", [128, 128], F32); st.dma("sp", negtri[:], C["NEGTRI"][:, :], writes=[negtri])
    pow2 = st.sb("pow2", [128, NIT], F32); st.dma("sp", pow2[:], C["POW2"][:, :], writes=[pow2])
    accs = [st.sb(f"acc{i}", [128, nkmax], F32) for i in range(2)]
    NMs = [st.sb(f"NM{i}", [128, nkmax], BF16) for i in range(2)]
    qis = [st.sb(f"qi{i}", [32, 8, 128], F32) for i in range(2)]; ws = [st.sb(f"w{i}", [128, 8], F32) for i in range(2)]
    qls = [st.sb(f"ql{i}", [128, 8, 128], BF16) for i in range(3)]
    rs = [st.sb(f"r{i}", [128, 512], F32) for i in range(3)]
    PTs = [st.sb(f"PT{i}", [128, 512], BF16) for i in range(3)]
    sm = {n: st.sb(n, s, F32) for n, s in dict(den=[128, 8], rden=[128, 8]).items()}
    ol = st.sb("ol", [128, 8, 128], F32); olT = [st.sb(f"olT{i}", [128, 8, 128], BF16) for i in range(1)]
    psL = [st.ps(f"psL{i}") for i in range(3)]; psO = [st.ps(f"psO{i}") for i in range(3)]; psI = [st.ps(f"psI{i}") for i in range(2)]
    OH = [(0, 0), (0, 1), (0, 2), (1, 0), (1, 1), (1, 2), (2, 0), (2, 1)]
    sms = [{n: st.sb(f"{n}{i}", s, F32) for n, s in dict(A=[128, 1], lo=[128, 1], steps=[128, NIT], mid=[128, 1], cnt=[128, 1], ge=[128, 1]).items()} for i in range(2)]
    state = dict(li=0, ii=0)

    def score(qt):
        nk = (qt + 1) * 128; qs = slice(qt*128, (qt+1)*128)
        qi = qis[qt % 2]; w = ws[qt % 2]; ql = qls[qt % 3]; acc = accs[qt % 2]
        st.dma("sp", qi[:], IQT[:, qs].rearrange("(g d) t -> d g t", d=32), reads=[IQT], writes=[qi])
        st.dma("sp", w[:], IW[qs, :], reads=[IW], writes=[w])
        st.dma("sp", ql[:], QLT[:, qt, :, :], reads=[QLT], writes=[ql])
        st.op("pool", lambda e, w=w: e.tensor_scalar(w[:], w[:], 1.0 / 16, None, ALU.mult), reads=[w], writes=[w])
        for kb in range((nk + 511) // 512):
            n = min(512, nk - kb*512); ks = slice(kb*512, kb*512 + n)
            for h in range(8):
                ii = state["ii"]; ps = psI[ii % 2]; r = rs[ii % 3]; state["ii"] += 1
                st.op("pe", lambda e, ps=ps, qi=qi, h=h, ks=ks, n=n: e.matmul(ps[:, 0:n], qi[:, h, :], kiT[:, ks], start=True, stop=True), reads=[qi, kiT], writes=[ps])
                st.op("act", lambda e, ps=ps, r=r, n=n: e.activation(r[:, 0:n], ps[:, 0:n], AF.Relu), reads=[ps], writes=[r])
                if h == 0:
                    st.op("dve", lambda e, r=r, w=w, ks=ks, n=n, acc=acc: e.tensor_scalar(acc[:, ks], r[:, 0:n], w[:, 0:1], None, ALU.mult), reads=[r, w], writes=[acc])
                else:
                    st.op("dve", lambda e, r=r, w=w, ks=ks, n=n, h=h, acc=acc: e.scalar_tensor_tensor(acc[:, ks], r[:, 0:n], w[:, h:h+1], acc[:, ks], ALU.mult, ALU.add), reads=[r, w, acc], writes=[acc])
                yield

    def bisect(qt):
        nk = (qt + 1) * 128; acc = accs[qt % 2]; NM = NMs[qt % 2]
        A, lo, steps, mid, cnt, ge = [sms[qt % 2][n] for n in ("A", "lo", "steps", "mid", "cnt", "ge")]
        st.op("dve", lambda e: e.reduce_max(A[:], acc[:, 0:nk], AX.X, apply_absolute_value=True), reads=[acc], writes=[A])
        st.op("dve", lambda e: e.tensor_tensor(acc[:, nk-128:nk], acc[:, nk-128:nk], negtri[:], ALU.add), reads=[acc, negtri], writes=[acc])
        st.op("dve", lambda e: e.tensor_scalar(A[:], A[:], 1.0, None, ALU.add), reads=[A], writes=[A])
        st.op("dve", lambda e: e.tensor_scalar(lo[:], A[:], -1.0, None, ALU.mult), reads=[A], writes=[lo])
        st.op("dve", lambda e: e.tensor_scalar(steps[:], pow2[:], A[:, 0:1], None, ALU.mult), reads=[pow2, A], writes=[steps])
        for k in range(NIT):
            st.op("dve", lambda e, k=k: e.tensor_tensor(mid[:], lo[:], steps[:, k:k+1], ALU.add), reads=[lo, steps], writes=[mid])
            st.op("dve", lambda e: e.tensor_scalar(NM[:, 0:nk], acc[:, 0:nk], mid[:, 0:1], 0.0, ALU.is_ge, ALU.add, accum_out=cnt[:, 0:1]), reads=[acc, mid], writes=[NM, cnt])
            st.op("dve", lambda e: e.tensor_scalar(ge[:], cnt[:], 255.5, None, ALU.is_ge), reads=[cnt], writes=[ge])
            st.op("dve", lambda e, k=k: e.scalar_tensor_tensor(lo[:], ge[:], steps[:, k:k+1], lo[:], ALU.mult, ALU.add), reads=[ge, steps, lo], writes=[lo])
            yield
        st.op("dve", lambda e: e.tensor_scalar(NM[:, 0:nk], acc[:, 0:nk], lo[:, 0:1], -30000.0, ALU.is_lt, ALU.mult), reads=[acc, lo], writes=[NM])

    def attn(qt):
        qs = slice(qt*128, (qt+1)*128)
        ql = qls[qt % 3]; NM = NMs[qt % 2]; oT = olT[0]
        den, rden = sm["den"], sm["rden"]
        its = [(kt, half) for kt in range(qt + 1) for half in range(2)]
        slots = []

        def qk(i):
            kt, half = its[i]
            kind = min(qt - kt, 2); kts = slice(kt*128, (kt+1)*128)
            li = state["li"]; ps = psL[li % 3]; PT = PTs[li % 3]; state["li"] += 1
            slots.append((ps, PT))
            hs = slice(half*512, (half+1)*512)
            st.op("pe", lambda e: e.matmul(ps[:], cktS[:, kts], ql[:, half*4:(half+1)*4, :].rearrange("p h t -> p (h t)"), start=True, stop=False), reads=[cktS, ql], writes=[ps])
            st.op("pe", lambda e: e.matmul(ps[:], identb[:], bias[:, kind, 0, hs], start=False, stop=False), reads=[identb, bias], writes=[ps])
            st.op("pe", lambda e: e.matmul(ps[:], identb[:], bias[:, kind, 1, hs], start=False, stop=False), reads=[identb, bias], writes=[ps])
            st.op("pe", lambda e: e.matmul(ps[:], NM[:, kts], irep[:], start=False, stop=True), reads=[NM, irep], writes=[ps])

        def pv(i):
            kt, half = its[i]; ps, PT = slots[i]
            st.op("act", lambda e: e.activation(PT[:], ps[:], AF.Exp, scale=0.125), reads=[ps], writes=[PT])
            for hh in range(4):
                h = half*4 + hh; bnk, slot = OH[h]; po = psO[bnk]
                st.op("pe", lambda e, po=po, slot=slot, hh=hh: e.matmul(po[:, slot*129:(slot+1)*129], PT[:, hh*128:(hh+1)*128], ckvS[:, kt, :], start=(kt == 0 and slot == 0), stop=(kt == qt), skip_group_check=True),
                      reads=[PT, ckvS], writes=[po])

        qk(0)
        for i in range(len(its)):
            if i + 1 < len(its):
                qk(i + 1)
            pv(i)
            yield
        for bnk in range(3):
            nh = 3 if bnk < 2 else 2; h0 = bnk*3; po = psO[bnk]
            st.op("act", lambda e, po=po, nh=nh, h0=h0: e.copy(den[:, h0:h0+nh], po[:, 0:nh*129].rearrange("p (i n) -> p i n", n=129)[:, :, 128]), reads=[po], writes=[den])
        st.op("dve", lambda e: e.reciprocal(rden[:], den[:]), reads=[den], writes=[rden])
        for bnk in range(3):
            nh = 3 if bnk < 2 else 2; h0 = bnk*3; po = psO[bnk]
            st.op("dve", lambda e, po=po, nh=nh, h0=h0: e.tensor_tensor(ol[:, h0:h0+nh, :], po[:, 0:nh*129].rearrange("p (i n) -> p i n", n=129)[:, :, 0:128], rden[:, h0:h0+nh].unsqueeze(2).to_broadcast([128, nh, 128]), ALU.mult), reads=[po, rden], writes=[ol])
        for half in range(2):
            li = state["li"]; ps = psL[li % 3]; state["li"] += 1
            for hh in range(4):
                st.op("pe", lambda e, ps=ps, hh=hh, half=half: e.transpose(ps[:, hh*128:(hh+1)*128], ol[:, half*4+hh, :], ident[:]), reads=[ol, ident], writes=[ps])
            st.op("act", lambda e, ps=ps, oT=oT, half=half: e.copy(oT[:, half*4:(half+1)*4, :].rearrange("p h t -> p (h t)"), ps[:]), reads=[ps], writes=[oT])
        st.dma("sp", OLT[:, :, qs], oT[:], reads=[oT], writes=[OLT])

    def n_score(qt):
        return 8 * (((qt + 1) * 128 + 511) // 512)

    def drain(g):
        for _ in g:
            pass

    def interleave(streams):
        live = [[g, 0, max(n, 1)] for g, n in streams]
        while live:
            live.sort(key=lambda x: x[1] / x[2])
            cur = live[0]
            try:
                next(cur[0]); cur[1] += 1
            except StopIteration:
                live.pop(0)

    drain(score(q0))
    drain(bisect(q0))
    if q0 + 1 < q1:
        drain(score(q0 + 1))
    for qt in range(q0, q1):
        streams = [(attn(qt), 2 * (qt + 1))]
        if qt + 1 < q1:
            streams.append((bisect(qt + 1), NIT))
        if qt + 2 < q1:
            streams.append((score(qt + 2), n_score(qt + 2)))
        interleave(streams)
    st.run()


OFF = dict(a_qk=0, a_v=512, a_o=1024, a_if=1536, b_q=1544, b_c=2056, i_q=2184, i_k=2440, i_w=2472, g_a=2480, g_b=3504)
ALPHA = 8 ** 0.25
D = 1024


def transpose_in_stage(nc, x_in, XT0, ident_d):
    st = Stage(nc, "s0"); st.track(XT0)
    ident = st.sb("ident", [128, 128], F32)
    st.dma("sp", ident[:], ident_d[:, :], writes=[ident])
    xin = [st.sb(f"xin{i}", [128, 4, D], F32) for i in range(2)]
    xo = [st.sb(f"xo{i}", [128, 8, 512], F32) for i in range(2)]
    pss = [st.ps(f"ps{i}") for i in range(4)]
    pi = 0
    for tt in range(T // 512):
        xi = xin[tt % 2]; xot = xo[tt % 2]
        st.dma("sp", xi[:], x_in[tt*512:(tt+1)*512, :].rearrange("(s p) d -> p s d", p=128), writes=[xi])
        for k in range(8):
            ps = pss[pi % 4]; pi += 1
            for s in range(4):
                st.op("pe", lambda e, ps=ps, xi=xi, s=s, k=k: e.transpose(ps[:, s*128:(s+1)*128], xi[:, s, k*128:(k+1)*128], ident[:]),
                      reads=[xi, ident], writes=[ps])
            if k % 2 == 0:
                st.op("dve", lambda e, ps=ps, xot=xot, k=k: e.tensor_copy(xot[:, k, :], ps[:]), reads=[ps], writes=[xot])
            else:
                st.op("act", lambda e, ps=ps, xot=xot, k=k: e.copy(xot[:, k, :], ps[:]), reads=[ps], writes=[xot])
        st.dma("sp", XT0[:, tt*512:(tt+1)*512].rearrange("(k p) t -> p k t", p=128), xot[:], reads=[xot], writes=[XT0])
    st.run()


def transpose_out_stage(nc, XO, y_out, ident_d):
    st = Stage(nc, "s9"); st.track(XO)
    Y = st.track(Buf(y_out, multi=True))
    ident = st.sb("ident", [128, 128], F32)
    st.dma("sp", ident[:], ident_d[:, :], writes=[ident])
    xin = [st.sb(f"xin{i}", [128, 8, 512], F32) for i in range(2)]
    yo = [st.sb(f"yo{i}", [128, 4, D], F32) for i in range(2)]
    pss = [st.ps(f"ps{i}") for i in range(4)]
    pi = 0
    for tt in range(T // 512):
        xi = xin[tt % 2]; yt = yo[tt % 2]
        st.dma("sp", xi[:], XO[:, tt*512:(tt+1)*512].rearrange("(k p) t -> p k t", p=128), reads=[XO], writes=[xi])
        for s in range(4):
            for half in range(2):
                ps = pss[pi % 4]; pi += 1
                for kk in range(4):
                    k = half * 4 + kk
                    st.op("pe", lambda e, ps=ps, xi=xi, s=s, k=k, kk=kk: e.transpose(ps[:, kk*128:(kk+1)*128], xi[:, k, s*128:(s+1)*128], ident[:]),
                          reads=[xi, ident], writes=[ps])
                if half == 0:
                    st.op("dve", lambda e, ps=ps, yt=yt, s=s: e.tensor_copy(yt[:, s, 0:512], ps[:]), reads=[ps], writes=[yt])
                else:
                    st.op("act", lambda e, ps=ps, yt=yt, s=s: e.copy(yt[:, s, 512:1024], ps[:]), reads=[ps], writes=[yt])
        st.dma("sp", Y[tt*512:(tt+1)*512, :].rearrange("(s p) d -> p s d", p=128), yt[:], reads=[yt], writes=[Y])
    st.run()


def inproj_stage(nc, XT0, w_in, S):
    st = Stage(nc, "sA"); st.track(XT0)
    for b in S.values(): st.track(b)
    wb = st.sb("wb", [128, 8, 4528], BF16)
    wf = st.sb("wf", [128, 8, 296], F32)
    for k in range(8):
        st.dma("pool", wb[:, k, :], w_in[k*128:(k+1)*128, :], writes=[wb])
    st.dma("sp", wf[:], w_in[:, 2184:2480].rearrange("(k p) n -> p k n", p=128), writes=[wf])
    xf = [st.sb(f"xf{i}", [128, 8, 512], F32) for i in range(2)]
    xb = [st.sb(f"xb{i}", [128, 8, 512], BF16) for i in range(2)]
    ofm = [st.sb(f"ofm{i}", [128, 8, 512], F32) for i in range(2)]
    otm = [st.sb(f"otm{i}", [128, 4, 1032], F32) for i in range(2)]
    otb = [st.sb(f"otb{i}", [128, 4, 136], F32) for i in range(2)]
    pss = [st.ps(f"ps{i}") for i in range(6)]
    pi = [0]; oi = [0]

    def evac(ps_ap, out_ap, ps, ob, func=None):
        if func is not None or pi[0] % 2 == 1:
            st.op("act", lambda e: e.activation(out_ap, ps_ap, func if func is not None else AF.Copy), reads=[ps], writes=[ob])
        else:
            st.op("dve", lambda e: e.tensor_copy(out_ap, ps_ap), reads=[ps], writes=[ob])

    def fm_group(xt_b, wt, col0, ncols, dst, tt, func=None):
        M = min(128, ncols); nch = ncols // M
        ob = ofm[oi[0] % 2]; oi[0] += 1
        for c in range(nch):
            ps = pss[pi[0] % 6]; pi[0] += 1
            for k in range(8):
                st.op("pe", lambda e, ps=ps, c=c, k=k: e.matmul(ps[0:M, :], wt[:, k, col0+c*M:col0+(c+1)*M], xt_b[:, k, :], start=(k == 0), stop=(k == 7)),
                      reads=[wt, xt_b], writes=[ps])
            evac(ps[0:M, :], ob[0:M, c, :], ps, ob, func)
        if M == 128:
            st.dma("sp", dst[:, tt*512:(tt+1)*512].rearrange("(k p) t -> p k t", p=128), ob[:, 0:nch, :], reads=[ob], writes=[dst])
        else:
            st.dma("sp", dst[:, tt*512:(tt+1)*512], ob[0:M, 0, :], reads=[ob], writes=[dst])

    for tt in range(T // 512):
        xft = xf[tt % 2]; xbt = xb[tt % 2]
        st.dma("sp", xft[:], XT0[:, tt*512:(tt+1)*512].rearrange("(k p) t -> p k t", p=128), reads=[XT0], writes=[xft])
        st.op("pool", lambda e, xft=xft, xbt=xbt: e.tensor_copy(xbt[:], xft[:]), reads=[xft], writes=[xbt])
        fm_group(xbt, wb, OFF["a_qk"], 512, S["QKT"], tt)
        fm_group(xbt, wb, OFF["b_q"], 512, S["BQT"], tt)
        fm_group(xbt, wb, OFF["g_a"], 1024, S["GAT"], tt, func=AF.Sigmoid)
        fm_group(xbt, wb, OFF["g_b"], 1024, S["GBT"], tt, func=AF.Sigmoid)
        fm_group(xft, wf, 0, 256, S["IQT"], tt)
        fm_group(xft, wf, 256, 32, S["IKT"], tt)
        ob = otm[tt % 2]; ob2 = otb[tt % 2]
        for s in range(4):
            for (c0, n) in ((0, 512), (512, 512), (1024, 8)):
                ps = pss[pi[0] % 6]; pi[0] += 1
                for k in range(8):
                    st.op("pe", lambda e, ps=ps, s=s, k=k, c0=c0, n=n, xbt=xbt: e.matmul(ps[:, 0:n], xbt[:, k, s*128:(s+1)*128], wb[:, k, 512+c0:512+c0+n], start=(k == 0), stop=(k == 7)),
                          reads=[wb, xbt], writes=[ps])
                evac(ps[:, 0:n], ob[:, s, c0:c0+n], ps, ob)
            ps = pss[pi[0] % 6]; pi[0] += 1
            for k in range(8):
                st.op("pe", lambda e, ps=ps, s=s, k=k, xbt=xbt: e.matmul(ps[:, 0:128], xbt[:, k, s*128:(s+1)*128], wb[:, k, OFF["b_c"]:OFF["b_c"]+128], start=(k == 0), stop=(k == 7)),
                      reads=[wb, xbt], writes=[ps])
            evac(ps[:, 0:128], ob2[:, s, 0:128], ps, ob2)
            ps = pss[pi[0] % 6]; pi[0] += 1
            for k in range(8):
                st.op("pe", lambda e, ps=ps, s=s, k=k, xft=xft: e.matmul(ps[:, 0:8], xft[:, k, s*128:(s+1)*128], wf[:, k, 288:296], start=(k == 0), stop=(k == 7)),
                      reads=[wf, xft], writes=[ps])
            evac(ps[:, 0:8], ob2[:, s, 128:136], ps, ob2)
        st.dma("sp", S["VAO"][tt*512:(tt+1)*512, :].rearrange("(s p) n -> p s n", p=128), ob[:], reads=[ob], writes=[S["VAO"]])
        st.dma("sp", S["BC"][tt*512:(tt+1)*512, :].rearrange("(s p) n -> p s n", p=128), ob2[:, :, 0:128], reads=[ob2], writes=[S["BC"]])
        st.dma("sp", S["IW"][tt*512:(tt+1)*512, :].rearrange("(s p) n -> p s n", p=128), ob2[:, :, 128:136], reads=[ob2], writes=[S["IW"]])
    st.run()


def wc_stage(nc, wuvT_d, wbb_d, WC):
    st = Stage(nc, "sWc"); st.track(WC)
    wuvT = st.sb("wuvT", [64, 8, 128], BF16); st.dma("pool", wuvT[:], wuvT_d.rearrange("h d c -> d h c"), writes=[wuvT])
    wbb = st.sb("wbb", [64, 8, 1024], BF16); st.dma("pool", wbb[:], wbb_d.rearrange("(h d) n -> d h n", d=64), writes=[wbb])
    wc = st.sb("wc", [128, 8, 1024], BF16)
    pss = [st.ps(f"ps{i}") for i in range(4)]
    pi = 0
    for h in range(8):
        for half in range(2):
            ps = pss[pi % 4]; pi += 1
            st.op("pe", lambda e, ps=ps, h=h, half=half: e.matmul(ps[:], wuvT[:, h, :], wbb[:, h, half*512:(half+1)*512], start=True, stop=True), reads=[wuvT, wbb], writes=[ps])
            if pi % 2 == 0:
                st.op("act", lambda e, ps=ps, h=h, half=half: e.copy(wc[:, h, half*512:(half+1)*512], ps[:]), reads=[ps], writes=[wc])
            else:
                st.op("dve", lambda e, ps=ps, h=h, half=half: e.tensor_copy(wc[:, h, half*512:(half+1)*512], ps[:]), reads=[ps], writes=[wc])
    st.dma("sp", WC[:, :, :], wc[:], reads=[wc], writes=[WC])
    st.run()


def merge_stage(nc, XI, XO, YAT, OLT, GAT, GBT, WC, wa_d, wo_d, g_d, b_d, ones_d):
    st = Stage(nc, "sD")
    for b in (XI, XO, YAT, OLT, GAT, GBT, WC): st.track(b)
    ones = st.sb("ones", [128, 128], F32); st.dma("sp", ones[:], ones_d[:, :], writes=[ones])
    gcol = st.sb("gcol", [128, 8], F32); st.dma("sp", gcol[:], g_d, writes=[gcol])
    bcol = st.sb("bcol", [128, 8], F32); st.dma("sp", bcol[:], b_d, writes=[bcol])
    wa = st.sb("wa", [128, 4, 1024], BF16); st.dma("pool", wa[:], wa_d.rearrange("(k p) n -> p k n", p=128), writes=[wa])
    wo = st.sb("wo", [128, 8, 1024], BF16); st.dma("pool", wo[:], wo_d.rearrange("(k p) n -> p k n", p=128), writes=[wo])
    wc = st.sb("wc", [128, 8, 1024], BF16); st.dma("sp", wc[:], WC[:, :, :], reads=[WC], writes=[wc])
    yab = st.sb("yab", [128, 4, 512], BF16); ol = st.sb("ol", [128, 8, 512], BF16)
    ga = st.sb("ga", [128, 8, 512], F32); gb = st.sb("gb", [128, 8, 512], F32); x = st.sb("x", [128, 8, 512], F32)
    mg = st.sb("mg", [128, 8, 512], BF16)
    t1 = [st.sb(f"t1{i}", [128, 512], F32) for i in range(2)]; t2 = [st.sb(f"t2{i}", [128, 512], F32) for i in range(2)]
    tmp = dict(sq=st.sb("sq", [128, 8, 512], F32), mean=st.sb("mean", [128, 512], F32), rstd=st.sb("rstd", [128, 512], F32))
    psA = [st.ps(f"psA{i}") for i in range(2)]; psB = [st.ps(f"psB{i}") for i in range(2)]; psO = [st.ps(f"psO{i}") for i in range(2)]
    psS = st.ps("psS"); psQ = st.ps("psQ")
    for tt in range(T // 512):
        ts = slice(tt*512, (tt+1)*512)
        st.dma("pool", yab[:], YAT[:, ts].rearrange("(k p) t -> p k t", p=128), reads=[YAT], writes=[yab])
        st.dma("sp", ol[:], OLT[:, :, ts], reads=[OLT], writes=[ol])
        st.dma("sp", ga[:], GAT[:, ts].rearrange("(k p) t -> p k t", p=128), reads=[GAT], writes=[ga])
        st.dma("sp", gb[:], GBT[:, ts].rearrange("(k p) t -> p k t", p=128), reads=[GBT], writes=[gb])
        st.dma("sp", x[:], XI[:, ts].rearrange("(k p) t -> p k t", p=128), reads=[XI], writes=[x])
        for c in range(8):
            pa = psA[c % 2]; pb = psB[c % 2]; a1 = t1[c % 2]; a2 = t2[c % 2]; cs = slice(c*128, (c+1)*128)
            for k in range(4):
                st.op("pe", lambda e, pa=pa, k=k, cs=cs: e.matmul(pa[:], wa[:, k, cs], yab[:, k, :], start=(k == 0), stop=(k == 3)), reads=[wa, yab], writes=[pa])
            for h in range(8):
                st.op("pe", lambda e, pb=pb, h=h, cs=cs: e.matmul(pb[:], wc[:, h, cs], ol[:, h, :], start=(h == 0), stop=(h == 7)), reads=[wc, ol], writes=[pb])
            st.op("dve", lambda e, pa=pa, a1=a1, c=c: e.tensor_tensor(a1[:], pa[:], ga[:, c, :], ALU.mult), reads=[pa, ga], writes=[a1])
            st.op("dve", lambda e, pb=pb, a2=a2, c=c: e.tensor_tensor(a2[:], pb[:], gb[:, c, :], ALU.mult), reads=[pb, gb], writes=[a2])
            st.op("pool", lambda e, a1=a1, a2=a2, c=c: e.tensor_tensor(mg[:, c, :], a1[:], a2[:], ALU.add), reads=[a1, a2], writes=[mg])
        for c in range(8):
            po = psO[c % 2]; cs = slice(c*128, (c+1)*128)
            for k in range(8):
                st.op("pe", lambda e, po=po, k=k, cs=cs: e.matmul(po[:], wo[:, k, cs], mg[:, k, :], start=(k == 0), stop=(k == 7)), reads=[wo, mg], writes=[po])
            st.op("dve", lambda e, po=po, c=c: e.scalar_tensor_tensor(x[:, c, :], x[:, c, :], ALPHA, po[:], ALU.mult, ALU.add), reads=[x, po], writes=[x])
        layer_norm(st, x, ones, gcol, bcol, tmp["sq"], (psS, psQ), tmp)
        st.dma("sp", XO[:, ts].rearrange("(k p) t -> p k t", p=128), tmp["sq"][:], reads=[tmp["sq"]], writes=[XO])
    st.run()


def ffn_stage(nc, name, tb0, tb1, XI, XO, wg_d, wu_d, wd_d, g_d, b_d, C, nexp, dff, G, rw_d=None):
    TB = 1024; NF = dff // 128; NG = NF // G
    st = Stage(nc, name); st.track(XI); st.track(XO)
    ones = st.sb("ones", [128, 128], F32); st.dma("sp", ones[:], C["ones"][:, :], writes=[ones])
    gcol = st.sb("gcol", [128, 8], F32); st.dma("sp", gcol[:], g_d, writes=[gcol])
    bcol = st.sb("bcol", [128, 8], F32); st.dma("sp", bcol[:], b_d, writes=[bcol])
    moe = rw_d is not None
    xb = st.sb("xb", [128, 8, TB], BF16); y = st.sb("y", [128, 8, TB], F32)
    wgg = st.sb("wgg", [128, 8, G*128], BF16); wug = st.sb("wug", [128, 8, G*128], BF16); wdg = st.sb("wdg", [128, G, 1024], BF16)
    hg = st.sb("hg", [128, G, 2, 512], BF16)
    sg = [st.sb(f"sg{i}", [128, 512], F32) for i in range(2)]
    psg = [st.ps(f"psg{i}") for i in range(2)]; psu = [st.ps(f"psu{i}") for i in range(2)]
    psd = [st.ps(f"psd{i}") for i in range(4)]
    tmp = dict(sq=st.sb("sq", [128, 8, 512], F32), mean=st.sb("mean", [128, 512], F32), rstd=st.sb("rstd", [128, 512], F32))
    xr = st.sb("xr", [128, 8, 512], F32)
    if moe:
        ident = st.sb("ident", [128, 128], F32); st.dma("sp", ident[:], C["ident"][:, :], writes=[ident])
        rw = st.sb("rw", [128, 8, 8], F32); st.dma("sp", rw[:], rw_d.rearrange("(k p) e -> p k e", p=128), writes=[rw])
        sel = st.sb("sel", [8, 8, 128], F32); st.dma("sp", sel[:], C["SEL"][:, :, :], writes=[sel])
        xs = [st.sb(f"xs{i}", [128, 8, 128], F32) for i in range(2)]
        GT = st.sb("GT", [8, TB], F32); gbc = st.sb("gbc", [128, TB], F32)
        t2 = [st.sb(f"t2{i}", [128, 512], F32) for i in range(2)]
        sm = {n: st.sb(n, s, F32) for n, s in dict(lg=[128, 8], m8=[128, 8], dl=[128, 1], g1=[128, 1], g2=[128, 1], e1=[128, 8], e2=[128, 8]).items()}
    it = 0
    for tb in range(tb0, tb1):
        t0 = tb * TB
        st.dma("pool", xb[:], XI[:, t0:t0+TB].rearrange("(k p) t -> p k t", p=128), reads=[XI], writes=[xb])
        if moe:
            lg, m8, dl, g1, g2, e1, e2 = [sm[n] for n in ("lg", "m8", "dl", "g1", "g2", "e1", "e2")]
            for s in range(TB // 128):
                xst = xs[s % 2]; pr = psd[s % 4]
                st.dma("sp", xst[:], XI[:, t0+s*128:t0+(s+1)*128].rearrange("(k p) t -> p k t", p=128), reads=[XI], writes=[xst])
                for k in range(8):
                    st.op("pe", lambda e, pr=pr, xst=xst, k=k: e.matmul(pr[:, 0:8], xst[:, k, :], rw[:, k, :], start=(k == 0), stop=(k == 7)), reads=[xst, rw], writes=[pr])
                st.op("act", lambda e, pr=pr: e.copy(lg[:], pr[:, 0:8]), reads=[pr], writes=[lg])
                st.op("dve", lambda e: e.max(m8[:], lg[:]), reads=[lg], writes=[m8])
                st.op("dve", lambda e: e.tensor_tensor(dl[:], m8[:, 0:1], m8[:, 1:2], ALU.subtract), reads=[m8], writes=[dl])
                st.op("act", lambda e: e.activation(g1[:], dl[:], AF.Sigmoid), reads=[dl], writes=[g1])
                st.op("act", lambda e: e.activation(g2[:], dl[:], AF.Sigmoid, scale=-1.0), reads=[dl], writes=[g2])
                st.op("dve", lambda e: e.tensor_scalar(e1[:], lg[:], m8[:, 0:1], g1[:, 0:1], ALU.is_equal, ALU.mult), reads=[lg, m8, g1], writes=[e1])
                st.op("dve", lambda e: e.tensor_scalar(e2[:], lg[:], m8[:, 1:2], g2[:, 0:1], ALU.is_equal, ALU.mult), reads=[lg, m8, g2], writes=[e2])
                st.op("dve", lambda e: e.tensor_tensor(e1[:], e1[:], e2[:], ALU.add), reads=[e1, e2], writes=[e1])
                st.op("pe", lambda e, pr=pr: e.transpose(pr[0:8, 128:256], e1[:], ident[:]), reads=[e1, ident], writes=[pr])
                st.op("act", lambda e, pr=pr, s=s: e.copy(GT[:, s*128:(s+1)*128], pr[0:8, 128:256]), reads=[pr], writes=[GT])
        first = True
        for ex in range(nexp):
            if moe:
                for sub in range(2):
                    pr = psd[sub]
                    st.op("pe", lambda e, pr=pr, ex=ex, sub=sub: e.matmul(pr[:], sel[:, ex, :], GT[:, sub*512:(sub+1)*512], start=True, stop=True), reads=[sel, GT], writes=[pr])
                    st.op("act", lambda e, pr=pr, sub=sub: e.copy(gbc[:, sub*512:(sub+1)*512], pr[:]), reads=[pr], writes=[gbc])
            for grp in range(NG):
                f0 = grp * G * 128
                st.dma("pool", wgg[:], wg_d[ex, :, f0:f0+G*128].rearrange("(k p) n -> p k n", p=128), writes=[wgg])
                st.dma("pool", wug[:], wu_d[ex, :, f0:f0+G*128].rearrange("(k p) n -> p k n", p=128), writes=[wug])
                st.dma("pool", wdg[:], wd_d[ex, f0:f0+G*128, :].rearrange("(g p) n -> p g n", p=128), writes=[wdg])
                for fi in range(G):
                    fs = slice(fi*128, (fi+1)*128)
                    for sub in range(2):
                        pg, pu, sgt = psg[it % 2], psu[it % 2], sg[it % 2]
                        ss = slice(sub*512, (sub+1)*512)
                        for k in range(8):
                            st.op("pe", lambda e, pg=pg, k=k, fs=fs, ss=ss: e.matmul(pg[:], wgg[:, k, fs], xb[:, k, ss], start=(k == 0), stop=(k == 7)), reads=[wgg, xb], writes=[pg])
                        for k in range(8):
                            st.op("pe", lambda e, pu=pu, k=k, fs=fs, ss=ss: e.matmul(pu[:], wug[:, k, fs], xb[:, k, ss], start=(k == 0), stop=(k == 7)), reads=[wug, xb], writes=[pu])
                        st.op("act", lambda e, pg=pg, sgt=sgt: e.activation(sgt[:], pg[:], AF.Silu), reads=[pg], writes=[sgt])
                        if moe:
                            tt2 = t2[it % 2]
                            st.op("dve", lambda e, pu=pu, tt2=tt2, ss=ss: e.tensor_tensor(tt2[:], pu[:], gbc[:, ss], ALU.mult), reads=[pu, gbc], writes=[tt2])
                            st.op("pool", lambda e, sgt=sgt, tt2=tt2, fi=fi, sub=sub: e.tensor_tensor(hg[:, fi, sub, :], sgt[:], tt2[:], ALU.mult), reads=[sgt, tt2], writes=[hg])
                        else:
                            st.op("dve", lambda e, pu=pu, sgt=sgt, fi=fi, sub=sub: e.tensor_tensor(hg[:, fi, sub, :], sgt[:], pu[:], ALU.mult), reads=[pu, sgt], writes=[hg])
                        it += 1
                for sub in range(2):
                    ss = slice(sub*512, (sub+1)*512)
                    for c in range(8):
                        pd = psd[c % 4]; cs = slice(c*128, (c+1)*128)
                        for fi in range(G):
                            st.op("pe", lambda e, pd=pd, fi=fi, cs=cs, sub=sub: e.matmul(pd[:], wdg[:, fi, cs], hg[:, fi, sub, :], start=(fi == 0), stop=(fi == G-1)), reads=[wdg, hg], writes=[pd])
                        if first:
                            st.op("dve", lambda e, pd=pd, c=c, ss=ss: e.tensor_copy(y[:, c, ss], pd[:]), reads=[pd], writes=[y])
                        else:
                            st.op("dve", lambda e, pd=pd, c=c, ss=ss: e.tensor_tensor(y[:, c, ss], y[:, c, ss], pd[:], ALU.add), reads=[pd, y], writes=[y])
                first = False
        for sub in range(2):
            ss = slice(sub*512, (sub+1)*512); ts = slice(t0+sub*512, t0+(sub+1)*512)
            st.dma("sp", xr[:], XI[:, ts].rearrange("(k p) t -> p k t", p=128), reads=[XI], writes=[xr])
            st.op("dve", lambda e, ss=ss: e.scalar_tensor_tensor(xr[:], xr[:], ALPHA, y[:, :, ss], ALU.mult, ALU.add), reads=[xr, y], writes=[xr])
            layer_norm(st, xr, ones, gcol, bcol, tmp["sq"], (psg[0], psu[0]), tmp)
            st.dma("sp", XO[:, ts].rearrange("(k p) t -> p k t", p=128), tmp["sq"][:], reads=[tmp["sq"]], writes=[XO])
    st.run()

NIT = 22


def dump_stage(nc, name, SRC, dst_ap):
    st = Stage(nc, name); st.track(SRC)
    Dst = st.track(Buf(dst_ap, multi=True))
    t = [st.sb(f"t{i}", [128, 8, 512], F32) for i in range(2)]
    for tt in range(T // 512):
        ts = slice(tt*512, (tt+1)*512)
        st.dma("sp", t[tt % 2][:], SRC[:, ts].rearrange("(k p) t -> p k t", p=128), reads=[SRC], writes=[t[tt % 2]])
        st.dma("sp", Dst[:, ts].rearrange("(k p) t -> p k t", p=128), t[tt % 2][:], reads=[t[tt % 2]], writes=[Dst])
    st.run()


def build_full(NL, debug=False):
    nc = bass.Bass("TRN2", target_bir_lowering=False)
    NQ = T // 128; ND = (NL + 1) // 2; NM = NL // 2
    dr = lambda n, s, k="Internal", dt=F32: nc.dram_tensor(n, list(s), dt, kind=k).ap()
    ein = lambda n, s: dr(n, s, "ExternalInput")
    x_in = ein("x", [T, D]); w_in = ein("w_in", [NL, D, 4528]); convT = ein("convT", [NL, 512, 4])
    gbias = ein("gbias", [NL, 1, 8]); ng = ein("ng", [NL, 1, 512]); kvg = ein("kvg", [NL, 1, 128])
    wuk = ein("wuk", [NL, 8, 64, 128]); wuvT = ein("wuvT", [NL, 8, 64, 128]); rb = ein("rb", [32, 8])
    wba = ein("wba", [NL, 512, 1024]); wbb = ein("wbb", [NL, 512, 1024]); wout = ein("wout", [NL, 1024, 1024])
    lng = ein("lng", [NL, 2, 128, 8]); lnb = ein("lnb", [NL, 2, 128, 8])
    dwg = ein("dwg", [ND, 1, D, 2816]); dwu = ein("dwu", [ND, 1, D, 2816]); dwd = ein("dwd", [ND, 1, 2816, D])
    if NM:
        rw = ein("rw", [NM, D, 8]); ewg = ein("ewg", [NM, 8, D, 3584]); ewu = ein("ewu", [NM, 8, D, 3584]); ewd = ein("ewd", [NM, 8, 3584, D])
    C = {n: ein(n, s) for n, s in dict(OH1=[32, 512], J=[128, 128], NEGTRI=[128, 128], POW2=[128, NIT], IREP=[128, 512], ident=[128, 128],
                                       ones=[128, 128], U=[64, 64], SEL=[8, 8, 128]).items()}
    y = dr("y", [T, D], "ExternalOutput")
    dbg = [dr(f"dbg{l}", [D, T], "ExternalOutput") for l in range(NL)] if debug else None
    mb = lambda n, s, dt=F32: Buf(dr(n, s, dt=dt), multi=True)
    XTa = mb("XTa", [D, T]); XTb = mb("XTb", [D, T])
    S = dict(QKT=mb("QKT", [512, T]), BQT=mb("BQT", [512, T]), IQT=mb("IQT", [256, T]), IKT=mb("IKT", [32, T]),
             GAT=mb("GAT", [1024, T]), GBT=mb("GBT", [1024, T]), VAO=mb("VAO", [T, 1032]), BC=mb("BC", [T, 128]), IW=mb("IW", [T, 8]))
    QKC = mb("QKC", [512, T]); YAT = mb("YAT", [512, T])
    CKT = mb("CKT", [128, T], BF16); CKV = mb("CKV", [T, 129], BF16); QLT = mb("QLT", [128, NQ, 8, 128], BF16); OLT = mb("OLT", [128, 8, T], BF16)
    WC = mb("WC", [128, 8, 1024], BF16)
    BVh = nc.dram_tensor("BV", [8, 512], F32); BIASD = dr("BIASD", [3, 128, 2, 1024], dt=BF16)
    SFX[0] = ""
    transpose_in_stage(nc, x_in, XTa, C["ident"])
    bias_setup(nc, rb, C["OH1"], C["J"], BVh, BIASD)
    bounds = [0]
    while bounds[-1] < NQ:
        a = bounds[-1]; b = a; pairs = 0
        while b < NQ and (pairs + b + 1 <= 1100 or b == a):
            pairs += b + 1; b += 1
        bounds.append(b)
    for l in range(NL):
        SFX[0] = f"_L{l}"
        inproj_stage(nc, XTa, w_in[l], S)
        conv_stage(nc, S["QKT"], QKC, convT[l])
        mlstm_stage(nc, QKC, S["VAO"], YAT, gbias[l], ng[l], C["U"], C["ident"])
        dsa_prep(nc, S["BQT"], S["BC"], wuk[l], kvg[l], C["ident"], CKT, CKV, QLT)
        for i in range(len(bounds) - 1):
            dsa_attn(nc, f"sC2_{i}", bounds[i], bounds[i+1], S["IQT"], S["IKT"], S["IW"], CKT, CKV, QLT, BIASD, OLT, C)
        wc_stage(nc, wuvT[l], wbb[l], WC)
        merge_stage(nc, XTa, XTb, YAT, OLT, S["GAT"], S["GBT"], WC, wba[l], wout[l], lng[l, 0], lnb[l, 0], C["ones"])
        NTB = T // 1024
        if l % 2 == 0:
            ffn_stage(nc, "sE", 0, NTB, XTb, XTa, dwg[l // 2], dwu[l // 2], dwd[l // 2], lng[l, 1], lnb[l, 1], C, 1, 2816, 11)
        else:
            j = l // 2
            for tb in range(0, NTB, 2):
                ffn_stage(nc, f"sF{tb}", tb, min(tb + 2, NTB), XTb, XTa, ewg[j], ewu[j], ewd[j], lng[l, 1], lnb[l, 1], C, 8, 3584, 7, rw_d=rw[j])
        if debug:
            dump_stage(nc, "sdbg", XTa, dbg[l])
    SFX[0] = "_fin"
    transpose_out_stage(nc, XTa, y, C["ident"])
    return nc


def host_inputs(inputs, NL):
    f = lambda a: np.ascontiguousarray(np.asarray(a, dtype=np.float32))
    ND = (NL + 1) // 2; NM = NL // 2
    colz = lambda v: f(np.asarray(v)[:NL].reshape(NL, 2, 8, 128).transpose(0, 1, 3, 2))
    d = dict(
        w_in=f(inputs["w_in"][:NL]), convT=f(np.asarray(inputs["mlstm_conv_w"])[:NL].transpose(0, 2, 1)),
        gbias=f(np.asarray(inputs["mlstm_gate_bias"])[:NL].reshape(NL, 1, 8)), ng=f(np.asarray(inputs["mlstm_norm_g"])[:NL].reshape(NL, 1, 512)),
        kvg=f(np.asarray(inputs["dsa_kv_norm_g"])[:NL].reshape(NL, 1, 128)), wuk=f(inputs["dsa_w_uk"][:NL]),
        wuvT=f(np.asarray(inputs["dsa_w_uv"])[:NL].transpose(0, 1, 3, 2)), rb=f(inputs["rel_bias"]),
        wba=f(inputs["w_branch_a"][:NL]), wbb=f(inputs["w_branch_b"][:NL]), wout=f(inputs["w_out"][:NL]),
        lng=colz(inputs["ln_g"]), lnb=colz(inputs["ln_b"]),
        dwg=f(np.asarray(inputs["dense_w_gate"])[:ND, None]), dwu=f(np.asarray(inputs["dense_w_up"])[:ND, None]), dwd=f(np.asarray(inputs["dense_w_down"])[:ND, None]),
    )
    if NM:
        d.update(rw=f(inputs["router_w"][:NM]), ewg=f(inputs["expert_w_gate"][:NM]), ewu=f(inputs["expert_w_up"][:NM]), ewd=f(inputs["expert_w_down"][:NM]))
    hc = host_consts()
    hc["ones"] = np.ones((128, 128), np.float32); hc["U"] = np.triu(np.ones((64, 64), np.float32))
    sel = np.zeros((8, 8, 128), np.float32)
    for e in range(8): sel[e, e, :] = 1.0
    hc["SEL"] = sel
    d.update(hc)
    return d


def kernel(**inputs):
    x = np.asarray(inputs["x"], dtype=np.float32)
    n = x.shape[0]
    NL = 4
    shared = host_inputs(inputs, NL)
    nc = build_full(NL, debug=False)
    in_maps = [dict(shared, x=np.ascontiguousarray(x[c])) for c in range(n)]
    res = run_bass_kernel_spmd(nc, in_maps, core_ids=list(range(n)))
    return np.stack([np.asarray(res.results[c]["y"], dtype=np.float32) for c in range(n)], axis=0)
```

```python
import numpy as np
from contextlib import ExitStack
import concourse.bass as bass
import concourse.mybir as mybir
from concourse.bass_utils import run_bass_kernel_spmd

F32, BF16 = mybir.dt.float32, mybir.dt.bfloat16
ALU = mybir.AluOpType
AF = mybir.ActivationFunctionType
AX = mybir.AxisListType
ENGS = ("pe", "act", "dve", "pool", "sp")
SFX = [""]
SCOUNT = [0]
GLOBAL = {}


def _merge(d, s):
    for k, v in s.items():
        if d.get(k, 0) < v:
            d[k] = v


class Buf:
    def __init__(self, t, multi=False):
        self.t = t
        self.multi = multi
        self.w = {}
        self.r = {}
        self.dsem = None

    def __getitem__(self, k):
        return self.t[k]


class Stage:
    def __init__(self, nc, name):
        self.nc = nc
        name = name + SFX[0]
        self.name = name
        self.es = ExitStack()
        self.ops = {e: [] for e in ENGS}
        g = GLOBAL.get(id(nc))
        if g is None:
            ges = ExitStack()
            g = dict(es=ges, sem={e: ges.enter_context(nc.semaphore(f"g_{e}")) for e in ENGS}, cnt={e: 0 for e in ENGS}, pool=[])
            GLOBAL[id(nc)] = g
        self.g = g
        self.sem = g["sem"]
        self.cnt = dict(g["cnt"])
        self.dsems = {}
        self.ndsem = 0
        self.bufs = []
        self.semkey = {}

    def sb(self, name, shape, dt):
        t = self.es.enter_context(self.nc.sbuf_tensor(f"{self.name}_{name}", list(shape), dt))
        return self.track(Buf(t))

    def ps(self, name, shape=(128, 512), dt=F32):
        t = self.es.enter_context(self.nc.psum_tensor(f"{self.name}_{name}", list(shape), dt))
        return self.track(Buf(t))

    def track(self, b):
        b.w = {}
        b.r = {}
        b.dsem = None
        self.bufs.append(b)
        return b

    def _deps(self, reads, writes):
        deps = {}
        for b in reads:
            _merge(deps, b.w)
        for b in writes:
            _merge(deps, b.r)
            if not b.multi:
                _merge(deps, b.w)
        return deps

    def _commit(self, tok, reads, writes):
        for b in reads:
            _merge(b.r, tok)
        for b in writes:
            if b.multi:
                _merge(b.w, tok)
            else:
                b.w = dict(tok)
                b.r = {}

    def op(self, eng, fn, reads=(), writes=()):
        deps = self._deps(reads, writes)
        self.cnt[eng] += 1
        s = self.sem[eng]
        tok = {id(s): self.cnt[eng]}
        self.semkey[id(s)] = s
        self.ops[eng].append((deps, fn, s, 1))
        self._commit(tok, reads, writes)

    def dma(self, eng, out, in_, reads=(), writes=()):
        deps = self._deps(reads, writes)
        b = writes[0]
        if b.dsem is None:
            pool = self.g["pool"]
            if self.ndsem >= len(pool):
                pool.append([self.g["es"].enter_context(self.nc.semaphore(f"g_d{len(pool)}")), 0])
            b.dsem = pool[self.ndsem][0]
            self.dsems[id(b.dsem)] = pool[self.ndsem][1]
            self.semkey[id(b.dsem)] = b.dsem
            self.ndsem += 1
        self.dsems[id(b.dsem)] += 16
        tok = {id(b.dsem): self.dsems[id(b.dsem)]}
        self.ops[eng].append((deps, lambda e, o=out, i=in_: e.dma_start(out=o, in_=i), b.dsem, 16))
        self._commit(tok, reads, writes)

    def run(self):
        nc = self.nc
        final = {}
        for e in ENGS:
            if self.cnt[e] > self.g["cnt"][e]:
                final[id(self.sem[e])] = self.cnt[e]
                self.semkey[id(self.sem[e])] = self.sem[e]
        final.update({k: v for k, v in self.dsems.items()})

        def emit(engname, eng):
            waited = {}
            own = id(self.sem[engname])
            for deps, fn, s, n in self.ops[engname]:
                for k, v in deps.items():
                    if engname == "pe" and k == own:
                        continue
                    if waited.get(k, 0) < v:
                        eng.wait_ge(self.semkey[k], v)
                        waited[k] = v
                fn(eng).then_inc(s, n)
            for k, v in final.items():
                if waited.get(k, 0) < v:
                    eng.wait_ge(self.semkey[k], v)

        with nc.Block() as block:
            @block.tensor
            def _(e):
                emit("pe", e)

            @block.scalar
            def _(e):
                emit("act", e)

            @block.vector
            def _(e):
                emit("dve", e)

            @block.gpsimd
            def _(e):
                emit("pool", e)

            @block.sync
            def _(e):
                emit("sp", e)
        for e in ENGS:
            self.g["cnt"][e] = self.cnt[e]
        for p in self.g["pool"]:
            if id(p[0]) in self.dsems:
                p[1] = self.dsems[id(p[0])]
        for b in self.bufs:
            b.w = {}
            b.r = {}
            b.dsem = None
        self.es.close()

import math, os
T = 8192

def conv_stage(nc, QKT, QKC, convT_d):
    st = Stage(nc, "sB1"); st.track(QKT); st.track(QKC)
    cw = st.sb("cw", [128, 4, 4], F32)
    st.dma("sp", cw[:], convT_d.rearrange("(k p) j -> p k j", p=128), writes=[cw])
    win = [st.sb(f"win{i}", [128, 4, 515], F32) for i in range(2)]
    acc = [st.sb(f"acc{i}", [128, 4, 512], F32) for i in range(2)]
    out = [st.sb(f"out{i}", [128, 4, 512], F32) for i in range(2)]
    for tt in range(T // 512):
        w = win[tt % 2]; a = acc[tt % 2]; o = out[tt % 2]
        if tt == 0:
            st.op("pool", lambda e, w=w: e.memset(w[:, :, 0:3], 0.0), writes=[w])
            st.dma("sp", w[:, :, 3:515], QKT[:, 0:512].rearrange("(k p) t -> p k t", p=128), reads=[QKT], writes=[w])
        else:
            st.dma("sp", w[:], QKT[:, tt*512-3:(tt+1)*512].rearrange("(k p) t -> p k t", p=128), reads=[QKT], writes=[w])
        for k in range(4):
            eng = "dve"
            st.op(eng, lambda e, w=w, a=a, k=k: e.tensor_scalar(a[:, k, :], w[:, k, 0:512], cw[:, k, 0:1], None, ALU.mult), reads=[w, cw], writes=[a])
            for j in range(1, 4):
                st.op(eng, lambda e, w=w, a=a, k=k, j=j: e.scalar_tensor_tensor(a[:, k, :], w[:, k, j:j+512], cw[:, k, j:j+1], a[:, k, :], ALU.mult, ALU.add), reads=[w, cw, a], writes=[a])
        st.op("act", lambda e, a=a, o=o: e.activation(o[:], a[:], AF.Silu), reads=[a], writes=[o])
        st.op("dve", lambda e, o=o: e.tensor_scalar(o[:, 2:4, :], o[:, 2:4, :], 0.125, None, ALU.mult), reads=[o], writes=[o])
        st.dma("sp", QKC[:, tt*512:(tt+1)*512].rearrange("(k p) t -> p k t", p=128), o[:], reads=[o], writes=[QKC])
    st.run()

def layer_norm(st, r, ones, gcol, bcol, out, pss, tmp):
    sq, mean, rstd = tmp["sq"], tmp["mean"], tmp["rstd"]
    ps_s, ps_q = pss
    st.op("act", lambda e: e.activation(sq[:], r[:], AF.Square), reads=[r], writes=[sq])
    for k in range(8):
        st.op("pe", lambda e, k=k: e.matmul(ps_s[:], ones[:], r[:, k, :], start=(k == 0), stop=(k == 7)), reads=[ones, r], writes=[ps_s])
    for k in range(8):
        st.op("pe", lambda e, k=k: e.matmul(ps_q[:], ones[:], sq[:, k, :], start=(k == 0), stop=(k == 7)), reads=[ones, sq], writes=[ps_q])
    st.op("dve", lambda e: e.tensor_scalar(mean[:], ps_s[:], 1.0 / 1024, None, ALU.mult), reads=[ps_s], writes=[mean])
    st.op("dve", lambda e: e.tensor_tensor(rstd[:], mean[:], mean[:], ALU.mult), reads=[mean], writes=[rstd])
    st.op("dve", lambda e: e.scalar_tensor_tensor(rstd[:], ps_q[:], 1.0 / 1024, rstd[:], ALU.mult, ALU.subtract), reads=[ps_q, rstd], writes=[rstd])
    st.op("dve", lambda e: e.tensor_scalar(rstd[:], rstd[:], 1e-5, None, ALU.add), reads=[rstd], writes=[rstd])
    st.op("act", lambda e: e.activation(rstd[:], rstd[:], AF.Ln), reads=[rstd], writes=[rstd])
    st.op("act", lambda e: e.activation(rstd[:], rstd[:], AF.Exp, scale=-0.5), reads=[rstd], writes=[rstd])
    st.op("dve", lambda e: e.tensor_tensor(sq[:], r[:], mean[:].unsqueeze(1).to_broadcast([128, 8, 512]), ALU.subtract), reads=[r, mean], writes=[sq])
    st.op("pool", lambda e: e.tensor_tensor(sq[:], sq[:], rstd[:].unsqueeze(1).to_broadcast([128, 8, 512]), ALU.mult), reads=[sq, rstd], writes=[sq])
    for k in range(8):
        eng = "dve" if k % 2 == 0 else "pool"
        st.op(eng, lambda e, k=k: e.tensor_scalar(out[:, k, :], sq[:, k, :], gcol[:, k:k+1], bcol[:, k:k+1], ALU.mult, ALU.add), reads=[sq, gcol, bcol], writes=[out])

def mlstm_stage(nc, QKC, VAO, YAT, gb_d, ng_d, U_d, ident_d):
    st = Stage(nc, "sB2"); st.track(QKC); st.track(VAO); st.track(YAT)
    U = st.sb("U", [64, 64], F32); st.dma("sp", U[:], U_d[:, :], writes=[U])
    ident = st.sb("ident", [128, 128], F32); st.dma("sp", ident[:], ident_d[:, :], writes=[ident])
    gb = st.sb("gb", [64, 8], F32); st.dma("sp", gb[:], gb_d.partition_broadcast(64), writes=[gb])
    ng = st.sb("ng", [64, 512], F32); st.dma("sp", ng[:], ng_d.partition_broadcast(64), writes=[ng])
    ones64 = st.sb("ones64", [64, 64], F32); st.op("pool", lambda e: e.memset(ones64[:], 1.0), writes=[ones64])
    maskb = st.sb("maskb", [64, 4, 64], F32)
    st.op("dve", lambda e: e.tensor_copy(maskb[:], U[:].unsqueeze(1).to_broadcast([64, 4, 64])), reads=[U], writes=[maskb])
    C = st.sb("C", [64, 4, 129], F32); st.op("pool", lambda e: e.memset(C[:], 0.0), writes=[C])
    q4s = [st.sb(f"q4{i}", [64, 8, 512], F32) for i in range(2)]
    vas = [st.sb(f"va{i}", [64, 8, 1032], F32) for i in range(2)]
    yos = [st.sb(f"yo{i}", [128, 4, 512], F32) for i in range(2)]
    def mk(i):
        d = {}
        for n, s in dict(g=[64, 8], e1=[64, 4], nl=[64, 4], nlb=[64, 4, 64], col=[64, 4], Dt=[64, 4, 64], Ebc=[64, 4, 64], wtmp=[64, 4], wcol=[64, 4],
                         dec=[64, 4], nbt=[64, 8], Wt=[64, 4, 64], qt=[64, 4, 64], kt=[64, 4, 64], vaug=[64, 4, 129], den=[64, 4], rden=[64, 4], hn=[64, 4, 128],
                         sm=[64, 4], xc=[64, 4, 128], sq=[64, 4, 128], ssq=[64, 4], rstd=[64, 4], sig=[64, 512], gs=[64, 512], y=[64, 512]).items():
            d[n] = st.sb(f"{n}{i}", s, F32)
        st.op("pool", lambda e, v=d["vaug"]: e.memset(v[:], 1.0), writes=[d["vaug"]])
        return d
    tmps = [mk(0), mk(1)]
    psA = st.ps("psA", [64, 512]); psB = st.ps("psB", [64, 512])
    pSK = st.ps("pSK", [64, 512])
    psN = [st.ps(f"psN{j}", [64, 512]) for j in range(2)]; psU = [st.ps(f"psU{j}", [64, 512]) for j in range(2)]
    psY = st.ps("psY", [128, 512])
    for tt in range(T // 512):
        q4 = q4s[tt % 2]; va = vas[tt % 2]; yo = yos[tt % 2]
        st.dma("sp", q4[:], QKC[:, tt*512:(tt+1)*512].rearrange("(g d) t -> d g t", d=64), reads=[QKC], writes=[q4])
        st.dma("sp", va[:], VAO[tt*512:(tt+1)*512, :].rearrange("(c p) n -> p c n", p=64), reads=[VAO], writes=[va])
        for c in range(8):
            d = tmps[c % 2]; cs = slice(c*64, (c+1)*64)
            g, e1, nl, nlb, col, Dt, Ebc, wtmp, wcol, dec, Wt, qt, kt, vaug, den, rden, hn, sm, xc, sq, ssq, rstd, sig, gs, y = [d[n] for n in
                ("g", "e1", "nl", "nlb", "col", "Dt", "Ebc", "wtmp", "wcol", "dec", "Wt", "qt", "kt", "vaug", "den", "rden", "hn", "sm", "xc", "sq", "ssq", "rstd", "sig", "gs", "y")]
            st.op("dve", lambda e, g=g, va=va, c=c: e.tensor_tensor(g[:], va[:, c, 1024:1032], gb[:], ALU.add), reads=[va, gb], writes=[g])
            st.op("act", lambda e, g=g, e1=e1: e.activation(e1[:], g[:, 4:8], AF.Exp, scale=-1.0), reads=[g], writes=[e1])
            st.op("dve", lambda e, e1=e1: e.tensor_scalar(e1[:], e1[:], 1.0, None, ALU.add), reads=[e1], writes=[e1])
            st.op("act", lambda e, nl=nl, e1=e1: e.activation(nl[:], e1[:], AF.Ln), reads=[e1], writes=[nl])
            st.op("dve", lambda e, nl=nl, nlb=nlb: e.tensor_copy(nlb[:], nl[:].unsqueeze(2).to_broadcast([64, 4, 64])), reads=[nl], writes=[nlb])
            st.op("pe", lambda e, nl=nl: e.matmul(psA[:, 0:4], U[:], nl[:], start=True, stop=True), reads=[U, nl], writes=[psA])
            st.op("pe", lambda e, nl=nl: e.matmul(psA[:, 4:8], ones64[:], nl[:], start=True, stop=True), reads=[ones64, nl], writes=[psA])
            for h in range(4):
                st.op("pe", lambda e, nlb=nlb, h=h: e.matmul(psB[:, h*64:(h+1)*64], nlb[:, h, :], U[:], start=True, stop=True), reads=[nlb, U], writes=[psB])
            nbt = d["nbt"]
            st.op("act", lambda e, nbt=nbt: e.copy(nbt[:], psA[:, 0:8]), reads=[psA], writes=[nbt])
            st.op("dve", lambda e, col=col, g=g, nbt=nbt: e.tensor_tensor(col[:], nbt[:, 0:4], g[:, 0:4], ALU.add), reads=[nbt, g], writes=[col])
            for h in range(4):
                st.op("act", lambda e, Dt=Dt, col=col, h=h: e.activation(Dt[:, h, :], psB[:, h*64:(h+1)*64], AF.Exp, bias=col[:, h:h+1], scale=-1.0), reads=[psB, col], writes=[Dt])
            st.op("act", lambda e, Ebc=Ebc: e.activation(Ebc[:].rearrange("p h t -> p (h t)"), psB[:, 0:256], AF.Exp, scale=-1.0), reads=[psB], writes=[Ebc])
            st.op("dve", lambda e, wtmp=wtmp, col=col, nbt=nbt: e.tensor_tensor(wtmp[:], col[:], nbt[:, 4:8], ALU.subtract), reads=[col, nbt], writes=[wtmp])
            st.op("act", lambda e, wcol=wcol, wtmp=wtmp: e.activation(wcol[:], wtmp[:], AF.Exp), reads=[wtmp], writes=[wcol])
            st.op("act", lambda e, dec=dec, nbt=nbt: e.activation(dec[:], nbt[:, 4:8], AF.Exp, scale=-1.0), reads=[nbt], writes=[dec])
            for h in range(4):
                st.op("pe", lambda e, q4=q4, h=h, cs=cs: e.matmul(pSK[:, h*64:(h+1)*64], q4[:, 4+h, cs], q4[:, h, cs], start=True, stop=True), reads=[q4], writes=[pSK])
            st.op("dve", lambda e, Wt=Wt, Dt=Dt: e.tensor_tensor(Wt[:], Dt[:], maskb[:], ALU.mult), reads=[Dt, maskb], writes=[Wt])
            st.op("dve", lambda e, Wt=Wt: e.tensor_tensor(Wt[:], Wt[:], pSK[:, 0:256].rearrange("p (h t) -> p h t", h=4), ALU.mult), reads=[Wt, pSK], writes=[Wt])
            st.op("pool", lambda e, qt=qt, q4=q4, Ebc=Ebc, cs=cs: e.tensor_tensor(qt[:], q4[:, 0:4, cs], Ebc[:], ALU.mult), reads=[q4, Ebc], writes=[qt])
            for h in range(4):
                st.op("pe", lambda e, q4=q4, h=h, cs=cs: e.transpose(pSK[:, 256+h*64:256+(h+1)*64], q4[:, 4+h, cs], ident[0:64, 0:64]), reads=[q4, ident], writes=[pSK])
            st.op("dve", lambda e, kt=kt, wcol=wcol: e.tensor_tensor(kt[:], pSK[:, 256:512].rearrange("p (h t) -> p h t", h=4), wcol[:].unsqueeze(2).to_broadcast([64, 4, 64]), ALU.mult), reads=[pSK, wcol], writes=[kt])
            st.op("pool", lambda e, vaug=vaug, va=va, c=c: e.tensor_copy(vaug[:, :, 0:128], va[:, c, 0:512].rearrange("p (h v) -> p h v", h=4)), reads=[va], writes=[vaug])
            for h in range(4):
                pn = psN[h // 2]; o = (h % 2) * 129
                st.op("pe", lambda e, pn=pn, o=o, Wt=Wt, vaug=vaug, h=h: e.matmul(pn[:, o:o+129], Wt[:, h, :], vaug[:, h, :], start=True, stop=False), reads=[Wt, vaug], writes=[pn])
                st.op("pe", lambda e, pn=pn, o=o, qt=qt, h=h: e.matmul(pn[:, o:o+129], qt[:, h, :], C[:, h, :], start=False, stop=True), reads=[qt, C], writes=[pn])
            for h in range(4):
                pu = psU[h // 2]; o = (h % 2) * 129
                st.op("pe", lambda e, pu=pu, o=o, kt=kt, vaug=vaug, h=h: e.matmul(pu[:, o:o+129], kt[:, h, :], vaug[:, h, :], start=True, stop=True), reads=[kt, vaug], writes=[pu])
            for h in range(4):
                pu = psU[h // 2]; o = (h % 2) * 129
                st.op("dve", lambda e, pu=pu, o=o, dec=dec, h=h: e.scalar_tensor_tensor(C[:, h, :], C[:, h, :], dec[:, h:h+1], pu[:, o:o+129], ALU.mult, ALU.add), reads=[C, dec, pu], writes=[C])
            for j in range(2):
                pv = psN[j]
                st.op("act", lambda e, pv=pv, den=den, j=j: e.activation(den[:, 2*j:2*j+2], pv[:, 0:258].rearrange("p (i n) -> p i n", n=129)[:, :, 128], AF.Abs), reads=[pv], writes=[den])
            st.op("dve", lambda e, den=den: e.tensor_scalar(den[:], den[:], 1.0, None, ALU.max), reads=[den], writes=[den])
            st.op("dve", lambda e, den=den, rden=rden: e.reciprocal(rden[:], den[:]), reads=[den], writes=[rden])
            for j in range(2):
                pv = psN[j]
                st.op("dve", lambda e, pv=pv, hn=hn, rden=rden, j=j: e.tensor_tensor(hn[:, 2*j:2*j+2, :], pv[:, 0:258].rearrange("p (i n) -> p i n", n=129)[:, :, 0:128], rden[:, 2*j:2*j+2].unsqueeze(2).to_broadcast([64, 2, 128]), ALU.mult), reads=[pv, rden], writes=[hn])
            st.op("dve", lambda e, sm=sm, hn=hn: e.tensor_reduce(sm[:], hn[:], AX.X, ALU.add), reads=[hn], writes=[sm])
            st.op("dve", lambda e, sm=sm: e.tensor_scalar(sm[:], sm[:], -1.0 / 128, None, ALU.mult), reads=[sm], writes=[sm])
            st.op("dve", lambda e, xc=xc, hn=hn, sm=sm: e.tensor_tensor(xc[:], hn[:], sm[:].unsqueeze(2).to_broadcast([64, 4, 128]), ALU.add), reads=[hn, sm], writes=[xc])
            st.op("pool", lambda e, sq=sq, xc=xc: e.tensor_tensor(sq[:], xc[:], xc[:], ALU.mult), reads=[xc], writes=[sq])
            st.op("dve", lambda e, ssq=ssq, sq=sq: e.tensor_reduce(ssq[:], sq[:], AX.X, ALU.add), reads=[sq], writes=[ssq])
            st.op("dve", lambda e, ssq=ssq: e.tensor_scalar(ssq[:], ssq[:], 1.0 / 128, 1e-5, ALU.mult, ALU.add), reads=[ssq], writes=[ssq])
            st.op("act", lambda e, ssq=ssq, rstd=rstd: e.activation(rstd[:], ssq[:], AF.Ln), reads=[ssq], writes=[rstd])
            st.op("act", lambda e, rstd=rstd: e.activation(rstd[:], rstd[:], AF.Exp, scale=-0.5), reads=[rstd], writes=[rstd])
            st.op("act", lambda e, sig=sig, va=va, c=c: e.activation(sig[:], va[:, c, 512:1024], AF.Sigmoid), reads=[va], writes=[sig])
            st.op("pool", lambda e, gs=gs, sig=sig: e.tensor_tensor(gs[:], sig[:], ng[:], ALU.mult), reads=[sig, ng], writes=[gs])
            st.op("dve", lambda e, y=y, xc=xc, rstd=rstd: e.tensor_tensor(y[:].rearrange("p (h v) -> p h v", h=4), xc[:], rstd[:].unsqueeze(2).to_broadcast([64, 4, 128]), ALU.mult), reads=[xc, rstd], writes=[y])
            st.op("pool", lambda e, y=y, gs=gs: e.tensor_tensor(y[:], y[:], gs[:], ALU.mult), reads=[y, gs], writes=[y])
            for h in range(4):
                st.op("pe", lambda e, y=y, h=h: e.transpose(psY[:, h*64:(h+1)*64], y[:, h*128:(h+1)*128], ident[0:64, 0:64]), reads=[y, ident], writes=[psY])
            st.op("act", lambda e, yo=yo, cs=cs: e.copy(yo[:, :, cs], psY[:, 0:256].rearrange("p (h t) -> p h t", h=4)), reads=[psY], writes=[yo])
        st.dma("sp", YAT[:, tt*512:(tt+1)*512].rearrange("(k p) t -> p k t", p=128), yo[:], reads=[yo], writes=[YAT])
    st.run()

def t5_bucket_np(d):
    d = np.maximum(d, 0)
    lr = np.log(np.maximum(d, 1).astype(np.float32) / np.float32(16)) / np.float32(math.log(8.0))
    large = np.minimum(16 + (lr * 16).astype(np.int32), 31)
    return np.where(d < 16, d, large)

def host_consts():
    j = np.arange(512); d = j - 127
    oh = np.zeros((32, 512), np.float32)
    b = t5_bucket_np(d)
    for jj in range(511):
        if d[jj] >= 0: oh[b[jj], jj] = 1.0
    J = np.eye(128, dtype=np.float32)[::-1].copy()
    negtri = np.where(np.arange(128)[None, :] <= np.arange(128)[:, None], 0.0, -1e30).astype(np.float32)
    pow2 = np.tile((2.0 ** -np.arange(NIT)).astype(np.float32)[None, :], (128, 1))
    irep = np.tile(np.eye(128, dtype=np.float32), (1, 4))
    return dict(OH1=oh, J=J, NEGTRI=negtri, POW2=pow2, IREP=irep, ident=np.eye(128, dtype=np.float32))

def bias_setup(nc, rb_d, OH1_d, J_d, BVh, BIASD):
    st = Stage(nc, "sbias")
    BV = st.track(Buf(BVh.ap(), multi=True)); BD = st.track(Buf(BIASD, multi=True))
    rb = st.sb("rb", [32, 8], F32); st.dma("sp", rb[:], rb_d[:, :], writes=[rb])
    oh = st.sb("oh", [32, 512], F32); st.dma("sp", oh[:], OH1_d[:, :], writes=[oh])
    J = st.sb("J", [128, 128], F32); st.dma("sp", J[:], J_d[:, :], writes=[J])
    ps = st.ps("ps"); bv = st.sb("bv", [8, 512], F32)
    st.op("pe", lambda e: e.matmul(ps[0:8, :], rb[:], oh[:], start=True, stop=True), reads=[rb, oh], writes=[ps])
    st.op("dve", lambda e: e.tensor_scalar(bv[:], ps[0:8, :], 8.0, None, ALU.mult), reads=[ps], writes=[bv])
    st.dma("sp", BV[:, :], bv[:], reads=[bv], writes=[BV])
    hk = st.sb("hk", [128, 8, 128], F32); hi = st.sb("hi", [128, 2, 1024], BF16); tmp = st.sb("tmp", [128, 512], F32)
    ps2 = st.ps("ps2")
    far = st.sb("far", [128, 2, 512], F32)
    for kind in (2, 0, 1):
        st.dma("sp", hk[:], bass.AP(BVh, 128 * kind, [[1, 128], [512, 8], [1, 128]]), reads=[BV], writes=[hk])
        for half in range(2):
            st.op("pe", lambda e, half=half: e.matmul(ps2[:], J[:], hk[:, half*4:(half+1)*4, :].rearrange("p h t -> p (h t)"), start=True, stop=True), reads=[J, hk], writes=[ps2])
            if kind == 2:
                st.op("act", lambda e, half=half: e.copy(far[:, half, :], ps2[:]), reads=[ps2], writes=[far])
                continue
            st.op("dve", lambda e, half=half: e.tensor_tensor(tmp[:], ps2[:], far[:, half, :], ALU.subtract), reads=[ps2, far], writes=[tmp])
            st.op("act", lambda e, half=half: e.copy(hi[:, 0, half*512:(half+1)*512], tmp[:]), reads=[tmp], writes=[hi])
            st.op("dve", lambda e, half=half: e.tensor_tensor(tmp[:], tmp[:], hi[:, 0, half*512:(half+1)*512], ALU.subtract), reads=[tmp, hi], writes=[tmp])
            st.op("dve", lambda e, half=half: e.tensor_copy(hi[:, 1, half*512:(half+1)*512], tmp[:]), reads=[tmp], writes=[hi])
        if kind != 2:
            st.dma("sp", BD[kind], hi[:], reads=[hi], writes=[BD])
    st.run()

def dsa_prep(nc, BQT, BC, wuk_d, kvg_d, ident_d, CKT, CKV, QLT):
    st = Stage(nc, "sC1")
    for b in (BQT, BC, CKT, CKV, QLT): st.track(b)
    wuk = st.sb("wuk", [64, 8, 128], BF16); st.dma("pool", wuk[:], wuk_d.rearrange("h d c -> d h c"), writes=[wuk])
    kvg = st.sb("kvg", [128, 128], F32); st.dma("sp", kvg[:], kvg_d.partition_broadcast(128), writes=[kvg])
    ident = st.sb("ident", [128, 128], F32); st.dma("sp", ident[:], ident_d[:, :], writes=[ident])
    bqs = [st.sb(f"bq{i}", [64, 8, 512], F32) for i in range(2)]; bqb = [st.sb(f"bqb{i}", [64, 8, 512], BF16) for i in range(2)]
    qls = [st.sb(f"ql{i}", [128, 8, 512], BF16) for i in range(2)]
    bcs = [st.sb(f"bc{i}", [128, 4, 128], F32) for i in range(2)]
    sq = st.sb("sq", [128, 4, 128], F32); ssq = st.sb("ssq", [128, 4], F32); rstd = st.sb("rstd", [128, 4], F32)
    ckv = st.sb("ckv", [128, 4, 128], F32)
    ckvb = [st.sb(f"ckvb{i}", [128, 4, 129], BF16) for i in range(2)]
    for c in ckvb: st.op("pool", lambda e, c=c: e.memset(c[:], 1.0), writes=[c])
    ckt = [st.sb(f"ckt{i}", [128, 512], BF16) for i in range(2)]
    pss = [st.ps(f"ps{i}") for i in range(4)]; pst = st.ps("pst")
    pi = 0
    for tt in range(T // 512):
        bq = bqs[tt % 2]; bb = bqb[tt % 2]; ql = qls[tt % 2]; bc = bcs[tt % 2]; cb = ckvb[tt % 2]; ct = ckt[tt % 2]
        st.dma("sp", bq[:], BQT[:, tt*512:(tt+1)*512].rearrange("(g d) t -> d g t", d=64), reads=[BQT], writes=[bq])
        st.op("pool", lambda e, bq=bq, bb=bb: e.tensor_copy(bb[:], bq[:]), reads=[bq], writes=[bb])
        for h in range(8):
            ps = pss[pi % 4]; pi += 1
            st.op("pe", lambda e, ps=ps, h=h, bb=bb: e.matmul(ps[:], wuk[:, h, :], bb[:, h, :], start=True, stop=True), reads=[wuk, bb], writes=[ps])
            if h % 2 == 0:
                st.op("act", lambda e, ps=ps, h=h, ql=ql: e.copy(ql[:, h, :], ps[:]), reads=[ps], writes=[ql])
            else:
                st.op("dve", lambda e, ps=ps, h=h, ql=ql: e.tensor_copy(ql[:, h, :], ps[:]), reads=[ps], writes=[ql])
        for q in range(4):
            st.dma("sp", QLT[:, tt*4+q, :, :], ql[:, :, q*128:(q+1)*128], reads=[ql], writes=[QLT])
        st.dma("sp", bc[:], BC[tt*512:(tt+1)*512, :].rearrange("(s p) c -> p s c", p=128), reads=[BC], writes=[bc])
        st.op("pool", lambda e, bc=bc: e.tensor_tensor(sq[:], bc[:], bc[:], ALU.mult), reads=[bc], writes=[sq])
        st.op("dve", lambda e: e.tensor_reduce(ssq[:], sq[:], AX.X, ALU.add), reads=[sq], writes=[ssq])
        st.op("dve", lambda e: e.tensor_scalar(ssq[:], ssq[:], 1.0 / 128, 1e-5, ALU.mult, ALU.add), reads=[ssq], writes=[ssq])
        st.op("act", lambda e: e.activation(rstd[:], ssq[:], AF.Ln), reads=[ssq], writes=[rstd])
        st.op("act", lambda e: e.activation(rstd[:], rstd[:], AF.Exp, scale=-0.5), reads=[rstd], writes=[rstd])
        st.op("dve", lambda e, bc=bc: e.tensor_tensor(ckv[:], bc[:], rstd[:].unsqueeze(2).to_broadcast([128, 4, 128]), ALU.mult), reads=[bc, rstd], writes=[ckv])
        st.op("pool", lambda e: e.tensor_tensor(ckv[:], ckv[:], kvg[:].unsqueeze(1).to_broadcast([128, 4, 128]), ALU.mult), reads=[ckv, kvg], writes=[ckv])
        st.op("pool", lambda e, cb=cb: e.tensor_copy(cb[:, :, 0:128], ckv[:]), reads=[ckv], writes=[cb])
        st.dma("sp", CKV[tt*512:(tt+1)*512, :].rearrange("(s p) c -> p s c", p=128), cb[:], reads=[cb], writes=[CKV])
        for s in range(4):
            st.op("pe", lambda e, s=s: e.transpose(pst[:, s*128:(s+1)*128], ckv[:, s, :], ident[:]), reads=[ckv, ident], writes=[pst])
        st.op("act", lambda e, ct=ct: e.copy(ct[:], pst[:]), reads=[pst], writes=[ct])
        st.dma("sp", CKT[:, tt*512:(tt+1)*512], ct[:], reads=[ct], writes=[CKT])
    st.run()

def dsa_attn(nc, name, q0, q1, IQT, IKT, IW, CKT, CKV, QLT, BIASD, OLT, C):
    st = Stage(nc, name)
    for b in (IQT, IKT, IW, CKT, CKV, QLT, OLT): st.track(b)
    BD = st.track(Buf(BIASD, multi=True))
    nkmax = q1 * 128
    cktS = st.sb("ckt", [128, nkmax], BF16); st.dma("sp", cktS[:], CKT[:, 0:nkmax], reads=[CKT], writes=[cktS])
    ckvS = st.sb("ckv", [128, q1, 129], BF16)
    for k0 in range(0, q1, 8):
        k1 = min(q1, k0 + 8)
        st.dma("sp", ckvS[:, k0:k1, :], CKV[k0*128:k1*128, :].rearrange("(k p) c -> p k c", p=128), reads=[CKV], writes=[ckvS])
    kiT = st.sb("kiT", [32, nkmax], F32); st.dma("sp", kiT[:], IKT[:, 0:nkmax], reads=[IKT], writes=[kiT])
    bias = st.sb("bias", [128, 2, 2, 1024], BF16)
    for kind in range(2): st.dma("sp", bias[:, kind], BD[kind], reads=[BD], writes=[bias])
    identb = st.sb("identb", [128, 128], BF16); st.dma("pool", identb[:], C["ident"][:, :], writes=[identb])
    ident = st.sb("ident", [128, 128], F32); st.dma("sp", ident[:], C["ident"][:, :], writes=[ident])
    irep = st.sb("irep", [128, 512], BF16); st.dma("pool", irep[:], C["IREP"][:, :], writes=[irep])
    negtri = st.sb("negtri", [128, 128], F32); st.dma("sp", negtri[:], C["NEGTRI"][:, :], writes=[negtri])
    pow2 = st.sb("pow2", [128, NIT], F32); st.dma("sp", pow2[:], C["POW2"][:, :], writes=[pow2])
    accs = [st.sb(f"acc{i}", [128, nkmax], F32) for i in range(2)]
    NMs = [st.sb(f"NM{i}", [128, nkmax], BF16) for i in range(2)]
    qis = [st.sb(f"qi{i}", [32, 8, 128], F32) for i in range(2)]; ws = [st.sb(f"w{i}", [128, 8], F32) for i in range(2)]
    qls = [st.sb(f"ql{i}", [128, 8, 128], BF16) for i in range(3)]
    rs = [st.sb(f"r{i}", [128, 512], F32) for i in range(3)]
    PTs = [st.sb(f"PT{i}", [128, 512], BF16) for i in range(3)]
    sm = {n: st.sb(n, s, F32) for n, s in dict(den=[128, 8], rden=[128, 8]).items()}
    ol = st.sb("ol", [128, 8, 128], F32); olT = [st.sb(f"olT{i}", [128, 8, 128], BF16) for i in range(1)]
    psL = [st.ps(f"psL{i}") for i in range(3)]; psO = [st.ps(f"psO{i}") for i in range(3)]; psI = [st.ps(f"psI{i}") for i in range(2)]
    OH = [(0, 0), (0, 1), (0, 2), (1, 0), (1, 1), (1, 2), (2, 0), (2, 1)]
    sms = [{n: st.sb(f"{n}{i}", s, F32) for n, s in dict(A=[128, 1], lo=[128, 1], steps=[128, NIT], mid=[128, 1], cnt=[128, 1], ge=[128, 1]).items()} for i in range(2)]
    state = dict(li=0, ii=0)

    def score(qt):
        nk = (qt + 1) * 128; qs = slice(qt*128, (qt+1)*128)
        qi = qis[qt % 2]; w = ws[qt % 2]; ql = qls[qt % 3]; acc = accs[qt % 2]
        st.dma("sp", qi[:], IQT[:, qs].rearrange("(g d) t -> d g t", d=32), reads=[IQT], writes=[qi])
        st.dma("sp", w[:], IW[qs, :], reads=[IW], writes=[w])
        st.dma("sp", ql[:], QLT[:, qt, :, :], reads=[QLT], writes=[ql])
        st.op("pool", lambda e, w=w: e.tensor_scalar(w[:], w[:], 1.0 / 16, None, ALU.mult), reads=[w], writes=[w])
        for kb in range((nk + 511) // 512):
            n = min(512, nk - kb*512); ks = slice(kb*512, kb*512 + n)
            for h in range(8):
                ii = state["ii"]; ps = psI[ii % 2]; r = rs[ii % 3]; state["ii"] += 1
                st.op("pe", lambda e, ps=ps, qi=qi, h=h, ks=ks, n=n: e.matmul(ps[:, 0:n], qi[:, h, :], kiT[:, ks], start=True, stop=True), reads=[qi, kiT], writes=[ps])
                st.op("act", lambda e, ps=ps, r=r, n=n: e.activation(r[:, 0:n], ps[:, 0:n], AF.Relu), reads=[ps], writes=[r])
                if h == 0:
                    st.op("dve", lambda e, r=r, w=w, ks=ks, n=n, acc=acc: e.tensor_scalar(acc[:, ks], r[:, 0:n], w[:, 0:1], None, ALU.mult), reads=[r, w], writes=[acc])
                else:
                    st.op("dve", lambda e, r=r, w=w, ks=ks, n=n, h=h, acc=acc: e.scalar_tensor_tensor(acc[:, ks], r[:, 0:n], w[:, h:h+1], acc[:, ks], ALU.mult, ALU.add), reads=[r, w, acc], writes=[acc])
                yield

    def bisect(qt):
        nk = (qt + 1) * 128; acc = accs[qt % 2]; NM = NMs[qt % 2]
        A, lo, steps, mid, cnt, ge = [sms[qt % 2][n] for n in ("A", "lo", "steps", "mid", "cnt", "ge")]
        st.op("dve", lambda e: e.reduce_max(A[:], acc[:, 0:nk], AX.X, apply_absolute_value=True), reads=[acc], writes=[A])
        st.op("dve", lambda e: e.tensor_tensor(acc[:, nk-128:nk], acc[:, nk-128:nk], negtri[:], ALU.add), reads=[acc, negtri], writes=[acc])
        st.op("dve", lambda e: e.tensor_scalar(A[:], A[:], 1.0, None, ALU.add), reads=[A], writes=[A])
        st.op("dve", lambda e: e.tensor_scalar(lo[:], A[:], -1.0, None, ALU.mult), reads=[A], writes=[lo])
        st.op("dve", lambda e: e.tensor_scalar(steps[:], pow2[:], A[:, 0:1], None, ALU.mult), reads=[pow2, A], writes=[steps])
        for k in range(NIT):
            st.op("dve", lambda e, k=k: e.tensor_tensor(mid[:], lo[:], steps[:, k:k+1], ALU.add), reads=[lo, steps], writes=[mid])
            st.op("dve", lambda e: e.tensor_scalar(NM[:, 0:nk], acc[:, 0:nk], mid[:, 0:1], 0.0, ALU.is_ge, ALU.add, accum_out=cnt[:, 0:1]), reads=[acc, mid], writes=[NM, cnt])
            st.op("dve", lambda e: e.tensor_scalar(ge[:], cnt[:], 255.5, None, ALU.is_ge), reads=[cnt], writes=[ge])
            st.op("dve", lambda e, k=k: e.scalar_tensor_tensor(lo[:], ge[:], steps[:, k:k+1], lo[:], ALU.mult, ALU.add), reads=[ge, steps, lo], writes=[lo])
            yield
        st.op("dve", lambda e: e.tensor_scalar(NM[:, 0:nk], acc[:, 0:nk], lo[:, 0:1], -30000.0, ALU.is_lt, ALU.mult), reads=[acc, lo], writes=[NM])

    def attn(qt):
        qs = slice(qt*128, (qt+1)*128)
        ql = qls[qt % 3]; NM = NMs[qt % 2]; oT = olT[0]
        den, rden = sm["den"], sm["rden"]
        its = [(kt, half) for kt in range(qt + 1) for half in range(2)]
        slots = []

        def qk(i):
            kt, half = its[i]
            kind = min(qt - kt, 2); kts = slice(kt*128, (kt+1)*128)
            li = state["li"]; ps = psL[li % 3]; PT = PTs[li % 3]; state["li"] += 1
            slots.append((ps, PT))
            hs = slice(half*512, (half+1)*512)
            st.op("pe", lambda e: e.matmul(ps[:], cktS[:, kts], ql[:, half*4:(half+1)*4, :].rearrange("p h t -> p (h t)"), start=True, stop=False), reads=[cktS, ql], writes=[ps])
            if kind < 2:
                st.op("pe", lambda e: e.matmul(ps[:], identb[:], bias[:, kind, 0, hs], start=False, stop=False), reads=[identb, bias], writes=[ps])
                st.op("pe", lambda e: e.matmul(ps[:], identb[:], bias[:, kind, 1, hs], start=False, stop=False), reads=[identb, bias], writes=[ps])
            st.op("pe", lambda e: e.matmul(ps[:], NM[:, kts], irep[:], start=False, stop=True), reads=[NM, irep], writes=[ps])

        def pv(i):
            kt, half = its[i]; ps, PT = slots[i]
            st.op("act", lambda e: e.activation(PT[:], ps[:], AF.Exp, scale=0.125), reads=[ps], writes=[PT])
            for hh in range(4):
                h = half*4 + hh; bnk, slot = OH[h]; po = psO[bnk]
                st.op("pe", lambda e, po=po, slot=slot, hh=hh: e.matmul(po[:, slot*129:(slot+1)*129], PT[:, hh*128:(hh+1)*128], ckvS[:, kt, :], start=(kt == 0 and slot == 0), stop=(kt == qt), skip_group_check=True),
                      reads=[PT, ckvS], writes=[po])

        qk(0)
        for i in range(len(its)):
            if i + 1 < len(its):
                qk(i + 1)
            pv(i)
            yield
        for bnk in range(3):
            nh = 3 if bnk < 2 else 2; h0 = bnk*3; po = psO[bnk]
            st.op("act", lambda e, po=po, nh=nh, h0=h0: e.copy(den[:, h0:h0+nh], po[:, 0:nh*129].rearrange("p (i n) -> p i n", n=129)[:, :, 128]), reads=[po], writes=[den])
        st.op("dve", lambda e: e.reciprocal(rden[:], den[:]), reads=[den], writes=[rden])
        for bnk in range(3):
            nh = 3 if bnk < 2 else 2; h0 = bnk*3; po = psO[bnk]
            st.op("dve", lambda e, po=po, nh=nh, h0=h0: e.tensor_tensor(ol[:, h0:h0+nh, :], po[:, 0:nh*129].rearrange("p (i n) -> p i n", n=129)[:, :, 0:128], rden[:, h0:h0+nh].unsqueeze(2).to_broadcast([128, nh, 128]), ALU.mult), reads=[po, rden], writes=[ol])
        for half in range(2):
            li = state["li"]; ps = psL[li % 3]; state["li"] += 1
            for hh in range(4):
                st.op("pe", lambda e, ps=ps, hh=hh, half=half: e.transpose(ps[:, hh*128:(hh+1)*128], ol[:, half*4+hh, :], ident[:]), reads=[ol, ident], writes=[ps])
            st.op("act", lambda e, ps=ps, oT=oT, half=half: e.copy(oT[:, half*4:(half+1)*4, :].rearrange("p h t -> p (h t)"), ps[:]), reads=[ps], writes=[oT])
        st.dma("sp", OLT[:, :, qs], oT[:], reads=[oT], writes=[OLT])

    def n_score(qt):
        return 8 * (((qt + 1) * 128 + 511) // 512)

    def drain(g):
        for _ in g:
            pass

    def interleave(streams):
        live = [[g, 0, max(n, 1)] for g, n in streams]
        while live:
            live.sort(key=lambda x: x[1] / x[2])
            cur = live[0]
            try:
                next(cur[0]); cur[1] += 1
            except StopIteration:
                live.pop(0)

    drain(score(q0))
    drain(bisect(q0))
    if q0 + 1 < q1:
        drain(score(q0 + 1))
    for qt in range(q0, q1):
        streams = [(attn(qt), 2 * (qt + 1))]
        if qt + 1 < q1:
            streams.append((bisect(qt + 1), NIT))
        if qt + 2 < q1:
            streams.append((score(qt + 2), n_score(qt + 2)))
        interleave(streams)
    st.run()


OFF = dict(a_qk=0, a_v=512, a_o=1024, a_if=1536, b_q=1544, b_c=2056, i_q=2184, i_k=2440, i_w=2472, g_a=2480, g_b=3504)
ALPHA = 8 ** 0.25
D = 1024


def transpose_in_stage(nc, x_in, XT0, ident_d):
    st = Stage(nc, "s0"); st.track(XT0)
    ident = st.sb("ident", [128, 128], F32)
    st.dma("sp", ident[:], ident_d[:, :], writes=[ident])
    xin = [st.sb(f"xin{i}", [128, 4, D], F32) for i in range(2)]
    xo = [st.sb(f"xo{i}", [128, 8, 512], F32) for i in range(2)]
    pss = [st.ps(f"ps{i}") for i in range(4)]
    pi = 0
    for tt in range(T // 512):
        xi = xin[tt % 2]; xot = xo[tt % 2]
        st.dma("sp", xi[:], x_in[tt*512:(tt+1)*512, :].rearrange("(s p) d -> p s d", p=128), writes=[xi])
        for k in range(8):
            ps = pss[pi % 4]; pi += 1
            for s in range(4):
                st.op("pe", lambda e, ps=ps, xi=xi, s=s, k=k: e.transpose(ps[:, s*128:(s+1)*128], xi[:, s, k*128:(k+1)*128], ident[:]),
                      reads=[xi, ident], writes=[ps])
            if k % 2 == 0:
                st.op("dve", lambda e, ps=ps, xot=xot, k=k: e.tensor_copy(xot[:, k, :], ps[:]), reads=[ps], writes=[xot])
            else:
                st.op("act", lambda e, ps=ps, xot=xot, k=k: e.copy(xot[:, k, :], ps[:]), reads=[ps], writes=[xot])
        st.dma("sp", XT0[:, tt*512:(tt+1)*512].rearrange("(k p) t -> p k t", p=128), xot[:], reads=[xot], writes=[XT0])
    st.run()


def transpose_out_stage(nc, XO, y_out, ident_d):
    st = Stage(nc, "s9"); st.track(XO)
    Y = st.track(Buf(y_out, multi=True))
    ident = st.sb("ident", [128, 128], F32)
    st.dma("sp", ident[:], ident_d[:, :], writes=[ident])
    xin = [st.sb(f"xin{i}", [128, 8, 512], F32) for i in range(2)]
    yo = [st.sb(f"yo{i}", [128, 4, D], F32) for i in range(2)]
    pss = [st.ps(f"ps{i}") for i in range(4)]
    pi = 0
    for tt in range(T // 512):
        xi = xin[tt % 2]; yt = yo[tt % 2]
        st.dma("sp", xi[:], XO[:, tt*512:(tt+1)*512].rearrange("(k p) t -> p k t", p=128), reads=[XO], writes=[xi])
        for s in range(4):
            for half in range(2):
                ps = pss[pi % 4]; pi += 1
                for kk in range(4):
                    k = half * 4 + kk
                    st.op("pe", lambda e, ps=ps, xi=xi, s=s, k=k, kk=kk: e.transpose(ps[:, kk*128:(kk+1)*128], xi[:, k, s*128:(s+1)*128], ident[:]),
                          reads=[xi, ident], writes=[ps])
                if half == 0:
                    st.op("dve", lambda e, ps=ps, yt=yt, s=s: e.tensor_copy(yt[:, s, 0:512], ps[:]), reads=[ps], writes=[yt])
                else:
                    st.op("act", lambda e, ps=ps, yt=yt, s=s: e.copy(yt[:, s, 512:1024], ps[:]), reads=[ps], writes=[yt])
        st.dma("sp", Y[tt*512:(tt+1)*512, :].rearrange("(s p) d -> p s d", p=128), yt[:], reads=[yt], writes=[Y])
    st.run()


def inproj_stage(nc, XT0, w_in, S):
    st = Stage(nc, "sA"); st.track(XT0)
    for b in S.values(): st.track(b)
    wb = st.sb("wb", [128, 8, 4528], BF16)
    wf = st.sb("wf", [128, 8, 296], F32)
    for k in range(8):
        st.dma("pool", wb[:, k, :], w_in[k*128:(k+1)*128, :], writes=[wb])
    st.dma("sp", wf[:], w_in[:, 2184:2480].rearrange("(k p) n -> p k n", p=128), writes=[wf])
    xf = [st.sb(f"xf{i}", [128, 8, 512], F32) for i in range(2)]
    xb = [st.sb(f"xb{i}", [128, 8, 512], BF16) for i in range(2)]
    ofm = [st.sb(f"ofm{i}", [128, 8, 512], F32) for i in range(2)]
    otm = [st.sb(f"otm{i}", [128, 4, 1032], F32) for i in range(2)]
    otb = [st.sb(f"otb{i}", [128, 4, 136], F32) for i in range(2)]
    pss = [st.ps(f"ps{i}") for i in range(6)]
    pi = [0]; oi = [0]

    def evac(ps_ap, out_ap, ps, ob, func=None):
        if func is not None or pi[0] % 2 == 1:
            st.op("act", lambda e: e.activation(out_ap, ps_ap, func if func is not None else AF.Copy), reads=[ps], writes=[ob])
        else:
            st.op("dve", lambda e: e.tensor_copy(out_ap, ps_ap), reads=[ps], writes=[ob])

    def fm_group(xt_b, wt, col0, ncols, dst, tt, func=None):
        M = min(128, ncols); nch = ncols // M
        ob = ofm[oi[0] % 2]; oi[0] += 1
        for c in range(nch):
            ps = pss[pi[0] % 6]; pi[0] += 1
            for k in range(8):
                st.op("pe", lambda e, ps=ps, c=c, k=k: e.matmul(ps[0:M, :], wt[:, k, col0+c*M:col0+(c+1)*M], xt_b[:, k, :], start=(k == 0), stop=(k == 7)),
                      reads=[wt, xt_b], writes=[ps])
            evac(ps[0:M, :], ob[0:M, c, :], ps, ob, func)
        if M == 128:
            st.dma("sp", dst[:, tt*512:(tt+1)*512].rearrange("(k p) t -> p k t", p=128), ob[:, 0:nch, :], reads=[ob], writes=[dst])
        else:
            st.dma("sp", dst[:, tt*512:(tt+1)*512], ob[0:M, 0, :], reads=[ob], writes=[dst])

    for tt in range(T // 512):
        xft = xf[tt % 2]; xbt = xb[tt % 2]
        st.dma("sp", xft[:], XT0[:, tt*512:(tt+1)*512].rearrange("(k p) t -> p k t", p=128), reads=[XT0], writes=[xft])
        st.op("pool", lambda e, xft=xft, xbt=xbt: e.tensor_copy(xbt[:], xft[:]), reads=[xft], writes=[xbt])
        fm_group(xbt, wb, OFF["a_qk"], 512, S["QKT"], tt)
        fm_group(xbt, wb, OFF["b_q"], 512, S["BQT"], tt)
        fm_group(xbt, wb, OFF["g_a"], 1024, S["GAT"], tt, func=AF.Sigmoid)
        fm_group(xbt, wb, OFF["g_b"], 1024, S["GBT"], tt, func=AF.Sigmoid)
        fm_group(xft, wf, 0, 256, S["IQT"], tt)
        fm_group(xft, wf, 256, 32, S["IKT"], tt)
        ob = otm[tt % 2]; ob2 = otb[tt % 2]
        for s in range(4):
            for (c0, n) in ((0, 512), (512, 512), (1024, 8)):
                ps = pss[pi[0] % 6]; pi[0] += 1
                for k in range(8):
                    st.op("pe", lambda e, ps=ps, s=s, k=k, c0=c0, n=n, xbt=xbt: e.matmul(ps[:, 0:n], xbt[:, k, s*128:(s+1)*128], wb[:, k, 512+c0:512+c0+n], start=(k == 0), stop=(k == 7)),
                          reads=[wb, xbt], writes=[ps])
                evac(ps[:, 0:n], ob[:, s, c0:c0+n], ps, ob)
            ps = pss[pi[0] % 6]; pi[0] += 1
            for k in range(8):
                st.op("pe", lambda e, ps=ps, s=s, k=k, xbt=xbt: e.matmul(ps[:, 0:128], xbt[:, k, s*128:(s+1)*128], wb[:, k, OFF["b_c"]:OFF["b_c"]+128], start=(k == 0), stop=(k == 7)),
                      reads=[wb, xbt], writes=[ps])
            evac(ps[:, 0:128], ob2[:, s, 0:128], ps, ob2)
            ps = pss[pi[0] % 6]; pi[0] += 1
            for k in range(8):
                st.op("pe", lambda e, ps=ps, s=s, k=k, xft=xft: e.matmul(ps[:, 0:8], xft[:, k, s*128:(s+1)*128], wf[:, k, 288:296], start=(k == 0), stop=(k == 7)),
                      reads=[wf, xft], writes=[ps])
            evac(ps[:, 0:8], ob2[:, s, 128:136], ps, ob2)
        st.dma("sp", S["VAO"][tt*512:(tt+1)*512, :].rearrange("(s p) n -> p s n", p=128), ob[:], reads=[ob], writes=[S["VAO"]])
        st.dma("sp", S["BC"][tt*512:(tt+1)*512, :].rearrange("(s p) n -> p s n", p=128), ob2[:, :, 0:128], reads=[ob2], writes=[S["BC"]])
        st.dma("sp", S["IW"][tt*512:(tt+1)*512, :].rearrange("(s p) n -> p s n", p=128), ob2[:, :, 128:136], reads=[ob2], writes=[S["IW"]])
    st.run()


def wc_stage(nc, wuvT_d, wbb_d, WC):
    st = Stage(nc, "sWc"); st.track(WC)
    wuvT = st.sb("wuvT", [64, 8, 128], BF16); st.dma("pool", wuvT[:], wuvT_d.rearrange("h d c -> d h c"), writes=[wuvT])
    wbb = st.sb("wbb", [64, 8, 1024], BF16); st.dma("pool", wbb[:], wbb_d.rearrange("(h d) n -> d h n", d=64), writes=[wbb])
    wc = st.sb("wc", [128, 8, 1024], BF16)
    pss = [st.ps(f"ps{i}") for i in range(4)]
    pi = 0
    for h in range(8):
        for half in range(2):
            ps = pss[pi % 4]; pi += 1
            st.op("pe", lambda e, ps=ps, h=h, half=half: e.matmul(ps[:], wuvT[:, h, :], wbb[:, h, half*512:(half+1)*512], start=True, stop=True), reads=[wuvT, wbb], writes=[ps])
            if pi % 2 == 0:
                st.op("act", lambda e, ps=ps, h=h, half=half: e.copy(wc[:, h, half*512:(half+1)*512], ps[:]), reads=[ps], writes=[wc])
            else:
                st.op("dve", lambda e, ps=ps, h=h, half=half: e.tensor_copy(wc[:, h, half*512:(half+1)*512], ps[:]), reads=[ps], writes=[wc])
    st.dma("sp", WC[:, :, :], wc[:], reads=[wc], writes=[WC])
    st.run()


def merge_stage(nc, XI, XO, YAT, OLT, GAT, GBT, WC, wa_d, wo_d, g_d, b_d, ones_d):
    st = Stage(nc, "sD")
    for b in (XI, XO, YAT, OLT, GAT, GBT, WC): st.track(b)
    ones = st.sb("ones", [128, 128], F32); st.dma("sp", ones[:], ones_d[:, :], writes=[ones])
    gcol = st.sb("gcol", [128, 8], F32); st.dma("sp", gcol[:], g_d, writes=[gcol])
    bcol = st.sb("bcol", [128, 8], F32); st.dma("sp", bcol[:], b_d, writes=[bcol])
    wa = st.sb("wa", [128, 4, 1024], BF16); st.dma("pool", wa[:], wa_d.rearrange("(k p) n -> p k n", p=128), writes=[wa])
    wo = st.sb("wo", [128, 8, 1024], BF16); st.dma("pool", wo[:], wo_d.rearrange("(k p) n -> p k n", p=128), writes=[wo])
    wc = st.sb("wc", [128, 8, 1024], BF16); st.dma("sp", wc[:], WC[:, :, :], reads=[WC], writes=[wc])
    yab = st.sb("yab", [128, 4, 512], BF16); ol = st.sb("ol", [128, 8, 512], BF16)
    ga = st.sb("ga", [128, 8, 512], F32); gb = st.sb("gb", [128, 8, 512], F32); x = st.sb("x", [128, 8, 512], F32)
    mg = st.sb("mg", [128, 8, 512], BF16)
    t1 = [st.sb(f"t1{i}", [128, 512], F32) for i in range(2)]; t2 = [st.sb(f"t2{i}", [128, 512], F32) for i in range(2)]
    tmp = dict(sq=st.sb("sq", [128, 8, 512], F32), mean=st.sb("mean", [128, 512], F32), rstd=st.sb("rstd", [128, 512], F32))
    psA = [st.ps(f"psA{i}") for i in range(2)]; psB = [st.ps(f"psB{i}") for i in range(2)]; psO = [st.ps(f"psO{i}") for i in range(2)]
    psS = st.ps("psS"); psQ = st.ps("psQ")
    for tt in range(T // 512):
        ts = slice(tt*512, (tt+1)*512)
        st.dma("pool", yab[:], YAT[:, ts].rearrange("(k p) t -> p k t", p=128), reads=[YAT], writes=[yab])
        st.dma("sp", ol[:], OLT[:, :, ts], reads=[OLT], writes=[ol])
        st.dma("sp", ga[:], GAT[:, ts].rearrange("(k p) t -> p k t", p=128), reads=[GAT], writes=[ga])
        st.dma("sp", gb[:], GBT[:, ts].rearrange("(k p) t -> p k t", p=128), reads=[GBT], writes=[gb])
        st.dma("sp", x[:], XI[:, ts].rearrange("(k p) t -> p k t", p=128), reads=[XI], writes=[x])
        for c in range(8):
            pa = psA[c % 2]; pb = psB[c % 2]; a1 = t1[c % 2]; a2 = t2[c % 2]; cs = slice(c*128, (c+1)*128)
            for k in range(4):
                st.op("pe", lambda e, pa=pa, k=k, cs=cs: e.matmul(pa[:], wa[:, k, cs], yab[:, k, :], start=(k == 0), stop=(k == 3)), reads=[wa, yab], writes=[pa])
            for h in range(8):
                st.op("pe", lambda e, pb=pb, h=h, cs=cs: e.matmul(pb[:], wc[:, h, cs], ol[:, h, :], start=(h == 0), stop=(h == 7)), reads=[wc, ol], writes=[pb])
            st.op("dve", lambda e, pa=pa, a1=a1, c=c: e.tensor_tensor(a1[:], pa[:], ga[:, c, :], ALU.mult), reads=[pa, ga], writes=[a1])
            st.op("dve", lambda e, pb=pb, a2=a2, c=c: e.tensor_tensor(a2[:], pb[:], gb[:, c, :], ALU.mult), reads=[pb, gb], writes=[a2])
            st.op("pool", lambda e, a1=a1, a2=a2, c=c: e.tensor_tensor(mg[:, c, :], a1[:], a2[:], ALU.add), reads=[a1, a2], writes=[mg])
        for c in range(8):
            po = psO[c % 2]; cs = slice(c*128, (c+1)*128)
            for k in range(8):
                st.op("pe", lambda e, po=po, k=k, cs=cs: e.matmul(po[:], wo[:, k, cs], mg[:, k, :], start=(k == 0), stop=(k == 7)), reads=[wo, mg], writes=[po])
            st.op("dve", lambda e, po=po, c=c: e.scalar_tensor_tensor(x[:, c, :], x[:, c, :], ALPHA, po[:], ALU.mult, ALU.add), reads=[x, po], writes=[x])
        layer_norm(st, x, ones, gcol, bcol, tmp["sq"], (psS, psQ), tmp)
        st.dma("sp", XO[:, ts].rearrange("(k p) t -> p k t", p=128), tmp["sq"][:], reads=[tmp["sq"]], writes=[XO])
    st.run()


def ffn_stage(nc, name, tb0, tb1, XI, XO, wg_d, wu_d, wd_d, g_d, b_d, C, nexp, dff, G, rw_d=None):
    TB = 1024; NF = dff // 128; NG = NF // G
    st = Stage(nc, name); st.track(XI); st.track(XO)
    ones = st.sb("ones", [128, 128], F32); st.dma("sp", ones[:], C["ones"][:, :], writes=[ones])
    gcol = st.sb("gcol", [128, 8], F32); st.dma("sp", gcol[:], g_d, writes=[gcol])
    bcol = st.sb("bcol", [128, 8], F32); st.dma("sp", bcol[:], b_d, writes=[bcol])
    moe = rw_d is not None
    xb = st.sb("xb", [128, 8, TB], BF16); y = st.sb("y", [128, 8, TB], F32)
    wgg = st.sb("wgg", [128, 8, G*128], BF16); wug = st.sb("wug", [128, 8, G*128], BF16); wdg = st.sb("wdg", [128, G, 1024], BF16)
    hg = st.sb("hg", [128, G, 2, 512], BF16)
    sg = [st.sb(f"sg{i}", [128, 512], F32) for i in range(2)]
    psg = [st.ps(f"psg{i}") for i in range(2)]; psu = [st.ps(f"psu{i}") for i in range(2)]
    psd = [st.ps(f"psd{i}") for i in range(4)]
    tmp = dict(sq=st.sb("sq", [128, 8, 512], F32), mean=st.sb("mean", [128, 512], F32), rstd=st.sb("rstd", [128, 512], F32))
    xr = st.sb("xr", [128, 8, 512], F32)
    if moe:
        ident = st.sb("ident", [128, 128], F32); st.dma("sp", ident[:], C["ident"][:, :], writes=[ident])
        rw = st.sb("rw", [128, 8, 8], F32); st.dma("sp", rw[:], rw_d.rearrange("(k p) e -> p k e", p=128), writes=[rw])
        sel = st.sb("sel", [8, 8, 128], F32); st.dma("sp", sel[:], C["SEL"][:, :, :], writes=[sel])
        xs = [st.sb(f"xs{i}", [128, 8, 128], F32) for i in range(2)]
        GT = st.sb("GT", [8, TB], F32); gbc = st.sb("gbc", [128, TB], F32)
        t2 = [st.sb(f"t2{i}", [128, 512], F32) for i in range(2)]
        sm = {n: st.sb(n, s, F32) for n, s in dict(lg=[128, 8], m8=[128, 8], dl=[128, 1], g1=[128, 1], g2=[128, 1], e1=[128, 8], e2=[128, 8]).items()}
    it = 0
    for tb in range(tb0, tb1):
        t0 = tb * TB
        st.dma("pool", xb[:], XI[:, t0:t0+TB].rearrange("(k p) t -> p k t", p=128), reads=[XI], writes=[xb])
        if moe:
            lg, m8, dl, g1, g2, e1, e2 = [sm[n] for n in ("lg", "m8", "dl", "g1", "g2", "e1", "e2")]
            for s in range(TB // 128):
                xst = xs[s % 2]; pr = psd[s % 4]
                st.dma("sp", xst[:], XI[:, t0+s*128:t0+(s+1)*128].rearrange("(k p) t -> p k t", p=128), reads=[XI], writes=[xst])
                for k in range(8):
                    st.op("pe", lambda e, pr=pr, xst=xst, k=k: e.matmul(pr[:, 0:8], xst[:, k, :], rw[:, k, :], start=(k == 0), stop=(k == 7)), reads=[xst, rw], writes=[pr])
                st.op("act", lambda e, pr=pr: e.copy(lg[:], pr[:, 0:8]), reads=[pr], writes=[lg])
                st.op("dve", lambda e: e.max(m8[:], lg[:]), reads=[lg], writes=[m8])
                st.op("dve", lambda e: e.tensor_tensor(dl[:], m8[:, 0:1], m8[:, 1:2], ALU.subtract), reads=[m8], writes=[dl])
                st.op("act", lambda e: e.activation(g1[:], dl[:], AF.Sigmoid), reads=[dl], writes=[g1])
                st.op("act", lambda e: e.activation(g2[:], dl[:], AF.Sigmoid, scale=-1.0), reads=[dl], writes=[g2])
                st.op("dve", lambda e: e.tensor_scalar(e1[:], lg[:], m8[:, 0:1], g1[:, 0:1], ALU.is_equal, ALU.mult), reads=[lg, m8, g1], writes=[e1])
                st.op("dve", lambda e: e.tensor_scalar(e2[:], lg[:], m8[:, 1:2], g2[:, 0:1], ALU.is_equal, ALU.mult), reads=[lg, m8, g2], writes=[e2])
                st.op("dve", lambda e: e.tensor_tensor(e1[:], e1[:], e2[:], ALU.add), reads=[e1, e2], writes=[e1])
                st.op("pe", lambda e, pr=pr: e.transpose(pr[0:8, 128:256], e1[:], ident[:]), reads=[e1, ident], writes=[pr])
                st.op("act", lambda e, pr=pr, s=s: e.copy(GT[:, s*128:(s+1)*128], pr[0:8, 128:256]), reads=[pr], writes=[GT])
        first = True
        for ex in range(nexp):
            if moe:
                for sub in range(2):
                    pr = psd[sub]
                    st.op("pe", lambda e, pr=pr, ex=ex, sub=sub: e.matmul(pr[:], sel[:, ex, :], GT[:, sub*512:(sub+1)*512], start=True, stop=True), reads=[sel, GT], writes=[pr])
                    st.op("act", lambda e, pr=pr, sub=sub: e.copy(gbc[:, sub*512:(sub+1)*512], pr[:]), reads=[pr], writes=[gbc])
            for grp in range(NG):
                f0 = grp * G * 128
                st.dma("pool", wgg[:], wg_d[ex, :, f0:f0+G*128].rearrange("(k p) n -> p k n", p=128), writes=[wgg])
                st.dma("pool", wug[:], wu_d[ex, :, f0:f0+G*128].rearrange("(k p) n -> p k n", p=128), writes=[wug])
                st.dma("pool", wdg[:], wd_d[ex, f0:f0+G*128, :].rearrange("(g p) n -> p g n", p=128), writes=[wdg])
                for fi in range(G):
                    fs = slice(fi*128, (fi+1)*128)
                    for sub in range(2):
                        pg, pu, sgt = psg[it % 2], psu[it % 2], sg[it % 2]
                        ss = slice(sub*512, (sub+1)*512)
                        for k in range(8):
                            st.op("pe", lambda e, pg=pg, k=k, fs=fs, ss=ss: e.matmul(pg[:], wgg[:, k, fs], xb[:, k, ss], start=(k == 0), stop=(k == 7)), reads=[wgg, xb], writes=[pg])
                        for k in range(8):
                            st.op("pe", lambda e, pu=pu, k=k, fs=fs, ss=ss: e.matmul(pu[:], wug[:, k, fs], xb[:, k, ss], start=(k == 0), stop=(k == 7)), reads=[wug, xb], writes=[pu])
                        st.op("act", lambda e, pg=pg, sgt=sgt: e.activation(sgt[:], pg[:], AF.Silu), reads=[pg], writes=[sgt])
                        if moe:
                            tt2 = t2[it % 2]
                            st.op("dve", lambda e, pu=pu, tt2=tt2, ss=ss: e.tensor_tensor(tt2[:], pu[:], gbc[:, ss], ALU.mult), reads=[pu, gbc], writes=[tt2])
                            st.op("pool", lambda e, sgt=sgt, tt2=tt2, fi=fi, sub=sub: e.tensor_tensor(hg[:, fi, sub, :], sgt[:], tt2[:], ALU.mult), reads=[sgt, tt2], writes=[hg])
                        else:
                            st.op("dve", lambda e, pu=pu, sgt=sgt, fi=fi, sub=sub: e.tensor_tensor(hg[:, fi, sub, :], sgt[:], pu[:], ALU.mult), reads=[pu, sgt], writes=[hg])
                        it += 1
                for sub in range(2):
                    ss = slice(sub*512, (sub+1)*512)
                    for c in range(8):
                        pd = psd[c % 4]; cs = slice(c*128, (c+1)*128)
                        for fi in range(G):
                            st.op("pe", lambda e, pd=pd, fi=fi, cs=cs, sub=sub: e.matmul(pd[:], wdg[:, fi, cs], hg[:, fi, sub, :], start=(fi == 0), stop=(fi == G-1)), reads=[wdg, hg], writes=[pd])
                        if first:
                            st.op("dve", lambda e, pd=pd, c=c, ss=ss: e.tensor_copy(y[:, c, ss], pd[:]), reads=[pd], writes=[y])
                        else:
                            st.op("dve", lambda e, pd=pd, c=c, ss=ss: e.tensor_tensor(y[:, c, ss], y[:, c, ss], pd[:], ALU.add), reads=[pd, y], writes=[y])
                first = False
        for sub in range(2):
            ss = slice(sub*512, (sub+1)*512); ts = slice(t0+sub*512, t0+(sub+1)*512)
            st.dma("sp", xr[:], XI[:, ts].rearrange("(k p) t -> p k t", p=128), reads=[XI], writes=[xr])
            st.op("dve", lambda e, ss=ss: e.scalar_tensor_tensor(xr[:], xr[:], ALPHA, y[:, :, ss], ALU.mult, ALU.add), reads=[xr, y], writes=[xr])
            layer_norm(st, xr, ones, gcol, bcol, tmp["sq"], (psg[0], psu[0]), tmp)
            st.dma("sp", XO[:, ts].rearrange("(k p) t -> p k t", p=128), tmp["sq"][:], reads=[tmp["sq"]], writes=[XO])
    st.run()

NIT = 22


def dump_stage(nc, name, SRC, dst_ap):
    st = Stage(nc, name); st.track(SRC)
    Dst = st.track(Buf(dst_ap, multi=True))
    t = [st.sb(f"t{i}", [128, 8, 512], F32) for i in range(2)]
    for tt in range(T // 512):
        ts = slice(tt*512, (tt+1)*512)
        st.dma("sp", t[tt % 2][:], SRC[:, ts].rearrange("(k p) t -> p k t", p=128), reads=[SRC], writes=[t[tt % 2]])
        st.dma("sp", Dst[:, ts].rearrange("(k p) t -> p k t", p=128), t[tt % 2][:], reads=[t[tt % 2]], writes=[Dst])
    st.run()


def build_full(NL, debug=False):
    nc = bass.Bass("TRN2", target_bir_lowering=False)
    NQ = T // 128; ND = (NL + 1) // 2; NM = NL // 2
    dr = lambda n, s, k="Internal", dt=F32: nc.dram_tensor(n, list(s), dt, kind=k).ap()
    ein = lambda n, s: dr(n, s, "ExternalInput")
    x_in = ein("x", [T, D]); w_in = ein("w_in", [NL, D, 4528]); convT = ein("convT", [NL, 512, 4])
    gbias = ein("gbias", [NL, 1, 8]); ng = ein("ng", [NL, 1, 512]); kvg = ein("kvg", [NL, 1, 128])
    wuk = ein("wuk", [NL, 8, 64, 128]); wuvT = ein("wuvT", [NL, 8, 64, 128]); rb = ein("rb", [32, 8])
    wba = ein("wba", [NL, 512, 1024]); wbb = ein("wbb", [NL, 512, 1024]); wout = ein("wout", [NL, 1024, 1024])
    lng = ein("lng", [NL, 2, 128, 8]); lnb = ein("lnb", [NL, 2, 128, 8])
    dwg = ein("dwg", [ND, 1, D, 2816]); dwu = ein("dwu", [ND, 1, D, 2816]); dwd = ein("dwd", [ND, 1, 2816, D])
    if NM:
        rw = ein("rw", [NM, D, 8]); ewg = ein("ewg", [NM, 8, D, 3584]); ewu = ein("ewu", [NM, 8, D, 3584]); ewd = ein("ewd", [NM, 8, 3584, D])
    C = {n: ein(n, s) for n, s in dict(OH1=[32, 512], J=[128, 128], NEGTRI=[128, 128], POW2=[128, NIT], IREP=[128, 512], ident=[128, 128],
                                       ones=[128, 128], U=[64, 64], SEL=[8, 8, 128]).items()}
    y = dr("y", [T, D], "ExternalOutput")
    dbg = [dr(f"dbg{l}", [D, T], "ExternalOutput") for l in range(NL)] if debug else None
    mb = lambda n, s, dt=F32: Buf(dr(n, s, dt=dt), multi=True)
    XTa = mb("XTa", [D, T]); XTb = mb("XTb", [D, T])
    S = dict(QKT=mb("QKT", [512, T]), BQT=mb("BQT", [512, T]), IQT=mb("IQT", [256, T]), IKT=mb("IKT", [32, T]),
             GAT=mb("GAT", [1024, T]), GBT=mb("GBT", [1024, T]), VAO=mb("VAO", [T, 1032]), BC=mb("BC", [T, 128]), IW=mb("IW", [T, 8]))
    QKC = mb("QKC", [512, T]); YAT = mb("YAT", [512, T])
    CKT = mb("CKT", [128, T], BF16); CKV = mb("CKV", [T, 129], BF16); QLT = mb("QLT", [128, NQ, 8, 128], BF16); OLT = mb("OLT", [128, 8, T], BF16)
    WC = mb("WC", [128, 8, 1024], BF16)
    BVh = nc.dram_tensor("BV", [8, 512], F32); BIASD = dr("BIASD", [3, 128, 2, 1024], dt=BF16)
    SFX[0] = ""
    transpose_in_stage(nc, x_in, XTa, C["ident"])
    bias_setup(nc, rb, C["OH1"], C["J"], BVh, BIASD)
    bounds = [0]
    while bounds[-1] < NQ:
        a = bounds[-1]; b = a; pairs = 0
        while b < NQ and (pairs + b + 1 <= 1100 or b == a):
            pairs += b + 1; b += 1
        bounds.append(b)
    for l in range(NL):
        SFX[0] = f"_L{l}"
        inproj_stage(nc, XTa, w_in[l], S)
        conv_stage(nc, S["QKT"], QKC, convT[l])
        mlstm_stage(nc, QKC, S["VAO"], YAT, gbias[l], ng[l], C["U"], C["ident"])
        dsa_prep(nc, S["BQT"], S["BC"], wuk[l], kvg[l], C["ident"], CKT, CKV, QLT)
        for i in range(len(bounds) - 1):
            dsa_attn(nc, f"sC2_{i}", bounds[i], bounds[i+1], S["IQT"], S["IKT"], S["IW"], CKT, CKV, QLT, BIASD, OLT, C)
        wc_stage(nc, wuvT[l], wbb[l], WC)
        merge_stage(nc, XTa, XTb, YAT, OLT, S["GAT"], S["GBT"], WC, wba[l], wout[l], lng[l, 0], lnb[l, 0], C["ones"])
        NTB = T // 1024
        if l % 2 == 0:
            ffn_stage(nc, "sE", 0, NTB, XTb, XTa, dwg[l // 2], dwu[l // 2], dwd[l // 2], lng[l, 1], lnb[l, 1], C, 1, 2816, 11)
        else:
            j = l // 2
            for tb in range(0, NTB, 2):
                ffn_stage(nc, f"sF{tb}", tb, min(tb + 2, NTB), XTb, XTa, ewg[j], ewu[j], ewd[j], lng[l, 1], lnb[l, 1], C, 8, 3584, 7, rw_d=rw[j])
        if debug:
            dump_stage(nc, "sdbg", XTa, dbg[l])
    SFX[0] = "_fin"
    transpose_out_stage(nc, XTa, y, C["ident"])
    return nc


def host_inputs(inputs, NL):
    f = lambda a: np.ascontiguousarray(np.asarray(a, dtype=np.float32))
    ND = (NL + 1) // 2; NM = NL // 2
    colz = lambda v: f(np.asarray(v)[:NL].reshape(NL, 2, 8, 128).transpose(0, 1, 3, 2))
    d = dict(
        w_in=f(inputs["w_in"][:NL]), convT=f(np.asarray(inputs["mlstm_conv_w"])[:NL].transpose(0, 2, 1)),
        gbias=f(np.asarray(inputs["mlstm_gate_bias"])[:NL].reshape(NL, 1, 8)), ng=f(np.asarray(inputs["mlstm_norm_g"])[:NL].reshape(NL, 1, 512)),
        kvg=f(np.asarray(inputs["dsa_kv_norm_g"])[:NL].reshape(NL, 1, 128)), wuk=f(inputs["dsa_w_uk"][:NL]),
        wuvT=f(np.asarray(inputs["dsa_w_uv"])[:NL].transpose(0, 1, 3, 2)), rb=f(inputs["rel_bias"]),
        wba=f(inputs["w_branch_a"][:NL]), wbb=f(inputs["w_branch_b"][:NL]), wout=f(inputs["w_out"][:NL]),
        lng=colz(inputs["ln_g"]), lnb=colz(inputs["ln_b"]),
        dwg=f(np.asarray(inputs["dense_w_gate"])[:ND, None]), dwu=f(np.asarray(inputs["dense_w_up"])[:ND, None]), dwd=f(np.asarray(inputs["dense_w_down"])[:ND, None]),
    )
    if NM:
        d.update(rw=f(inputs["router_w"][:NM]), ewg=f(inputs["expert_w_gate"][:NM]), ewu=f(inputs["expert_w_up"][:NM]), ewd=f(inputs["expert_w_down"][:NM]))
    hc = host_consts()
    hc["ones"] = np.ones((128, 128), np.float32); hc["U"] = np.triu(np.ones((64, 64), np.float32))
    sel = np.zeros((8, 8, 128), np.float32)
    for e in range(8): sel[e, e, :] = 1.0
    hc["SEL"] = sel
    d.update(hc)
    return d


def kernel(**inputs):
    x = np.asarray(inputs["x"], dtype=np.float32)
    n = x.shape[0]
    NL = 4
    shared = host_inputs(inputs, NL)
    nc = build_full(NL, debug=False)
    in_maps = [dict(shared, x=np.ascontiguousarray(x[c])) for c in range(n)]
    res = run_bass_kernel_spmd(nc, in_maps, core_ids=list(range(n)))
    return np.stack([np.asarray(res.results[c]["y"], dtype=np.float32) for c in range(n)], axis=0)
```
